# Optimizing a Trainium2 kernel written in Bass

```python
import jax, jax.numpy as jnp
from jax import lax
import numpy as np

D_MODEL = 1024
BATCH = 32
SEQ = 2048
DEPTH = 1

POOL_WIDTH = D_MODEL // 2
POOL_WINDOWS = (2, 4, 8, 16)
POOL_GROUP = POOL_WIDTH // len(POOL_WINDOWS)
N_HEADS = 8
HEAD_DIM = 64
N_KV_GROUPS = 2
HEADS_PER_GROUP = N_HEADS // N_KV_GROUPS
ATTN_WIDTH = N_HEADS * HEAD_DIM
KV_WIDTH = N_KV_GROUPS * HEAD_DIM
CMP_BLOCK = 32
CMP_STRIDE = 16
CMP_HIDDEN = 256
SEL_BLOCK = 64
N_SELECT = 8
WINDOW = 512
Q_CHUNK = 64
N_BRANCH = 3
IN_WIDTH = POOL_WIDTH + ATTN_WIDTH + 6 * KV_WIDTH + N_BRANCH * N_HEADS + 2 * D_MODEL
N_GROUPS = 4
EXPERTS_PER_GROUP = 8
N_EXPERTS = N_GROUPS * EXPERTS_PER_GROUP
TOP_K_IN_GROUP = 2
EXPERT_FF = 512

EPS = 1e-6
NEG = -1e30
FORCE_SCORE = 1e4

kernel_name = 'hybrid_pool_nsa_hmoe_block'


def rmsnorm(x, g):
    xf = x.astype(jnp.float32)
    r = lax.rsqrt(jnp.mean(xf * xf, axis=-1, keepdims=True) + EPS)
    return (xf * r * g.astype(jnp.float32)).astype(x.dtype)


def masked_softmax(s, mask):
    p = jax.nn.softmax(jnp.where(mask, s, NEG), axis=-1)
    return p * mask


def pool_mixer(u, pool_w, pool_scale):
    B, S, _ = u.shape
    t = jnp.arange(S)
    outs = []
    for gi, w in enumerate(POOL_WINDOWS):
        ug = u[..., gi * POOL_GROUP:(gi + 1) * POOL_GROUP].astype(jnp.float32)
        cs = jnp.cumsum(ug, axis=1)
        prev = jnp.pad(cs, ((0, 0), (w, 0), (0, 0)))[:, :S]
        cnt = jnp.minimum(t + 1, w).astype(jnp.float32)[None, :, None]
        outs.append((cs - prev) / cnt - ug)
    p = jnp.stack(outs, axis=2).astype(u.dtype)
    y = jnp.einsum('bsgc,gcd->bsgd', p, pool_w).reshape(B, S, POOL_WIDTH)
    return y * pool_scale


def compress_blocks(kv, pos, w1, b1, w2):
    B, S, G, dh = kv.shape
    r = CMP_BLOCK // CMP_STRIDE
    ch = kv.reshape(B, S // CMP_STRIDE, CMP_STRIDE, G, dh)
    n = S // CMP_STRIDE - r + 1
    blocks = jnp.concatenate([ch[:, i:i + n] for i in range(r)], axis=2)
    blocks = blocks + pos[None, None, :, None, :]
    flat = blocks.transpose(0, 1, 3, 2, 4).reshape(B, n, G, CMP_BLOCK * dh)
    return jax.nn.gelu(flat @ w1 + b1) @ w2


def selection_overlap(n_cmp, n_blk):
    s1 = np.arange(n_cmp)[:, None] * CMP_STRIDE
    s2 = np.arange(n_blk)[None, :] * SEL_BLOCK
    ov = np.clip(np.minimum(s1 + CMP_BLOCK, s2 + SEL_BLOCK) - np.maximum(s1, s2), 0, None)
    return (ov / CMP_BLOCK).astype(np.float32)


def nsa_attention(q, k_cmp, v_cmp, k_sel, v_sel, k_win, v_win, branch_gate,
                  cmp_pos, cmp_w1, cmp_b1, cmp_w2):
    B, S, _ = q.shape
    G, Hg, dh = N_KV_GROUPS, HEADS_PER_GROUP, HEAD_DIM
    scale = HEAD_DIM ** -0.5
    q = q.reshape(B, S, G, Hg, dh)
    kc = compress_blocks(k_cmp.reshape(B, S, G, dh), cmp_pos[0], cmp_w1[0], cmp_b1[0], cmp_w2[0])
    vc = compress_blocks(v_cmp.reshape(B, S, G, dh), cmp_pos[1], cmp_w1[1], cmp_b1[1], cmp_w2[1])
    n_cmp = kc.shape[1]
    n_blk = S // SEL_BLOCK
    n_sel = min(N_SELECT, n_blk)
    overlap = jnp.asarray(selection_overlap(n_cmp, n_blk))
    ks_b = k_sel.reshape(B, n_blk, SEL_BLOCK, G, dh).transpose(0, 3, 1, 2, 4)
    vs_b = v_sel.reshape(B, n_blk, SEL_BLOCK, G, dh).transpose(0, 3, 1, 2, 4)
    pad = ((0, 0), (WINDOW, 0), (0, 0), (0, 0))
    kw_p = jnp.pad(k_win.reshape(B, S, G, dh), pad)
    vw_p = jnp.pad(v_win.reshape(B, S, G, dh), pad)
    gates = jax.nn.sigmoid(branch_gate.astype(jnp.float32)).astype(q.dtype)
    gates = gates.reshape(B, S, N_BRANCH, G, Hg)
    cmp_end = jnp.arange(n_cmp) * CMP_STRIDE + CMP_BLOCK - 1
    blk = jnp.arange(n_blk)
    gather = jax.vmap(jax.vmap(lambda a, i: a[i]))

    def chunk(ci):
        q0 = ci * Q_CHUNK
        qc = lax.dynamic_slice_in_dim(q, q0, Q_CHUNK, axis=1)
        gc = lax.dynamic_slice_in_dim(gates, q0, Q_CHUNK, axis=1)
        t = q0 + jnp.arange(Q_CHUNK)
        s1 = jnp.einsum('bqghd,bngd->bghqn', qc, kc).astype(jnp.float32) * scale
        m1 = cmp_end[None, :] <= t[:, None]
        p1 = masked_softmax(s1, m1)
        o_cmp = jnp.einsum('bghqn,bngd->bqghd', p1.astype(vc.dtype), vc)
        ps = jnp.einsum('bghqn,nj->bgqj', p1, overlap)
        cur = t // SEL_BLOCK
        valid = blk[None, :] <= cur[:, None]
        forced = (blk[None, :] == 0) | (blk[None, :] == cur[:, None]) | (blk[None, :] == cur[:, None] - 1)
        score = jnp.where(forced, FORCE_SCORE, jnp.where(valid, ps, NEG))
        _, idx = lax.top_k(score, n_sel)
        flat_idx = idx.reshape(B, G, Q_CHUNK * n_sel)
        kg = gather(ks_b, flat_idx).reshape(B, G, Q_CHUNK, n_sel * SEL_BLOCK, dh)
        vg = gather(vs_b, flat_idx).reshape(B, G, Q_CHUNK, n_sel * SEL_BLOCK, dh)
        key_pos = (idx[..., None] * SEL_BLOCK + jnp.arange(SEL_BLOCK)).reshape(B, G, Q_CHUNK, n_sel * SEL_BLOCK)
        m2 = (key_pos <= t[None, None, :, None])[:, :, None]
        s2 = jnp.einsum('bqghd,bgqkd->bghqk', qc, kg).astype(jnp.float32) * scale
        p2 = masked_softmax(s2, m2)
        o_sel = jnp.einsum('bghqk,bgqkd->bqghd', p2.astype(vg.dtype), vg)
        kwc = lax.dynamic_slice_in_dim(kw_p, q0, WINDOW + Q_CHUNK, axis=1)
        vwc = lax.dynamic_slice_in_dim(vw_p, q0, WINDOW + Q_CHUNK, axis=1)
        kpos = q0 - WINDOW + jnp.arange(WINDOW + Q_CHUNK)
        m3 = (kpos[None, :] <= t[:, None]) & (kpos[None, :] > t[:, None] - WINDOW) & (kpos[None, :] >= 0)
        s3 = jnp.einsum('bqghd,bkgd->bghqk', qc, kwc).astype(jnp.float32) * scale
        p3 = masked_softmax(s3, m3)
        o_win = jnp.einsum('bghqk,bkgd->bqghd', p3.astype(vwc.dtype), vwc)
        o = (gc[:, :, 0, :, :, None] * o_cmp + gc[:, :, 1, :, :, None] * o_sel
             + gc[:, :, 2, :, :, None] * o_win)
        return o.reshape(B, Q_CHUNK, ATTN_WIDTH)

    out = lax.map(chunk, jnp.arange(S // Q_CHUNK))
    return out.transpose(1, 0, 2, 3).reshape(B, S, ATTN_WIDTH)


def token_mixer(h, w_in, pool_w, pool_scale, cmp_pos, cmp_w1, cmp_b1, cmp_w2,
                w_up_pool, w_up_attn, w_out):
    z = h @ w_in
    cuts = np.cumsum([POOL_WIDTH, ATTN_WIDTH] + [KV_WIDTH] * 6 + [N_BRANCH * N_HEADS])
    u_pool, q, kc, vc, ks, vs, kw, vw, bgate, mgate = jnp.split(z, [int(i) for i in cuts], axis=-1)
    y_pool = pool_mixer(u_pool, pool_w, pool_scale) @ w_up_pool
    y_attn = nsa_attention(q, kc, vc, ks, vs, kw, vw, bgate, cmp_pos, cmp_w1, cmp_b1, cmp_w2) @ w_up_attn
    g = jax.nn.sigmoid(mgate.astype(jnp.float32)).astype(h.dtype)
    return (g[..., :D_MODEL] * y_pool + g[..., D_MODEL:] * y_attn) @ w_out


def hier_moe(h, rg_w, rg_b, re_w, re_b, w_gate, w_up, w_down):
    B, S, D = h.shape
    xt = h.reshape(-1, D)
    n = xt.shape[0]
    lg = (xt @ rg_w + rg_b).astype(jnp.float32)
    gidx = jnp.argmax(lg, axis=-1)
    gp = jnp.max(jax.nn.softmax(lg, axis=-1), axis=-1)
    le = (xt @ re_w + re_b).astype(jnp.float32).reshape(n, N_GROUPS, EXPERTS_PER_GROUP)
    le_sel = le[jnp.arange(n), gidx]
    tv, ti = lax.top_k(le_sel, TOP_K_IN_GROUP)
    w = gp[:, None] * jax.nn.softmax(tv, axis=-1)
    eid = gidx[:, None] * EXPERTS_PER_GROUP + ti
    combine = jnp.einsum('nk,nke->ne', w, jax.nn.one_hot(eid, N_EXPERTS, dtype=jnp.float32)).astype(xt.dtype)
    y = jnp.zeros_like(xt)
    for e in range(N_EXPERTS):
        he = jax.nn.silu(xt @ w_gate[e]) * (xt @ w_up[e])
        y = y + combine[:, e:e + 1] * (he @ w_down[e])
    return y.reshape(B, S, D)


def setup_inputs(seed: int = 0) -> dict:
    key = jax.random.key(seed)
    ks = jax.random.split(key, 26)
    L, D, f32 = DEPTH, D_MODEL, jnp.float32
    nrm = lambda k, shape: jax.random.normal(k, shape, f32)
    lin = lambda k, shape, fan: nrm(k, shape) * fan ** -0.5
    return {
        'x': nrm(ks[0], (BATCH, SEQ, D)),
        'c': nrm(ks[1], (BATCH, D)),
        'ada_w': 0.5 * lin(ks[2], (L, D, 6 * D), D),
        'ada_b': 0.02 * nrm(ks[3], (L, 6 * D)),
        'norm1_g': 1.0 + 0.02 * nrm(ks[4], (L, D)),
        'w_in': lin(ks[5], (L, D, IN_WIDTH), D),
        'pool_w': lin(ks[6], (L, len(POOL_WINDOWS), POOL_GROUP, POOL_GROUP), POOL_GROUP),
        'pool_scale': 1.0 + 0.1 * nrm(ks[7], (L, POOL_WIDTH)),
        'cmp_pos': 0.02 * nrm(ks[8], (L, 2, CMP_BLOCK, HEAD_DIM)),
        'cmp_w1': lin(ks[9], (L, 2, CMP_BLOCK * HEAD_DIM, CMP_HIDDEN), CMP_BLOCK * HEAD_DIM),
        'cmp_b1': 0.02 * nrm(ks[10], (L, 2, CMP_HIDDEN)),
        'cmp_w2': lin(ks[11], (L, 2, CMP_HIDDEN, HEAD_DIM), CMP_HIDDEN),
        'w_up_pool': lin(ks[12], (L, POOL_WIDTH, D), POOL_WIDTH),
        'w_up_attn': lin(ks[13], (L, ATTN_WIDTH, D), ATTN_WIDTH),
        'w_out': lin(ks[14], (L, D, D), D),
        'norm2_g': 1.0 + 0.02 * nrm(ks[15], (L, D)),
        'router_g_w': lin(ks[16], (L, D, N_GROUPS), D),
        'router_g_b': 0.01 * nrm(ks[17], (L, N_GROUPS)),
        'router_e_w': lin(ks[18], (L, D, N_EXPERTS), D),
        'router_e_b': 0.01 * nrm(ks[19], (L, N_EXPERTS)),
        'exp_w_gate': lin(ks[20], (L, N_EXPERTS, D, EXPERT_FF), D),
        'exp_w_up': lin(ks[21], (L, N_EXPERTS, D, EXPERT_FF), D),
        'exp_w_down': lin(ks[22], (L, N_EXPERTS, EXPERT_FF, D), EXPERT_FF),
        'final_g': 1.0 + 0.02 * nrm(ks[23], (D,)),
    }


def reference(x, c, ada_w, ada_b, norm1_g, w_in, pool_w, pool_scale, cmp_pos, cmp_w1, cmp_b1,
              cmp_w2, w_up_pool, w_up_attn, w_out, norm2_g, router_g_w, router_g_b,
              router_e_w, router_e_b, exp_w_gate, exp_w_up, exp_w_down, final_g):
    for l in range(DEPTH):
        mod = c @ ada_w[l] + ada_b[l]
        sh1, sc1, g1, sh2, sc2, g2 = [m[:, None, :] for m in jnp.split(mod, 6, axis=-1)]
        h = rmsnorm(x, norm1_g[l]) * (1.0 + sc1) + sh1
        x = x + g1 * token_mixer(h, w_in[l], pool_w[l], pool_scale[l], cmp_pos[l], cmp_w1[l],
                                 cmp_b1[l], cmp_w2[l], w_up_pool[l], w_up_attn[l], w_out[l])
        h = rmsnorm(x, norm2_g[l]) * (1.0 + sc2) + sh2
        x = x + g2 * hier_moe(h, router_g_w[l], router_g_b[l], router_e_w[l], router_e_b[l],
                              exp_w_gate[l], exp_w_up[l], exp_w_down[l])
    return rmsnorm(x, final_g)
```

```python
import numpy as np
from contextlib import ExitStack
import concourse.bass as bass
import concourse.mybir as mybir
from concourse.bass_utils import run_bass_kernel_spmd

F32 = mybir.dt.float32
BF16 = mybir.dt.bfloat16
AF = mybir.ActivationFunctionType
ALU = mybir.AluOpType
AX = mybir.AxisListType

D = 1024
S = 2048
NCORES = 8
NR = 4
NEXP = 32
EPS = 1e-6
CUT = 100


class Prog:
    ENGS = ['pe', 'act', 'dve', 'pool', 'sp']

    def __init__(self, nc, es):
        self.nc = nc
        self.es = es
        self.plan = False
        self.ops = {e: [] for e in self.ENGS}
        self.sems = {}
        self.semcount = {}
        self.known = {e: {} for e in self.ENGS}
        self.lastw = {}
        self.readers = {}
        self.nbank = 0
        for e in ['pe', 'act', 'dve', 'pool']:
            self.sem('c_' + e)

    def sem(self, name):
        if name not in self.sems:
            self.sems[name] = self.es.enter_context(self.nc.semaphore(name))
            self.semcount[name] = 0
        return name

    @staticmethod
    def _key(r):
        if isinstance(r, (str, tuple)):
            return r
        t = getattr(r, 'tensor', r)
        return t.name

    def emit(self, eng, fn, reads=(), writes=(), sem=None):
        if self.plan:
            return
        is_dma = sem is not None
        if not is_dma:
            sem = 'c_' + eng
            inc = 1
        else:
            self.sem(sem)
            inc = 16
        waits = {}

        def need(dep, raw):
            s, v, e, d = dep
            if (not d) and e == eng:
                if eng == 'pe' or not raw:
                    return
            if d and is_dma and (not raw) and s == sem:
                return
            if self.known[eng].get(s, 0) >= v:
                return
            waits[s] = max(waits.get(s, 0), v)

        for r in reads:
            k = self._key(r)
            if k in self.lastw:
                need(self.lastw[k], True)
        for w in writes:
            k = self._key(w)
            if k in self.lastw:
                need(self.lastw[k], False)
            for dep in self.readers.get(k, {}).values():
                need(dep, False)
        for s, v in waits.items():
            self.known[eng][s] = v
        self.semcount[sem] += inc
        tick = self.semcount[sem]
        me = (sem, tick, eng, is_dma)
        self.ops[eng].append((list(waits.items()), fn, sem, inc))
        for r in reads:
            k = self._key(r)
            self.readers.setdefault(k, {})[(eng, sem)] = me
        for w in writes:
            k = self._key(w)
            self.lastw[k] = me
            self.readers[k] = {}

    def barrier(self):
        if self.plan:
            return
        for eng in self.ENGS:
            waits = []
            for s, c in self.semcount.items():
                if c > 0 and self.known[eng].get(s, 0) < c:
                    waits.append((s, c))
                    self.known[eng][s] = c
            if waits:
                self.ops[eng].append((waits, None, None, 0))

    def run(self):
        nc = self.nc
        P = self

        def replay(e, eng):
            for (waits, fn, sem, inc) in P.ops[e]:
                for (s, v) in waits:
                    eng.wait_ge(P.sems[s], v)
                if fn is not None:
                    ins = fn(eng)
                    ins.then_inc(P.sems[sem], inc)

        with nc.Block() as block:
            @block.tensor
            def _(eng):
                replay('pe', eng)

            @block.scalar
            def _(eng):
                replay('act', eng)

            @block.vector
            def _(eng):
                replay('dve', eng)

            @block.gpsimd
            def _(eng):
                replay('pool', eng)

            @block.sync
            def _(eng):
                replay('sp', eng)


class Ring:
    def __init__(self, P, slots):
        self.P = P
        self.slots = slots
        self.seq = []
        self.reset()

    def reset(self):
        self.idx = 0
        self.loaded = 0
        self.dead = set()

    def get(self, loader):
        i = self.idx
        self.idx += 1
        if self.P.plan:
            self.seq.append(loader)
            return i, self.slots[i % NR]
        self.top_up()
        assert self.loaded > i, ("ring too many live chunks", i)
        return i, self.slots[i % NR]

    def done(self, i):
        if self.P.plan:
            return
        self.dead.add(i)
        self.top_up()

    def top_up(self):
        while self.loaded < len(self.seq) and (self.loaded < NR or (self.loaded - NR) in self.dead):
            c = self.loaded
            self.seq[c](self.slots[c % NR], c % NR)
            self.loaded += 1


def build(nseq, dbg=False):
    nc = bass.Bass("TRN2", target_bir_lowering=False)
    ntok = nseq * S

    def din(name, shape, dt=F32):
        return nc.dram_tensor(name, list(shape), dt, kind="ExternalInput").ap()

    x_d = din("x", [ntok, D])
    crep_d = din("crep", [nseq, 128, 8, 128])
    cT_d = din("cT", [128, 8, nseq])
    adaw_d = din("ada_w", [D, 6 * D])
    adabT_d = din("adabT", [128, 48])
    adabbc_d = din("adab_bc", [128, 2, D])
    n1g_d = din("n1g", [128, 8])
    n2g_d = din("n2g", [128, 8])
    fgbc_d = din("fg_bc", [128, D])
    win_d = din("w_in_p", [D, 3864])
    poolw_d = din("pool_w_r", [128, 4, 128])
    psc_d = din("pscT", [128, 4])
    posT_d = din("posT", [128, 2, 32])
    w1_d = din("cmp_w1", [2, 2048, 256])
    b1T_d = din("b1T", [128, 2, 2])
    w2r_d = din("w2r", [128, 2, 2, 64])
    wup_d = din("w_up_pool", [512, D])
    wua_d = din("w_up_attn", [512, D])
    wout_d = din("w_out", [D, D])
    rw_d = din("rw", [128, 8, 36])
    rbbc_d = din("rb_bc", [128, 36])
    eg_d = din("exp_w_gate", [NEXP, D, 512])
    eu_d = din("exp_w_up", [NEXP, D, 512])
    ed_d = din("exp_w_down", [NEXP, 512, D])
    identf_d = din("identf", [128, 128])
    maskD_d = din("maskD4", [128, 512])
    maskW_d = din("maskW4", [128, 512])
    maskC_d = din("maskC", [128, 16, 128])
    eall_d = din("Eall", [32, 2048])
    ptcur_d = din("ptcur", [128, 4, 128])
    ptfirst_d = din("ptfirst", [128, 4, 128])
    ptprev_d = din("ptprev", [128, 4, 128])
    ovl_d = din("ovl1", [128, 33])
    selv_d = din("selvalid", [128, 16, 32])
    selb_d = din("selbias", [128, 16, 32])
    y_d = nc.dram_tensor("y", [ntok, D], F32, kind="ExternalOutput").ap()
    if dbg:
        dbg_d = nc.dram_tensor("dbg_x1", [ntok, D], F32, kind="ExternalOutput").ap()

    with ExitStack() as es:
        P = Prog(nc, es)

        uniq = [0]

        def sb(name, shape, dt, stack=es):
            uniq[0] += 1
            return stack.enter_context(nc.sbuf_tensor(f"{name}_u{uniq[0]}", list(shape), dt))

        identb = sb("identb", [128, 128], BF16)
        identf = sb("identf_s", [128, 128], F32)
        maskD4 = sb("maskD4_s", [128, 512], BF16)
        maskW4 = sb("maskW4_s", [128, 512], BF16)
        maskC = sb("maskC_s", [128, 16, 128], BF16)
        Eall = sb("Eall_s", [128, 2048], BF16)
        ptcur = sb("ptcur_s", [128, 4, 128], BF16)
        ptfirst = sb("ptfirst_s", [128, 4, 128], BF16)
        ptprev = sb("ptprev_s", [128, 4, 128], BF16)
        selvalid = sb("selvalid_s", [128, 16, 32], F32)
        selbiasc = sb("selbias_s", [128, 16, 32], F32)
        modT = sb("modT", [128, 32, nseq], F32)
        s1T = sb("s1T", [128, 8, nseq], F32)
        s2T = sb("s2T", [128, 8, nseq], F32)
        adabT = sb("adabT_s", [128, 48], F32)
        n1g = sb("n1g_s", [128, 8], F32)
        n2g = sb("n2g_s", [128, 8], F32)
        fg_bc = sb("fg_bc_s", [128, D], F32)
        g1bc = sb("g1bc", [128, D], F32)
        g2bc = sb("g2bc", [128, D], F32)
        poolw = sb("poolw_s", [128, 4, 128], BF16)
        pscT = sb("pscT_s", [128, 4], F32)
        posT = sb("posT_s", [128, 2, 32], BF16)
        b1T = sb("b1T_s", [128, 2, 2], F32)
        biasc = sb("biasc", [128, 2, 2], F32)
        w2sb = sb("w2sb", [128, 2, 2, 64], BF16)
        rw = sb("rw_s", [128, 8, 36], F32)
        rb_bc = sb("rb_bc_s", [128, 36], F32)
        crep = sb("crep_s", [128, 8, 128], BF16)
        cT = sb("cT_s", [128, 8, nseq], BF16)
        acc = sb("acc", [128, 8, D], F32)
        comb = sb("comb", [128, 8, 32], F32)
        kcT = sb("kcT", [128, S], BF16)
        vcT = sb("vcT", [128, S], BF16)
        ksT = sb("ksT", [128, S], BF16)
        kwT = sb("kwT", [128, S], BF16)
        kcmpT = sb("kcmpT", [128, 128], BF16)
        vcaug = sb("vcaug", [128, 2, 97], BF16)
        hidv = sb("hidv", [128, 2, 2, 128], BF16)
        vs_aug = sb("vs_aug", [128, 16, 2, 65], BF16)
        vw_aug = sb("vw_aug", [128, 16, 2, 65], BF16)
        bg = sb("bg", [128, 16, 24], F32)
        ucarry = sb("ucarry", [128, 512], BF16)
        ring = [sb(f"ring{i}", [128, 4096], BF16) for i in range(NR)]
        R = Ring(P, ring)

        banks = [es.enter_context(nc.psum_tensor(f"bank{i}", [128, 512], F32)) for i in range(6)]
        ACCA = es.enter_context(nc.psum_tensor("ACCA", [128, 512], F32))
        ACCB = es.enter_context(nc.psum_tensor("ACCB", [128, 512], F32))

        def PS():
            b = banks[P.nbank % 6]
            P.nbank += 1
            return b

        def MM(out, lhsT, rhs, start=True, stop=True, rd=(), wr=()):
            P.emit('pe', lambda e: e.matmul(out, lhsT, rhs, start=start, stop=stop, skip_group_check=True), rd, wr)

        def TR(out, in_, ident, rd=(), wr=()):
            P.emit('pe', lambda e: e.transpose(out, in_, ident), rd, wr)

        def ACT(out, in_, func, rd=(), wr=(), **kw):
            P.emit('act', lambda e: e.activation(out=out, in_=in_, func=func, **kw), rd, wr)

        def ACOPY(out, in_, rd=(), wr=()):
            P.emit('act', lambda e: e.copy(out=out, in_=in_), rd, wr)

        def TS(out, in0, s1, s2, op0, op1=None, rd=(), wr=(), eng='dve'):
            if op1 is None:
                P.emit(eng, lambda e: e.tensor_scalar(out=out, in0=in0, scalar1=s1, scalar2=None, op0=op0), rd, wr)
            else:
                P.emit(eng, lambda e: e.tensor_scalar(out=out, in0=in0, scalar1=s1, scalar2=s2, op0=op0, op1=op1), rd, wr)

        def TT(out, in0, in1, op, rd=(), wr=(), eng='dve'):
            P.emit(eng, lambda e: e.tensor_tensor(out=out, in0=in0, in1=in1, op=op), rd, wr)

        def STT(out, in0, scalar, in1, op0, op1, rd=(), wr=()):
            P.emit('dve', lambda e: e.scalar_tensor_tensor(out=out, in0=in0, scalar=scalar, in1=in1, op0=op0, op1=op1), rd, wr)

        def CP(out, in_, rd=(), wr=(), eng='dve'):
            P.emit(eng, lambda e: e.tensor_copy(out=out, in_=in_), rd, wr)

        def DMA(eng, out, in_, sem, rd=(), wr=()):
            P.emit(eng, lambda e: e.dma_start(out=out, in_=in_), rd, wr, sem=sem)

        def wchunk(src2d, ncols, nk=8):
            def loader(slot, si):
                dst = slot[:, 0:nk * ncols].rearrange("p (k n) -> p k n", k=nk)
                DMA('pool', dst, src2d.rearrange("(k p) n -> p k n", p=128), f'ring{si}', (), [slot])
            return loader

        def w1chunk(kv, half):
            def loader(slot, si):
                src = w1_d[kv].rearrange("(l d) c -> d l c", d=64)[:, 16 * half:16 * half + 16, :]
                for hp in range(2):
                    dst = slot[64 * hp:64 * hp + 64, :].rearrange("p (l c) -> p l c", l=16)
                    DMA('pool', dst, src, f'ring{si}', (), [slot])
            return loader

        def V3(ap2d, a):
            return ap2d.rearrange("p (a b) -> p a b", a=a)

        def gen():
            P.nbank = 0
            R.reset()
            for (dst, src) in [(identf, identf_d), (selvalid, selv_d), (selbiasc, selb_d), (adabT, adabT_d),
                               (n1g, n1g_d), (n2g, n2g_d), (fg_bc, fgbc_d), (pscT, psc_d), (b1T, b1T_d),
                               (rw, rw_d), (rb_bc, rbbc_d)]:
                DMA('sp', dst[:], src, 'cst_sp', (), [dst])
            DMA('pool', Eall[0:32, :], eall_d, 'cst_pool', (), [Eall])
            DMA('pool', Eall[64:96, :], eall_d, 'cst_pool', (), [Eall])
            for (dst, src) in [(maskD4, maskD_d), (maskW4, maskW_d), (maskC, maskC_d),
                               (ptcur, ptcur_d), (ptfirst, ptfirst_d), (ptprev, ptprev_d),
                               (poolw, poolw_d), (posT, posT_d), (w2sb, w2r_d), (cT, cT_d)]:
                DMA('pool', dst[:], src, 'cst_pool', (), [dst])
            DMA('pool', identb[:], identf_d, 'cst_pool', (), [identb])
            for g in range(2):
                DMA('pool', vcaug[:, g, 64:97], ovl_d, 'cst_pool', (), [vcaug])
            P.emit('dve', lambda e: e.memset(hidv[:], 0.0), (), [hidv])
            P.emit('dve', lambda e: e.memset(kcmpT[:], 0.0), (), [kcmpT])
            P.emit('dve', lambda e: e.memset(vs_aug[:], 1.0), (), [vs_aug])
            P.emit('dve', lambda e: e.memset(vw_aug[:], 1.0), (), [vw_aug])
            P.emit('dve', lambda e: e.memset(ucarry[:], 0.0), (), [ucarry])
            for g in range(2):
                P.emit('dve', lambda e, g=g: e.memset(vcaug[:, g, 0:64], 0.0), (), [vcaug])
            P.barrier()

            for mi, c0 in enumerate([0, 1024, 3072, 4096]):
                for hf in range(2):
                    ci, w = R.get(wchunk(adaw_d[:, c0 + 512 * hf:c0 + 512 * hf + 512], 512))
                    wv = V3(w[:, :], 8)
                    bk = PS()
                    for fc in range(4):
                        for k in range(8):
                            MM(bk[:, fc * nseq:(fc + 1) * nseq], wv[:, k, fc * 128:(fc + 1) * 128], cT[:, k, :],
                               start=(k == 0), stop=(k == 7), rd=[w, cT], wr=[bk])
                    R.done(ci)
                    j0 = mi * 8 + hf * 4
                    cb = (c0 // 128) + hf * 4
                    TT(modT[:, j0:j0 + 4, :], V3(bk[:, 0:4 * nseq], 4),
                       adabT[:, cb:cb + 4].unsqueeze(2).broadcast_to([128, 4, nseq]), ALU.add, rd=[bk, adabT], wr=[modT])
            TS(s1T[:], modT[:, 8:16, :], 1.0, None, ALU.add, rd=[modT], wr=[s1T])
            TT(s1T[:], s1T[:], n1g[:].unsqueeze(2).broadcast_to([128, 8, nseq]), ALU.mult, rd=[s1T, n1g], wr=[s1T])
            TS(s2T[:], modT[:, 24:32, :], 1.0, None, ALU.add, rd=[modT], wr=[s2T])
            TT(s2T[:], s2T[:], n2g[:].unsqueeze(2).broadcast_to([128, 8, nseq]), ALU.mult, rd=[s2T, n2g], wr=[s2T])

            for kv in range(2):
                ca, wa = R.get(w1chunk(kv, 0))
                cb_, wb = R.get(w1chunk(kv, 1))
                for cc in range(2):
                    bk = PS()
                    for l in range(32):
                        w = wa if l < 16 else wb
                        wv = V3(w[:, :], 16)
                        MM(bk[:, 0:1], wv[0:64, l % 16, cc * 128:(cc + 1) * 128], posT[0:64, kv, l:l + 1],
                           start=(l == 0), stop=(l == 31), rd=[w, posT], wr=[bk])
                    TT(biasc[:, kv, cc:cc + 1], bk[:, 0:1], b1T[:, kv, cc:cc + 1], ALU.add, rd=[bk, b1T], wr=[biasc])
                R.done(ca)
                R.done(cb_)

            for b in range(nseq):
                seq_prologue(b)
                for grp in range(2):
                    with ExitStack() as ph:
                        T = alloc_mixer(ph, f"{b}_{grp}")
                        for sth in range(2):
                            mixer_supertile(T, b, grp * 2 + sth)
                    P.barrier()
                    if dbg:
                        for j in range(8):
                            r0 = b * S + grp * 1024 + j * 128
                            DMA('sp', dbg_d[r0:r0 + 128, :], acc[:, j, :], f'dbgst', [('acc', j)], ['dbgout'])
                        P.barrier()
                    with ExitStack() as ph:
                        T = alloc_moe(ph, f"{b}_{grp}")
                        moe_group(T, b, grp)
                    P.barrier()
            P.barrier()

        def seq_prologue(b):
            DMA('pool', crep[:], crep_d[b], 'crep', (), [crep])
            DMA('sp', g1bc[:], adabbc_d[:, 0, :], 'g1l', (), [g1bc])
            DMA('sp', g2bc[:], adabbc_d[:, 1, :], 'g2l', (), [g2bc])
            for (dst, c0) in [(g1bc, 2048), (g2bc, 5120)]:
                for hf in range(2):
                    ci, w = R.get(wchunk(adaw_d[:, c0 + 512 * hf:c0 + 512 * hf + 512], 512))
                    wv = V3(w[:, :], 8)
                    bk = PS()
                    for k in range(8):
                        MM(bk[:, :], crep[:, k, :], wv[:, k, :], start=(k == 0), stop=(k == 7), rd=[w, crep], wr=[bk])
                    R.done(ci)
                    TT(dst[:, hf * 512:(hf + 1) * 512], bk[:, :], dst[:, hf * 512:(hf + 1) * 512], ALU.add,
                       rd=[bk, dst], wr=[dst])

        def alloc_mixer(ph, tag):
            T = {}

            def a(name, shape, dt):
                T[name] = sb(f"{name}_{tag}", shape, dt, ph)
            a('sqj', [128, D], BF16)
            a('xn0', [128, D], BF16)
            a('xn1', [128, D], BF16)
            a('ss', [128, 4], F32)
            a('lnv', [128, 4], F32)
            a('rstd', [128, 4], F32)
            a('h1T', [128, 8, 512], BF16)
            a('qT', [128, 4, 512], BF16)
            a('u_tm', [128, 4, 512], BF16)
            a('pooledT', [128, 4, 512], BF16)
            a('ypoolT', [128, 4, 512], BF16)
            a('hidk', [128, 2, 32], BF16)
            for i in range(4):
                a(f'pe{i}', [128, 512], BF16)
            a('pc', [128, 512], BF16)
            a('pcm', [128, 512], BF16)
            a('ocr', [128, 4, 97], F32)
            a('osel', [128, 4, 65], F32)
            a('owin', [128, 4, 65], F32)
            a('rs4', [128, 4], F32)
            a('psc', [128, 32], F32)
            a('score', [128, 32], F32)
            a('m8', [128, 8], F32)
            a('selb', [128, 32], BF16)
            a('selbT', [128, 4, 128], BF16)
            a('den', [128, 3, 4], F32)
            a('coef', [128, 3, 4], F32)
            a('o1', [128, 4, 64], F32)
            a('o2', [128, 4, 64], F32)
            a('oattn', [128, 512], BF16)
            a('oattnT', [128, 4, 512], BF16)
            a('sig', [128, 512], F32)
            a('t2', [128, 512], F32)
            a('mixT', [128, 8, 512], BF16)
            a('tmp', [128, 512], F32)
            return T

        def mixer_supertile(T, b, st):
            h1T, qT, u_tm = T['h1T'], T['qT'], T['u_tm']
            tok0 = st * 512
            jb = (st % 2) * 4
            for i in range(4):
                r0 = b * S + tok0 + i * 128
                DMA('sp', acc[:, jb + i, :], x_d[r0:r0 + 128, :], f'xl{jb + i}', (), [('acc', jb + i)])
            ss, lnv, rstd = T['ss'], T['lnv'], T['rstd']
            for i in range(4):
                ACT(T['sqj'][:], acc[:, jb + i, :], AF.Square, rd=[('acc', jb + i)], wr=[T['sqj'], (ss.name, i)],
                    accum_out=ss[:, i:i + 1])
            ACT(lnv[:], ss[:], AF.Ln, rd=[(ss.name, i) for i in range(4)], wr=[lnv], scale=1.0 / D, bias=EPS)
            ACT(rstd[:], lnv[:], AF.Exp, rd=[lnv], wr=[rstd], scale=-0.5)
            tb = [PS() for _ in range(4)]
            tbv = [t[:, :].bitcast(BF16) for t in tb]
            for i in range(4):
                xn = T[f'xn{i % 2}']
                TS(xn[:], acc[:, jb + i, :], rstd[:, i:i + 1], None, ALU.mult, rd=[('acc', jb + i), rstd], wr=[xn])
                for c in range(8):
                    o = tbv[c // 2][:, (c % 2) * 512 + i * 128:(c % 2) * 512 + (i + 1) * 128]
                    TR(o, xn[:, c * 128:(c + 1) * 128], identb[:], rd=[xn], wr=[tb[c // 2]])
            for c in range(8):
                TS(h1T[:, c, :], tbv[c // 2][:, (c % 2) * 512:(c % 2 + 1) * 512], s1T[:, c, b:b + 1], modT[:, c, b:b + 1],
                   ALU.mult, ALU.add, rd=[tb[c // 2], s1T, modT], wr=[h1T])
            if CUT < 10:
                return
            ci, w = R.get(wchunk(win_d[:, 0:512], 512))
            wv = V3(w[:, :], 8)
            for i in range(4):
                bk = PS()
                for k in range(8):
                    MM(bk[:, :], h1T[:, k, i * 128:(i + 1) * 128], wv[:, k, :], start=(k == 0), stop=(k == 7), rd=[h1T, w], wr=[bk])
                ACOPY(u_tm[:, i, :], bk[:, :], rd=[bk], wr=[u_tm])
            R.done(ci)
            ci, w = R.get(wchunk(win_d[:, 512:792], 280))
            wv = w[:, 0:8 * 280].rearrange("p (k n) -> p k n", k=8)
            for i in range(4):
                tt = st * 4 + i
                bk = PS()
                for k in range(8):
                    MM(bk[:, 0:280], h1T[:, k, i * 128:(i + 1) * 128], wv[:, k, :], start=(k == 0), stop=(k == 7), rd=[h1T, w], wr=[bk])
                ACOPY(vs_aug[:, tt, :, 0:64], V3(bk[:, 0:128], 2), rd=[bk], wr=[vs_aug])
                ACOPY(vw_aug[:, tt, :, 0:64], V3(bk[:, 128:256], 2), rd=[bk], wr=[vw_aug])
                ACT(bg[:, tt, :], bk[:, 256:280], AF.Sigmoid, rd=[bk], wr=[bg])
            R.done(ci)
            ci, w = R.get(wchunk(win_d[:, 792:1304], 512))
            wv = V3(w[:, :], 8)
            for hh in range(4):
                bk = PS()
                for k in range(8):
                    MM(bk[:, :], wv[:, k, hh * 128:(hh + 1) * 128], h1T[:, k, :], start=(k == 0), stop=(k == 7), rd=[h1T, w], wr=[bk])
                if hh % 2 == 0:
                    ACOPY(qT[:, hh, :], bk[:, :], rd=[bk], wr=[qT])
                else:
                    CP(qT[:, hh, :], bk[:, :], rd=[bk], wr=[qT])
            R.done(ci)
            ci, w = R.get(wchunk(win_d[:, 1304:1816], 512))
            wv = V3(w[:, :], 8)
            for n, dst in enumerate([kcT, vcT, ksT, kwT]):
                bk = PS()
                for k in range(8):
                    MM(bk[:, :], wv[:, k, n * 128:(n + 1) * 128], h1T[:, k, :], start=(k == 0), stop=(k == 7), rd=[h1T, w], wr=[bk])
                if n % 2 == 0:
                    ACOPY(dst[:, tok0:tok0 + 512], bk[:, :], rd=[bk], wr=[dst])
                else:
                    CP(dst[:, tok0:tok0 + 512], bk[:, :], rd=[bk], wr=[dst])
            R.done(ci)
            if CUT < 20:
                return
            pooledT, ypoolT = T['pooledT'], T['ypoolT']
            for g in range(4):
                bk = PS()
                gs = slice(g * 128, (g + 1) * 128)
                for i in range(4):
                    tt = st * 4 + i
                    o = bk[:, i * 128:(i + 1) * 128]
                    if tt == 0:
                        MM(o, u_tm[:, 0, gs], ptfirst[:, g, :], rd=[u_tm, ptfirst], wr=[bk])
                    else:
                        MM(o, u_tm[:, i, gs], ptcur[:, g, :], start=True, stop=False, rd=[u_tm, ptcur], wr=[bk])
                        if i > 0:
                            MM(o, u_tm[64:128, i - 1, gs], ptprev[64:128, g, :], start=False, stop=True, rd=[u_tm, ptprev], wr=[bk])
                        else:
                            MM(o, ucarry[64:128, gs], ptprev[64:128, g, :], start=False, stop=True, rd=[ucarry, ptprev], wr=[bk])
                ACOPY(pooledT[:, g, :], bk[:, :], rd=[bk], wr=[pooledT])
                bk2 = PS()
                MM(bk2[:, :], poolw[:, g, :], pooledT[:, g, :], rd=[poolw, pooledT], wr=[bk2])
                TS(ypoolT[:, g, :], bk2[:, :], pscT[:, g:g + 1], None, ALU.mult, rd=[bk2, pscT], wr=[ypoolT])
            CP(ucarry[:], u_tm[:, 3, :], rd=[u_tm], wr=[ucarry])
            if CUT < 30:
                return
            i0 = max(0, 32 * st - 1)
            i1 = 32 * (st + 1) - 1
            nb = i1 - i0
            hidk = T['hidk']
            for kv in range(2):
                src = kcT if kv == 0 else vcT
                ca, wa = R.get(w1chunk(kv, 0))
                cb_, wb = R.get(w1chunk(kv, 1))
                for g in range(2):
                    for cc in range(2):
                        bk = PS()
                        for l in range(32):
                            w = wa if l < 16 else wb
                            wv = V3(w[:, :], 16)
                            t0 = 16 * i0 + l
                            MM(bk[:, 0:nb], wv[64 * g:64 * g + 64, l % 16, cc * 128:(cc + 1) * 128],
                               src[64 * g:64 * g + 64, t0:t0 + 16 * (nb - 1) + 1:16],
                               start=(l == 0), stop=(l == 31), rd=[w, src], wr=[bk])
                        if kv == 0:
                            ACT(hidk[:, cc, 0:nb], bk[:, 0:nb], AF.Gelu_apprx_tanh, rd=[bk, biasc], wr=[hidk],
                                bias=biasc[:, kv, cc:cc + 1])
                        else:
                            ACT(hidv[:, g, cc, i0:i1], bk[:, 0:nb], AF.Gelu_apprx_tanh, rd=[bk, biasc], wr=[hidv],
                                bias=biasc[:, kv, cc:cc + 1])
                    bk = PS()
                    if kv == 0:
                        for cc in range(2):
                            MM(bk[64 * g:64 * g + 64, 0:nb], w2sb[:, 0, cc, :], hidk[:, cc, 0:nb],
                               start=(cc == 0), stop=(cc == 1), rd=[w2sb, hidk], wr=[bk])
                        CP(kcmpT[64 * g:64 * g + 64, i0:i1], bk[64 * g:64 * g + 64, 0:nb], rd=[bk], wr=[kcmpT])
                    else:
                        for cc in range(2):
                            MM(bk[:, 0:64], hidv[:, g, cc, :], w2sb[:, 1, cc, :],
                               start=(cc == 0), stop=(cc == 1), rd=[w2sb, hidv], wr=[bk])
                        CP(vcaug[:, g, 0:64], bk[:, 0:64], rd=[bk], wr=[vcaug])
                R.done(ca)
                R.done(cb_)
            if CUT < 40:
                return
            ocr, osel, owin = T['ocr'], T['osel'], T['owin']
            oattn, oattnT = T['oattn'], T['oattnT']
            npe = [0]

            def next_pe():
                t = T[f'pe{npe[0] % 4}']
                npe[0] += 1
                return t

            for i in range(4):
                qi = st * 4 + i
                for g in range(2):
                    gp = slice(64 * g, 64 * g + 64)
                    rq = qT[gp, :, i * 128:(i + 1) * 128]
                    bk = PS()
                    MM(V3(bk[:, :], 4), kcmpT[gp, :], rq, rd=[kcmpT, qT], wr=[bk])
                    pc, pcm = T['pc'], T['pcm']
                    ACT(pc[:], bk[:, :], AF.Exp, rd=[bk], wr=[pc], scale=0.125)
                    TT(V3(pcm[:, :], 4), V3(pc[:, :], 4), maskC[:, qi, :].unsqueeze(1).broadcast_to([128, 4, 128]), ALU.mult,
                       rd=[pc, maskC], wr=[pcm])
                    bk2 = PS()
                    for hh in range(4):
                        MM(bk2[:, hh * 97:(hh + 1) * 97], pcm[:, hh * 128:(hh + 1) * 128], vcaug[:, g, :], rd=[pcm, vcaug], wr=[bk2])
                    ACOPY(ocr[:].rearrange("p a b -> p (a b)"), bk2[:, 0:388], rd=[bk2], wr=[ocr])
                    rs4, psc, score, m8, selb, selbT = T['rs4'], T['psc'], T['score'], T['m8'], T['selb'], T['selbT']
                    TS(rs4[:], ocr[:, :, 64], 1e-30, None, ALU.max, rd=[ocr], wr=[rs4])
                    P.emit('dve', lambda e: e.reciprocal(out=rs4[:], in_=rs4[:]), [rs4], [rs4])
                    TS(psc[:], ocr[:, 0, 65:97], rs4[:, 0:1], None, ALU.mult, rd=[ocr, rs4], wr=[psc])
                    for hh in range(1, 4):
                        STT(psc[:], ocr[:, hh, 65:97], rs4[:, hh:hh + 1], psc[:], ALU.mult, ALU.add, rd=[ocr, rs4, psc], wr=[psc])
                    TT(score[:], psc[:], selvalid[:, qi, :], ALU.mult, rd=[psc, selvalid], wr=[score])
                    TT(score[:], score[:], selbiasc[:, qi, :], ALU.add, rd=[score, selbiasc], wr=[score])
                    P.emit('dve', lambda e: e.max(out=m8[:], in_=score[:]), [score], [m8])
                    TS(score[:], score[:], m8[:, 7:8], None, ALU.is_ge, rd=[score, m8], wr=[score])
                    TS(selb[:], score[:], 1.0, 30000.0, ALU.subtract, ALU.mult, rd=[score], wr=[selb])
                    bkt = PS()
                    bktv = bkt[:, :].bitcast(BF16)
                    sp_ = slice(64 * g, 64 * g + 32)
                    TR(bktv[sp_, 0:128], selb[:, :], identb[:], rd=[selb], wr=[bkt])
                    CP(selbT[sp_], bktv[sp_, 0:128].unsqueeze(1).broadcast_to([32, 4, 128]), rd=[bkt], wr=[selbT])
                    for kj in range(qi + 1):
                        bk = PS()
                        MM(V3(bk[:, :], 4), ksT[gp, kj * 128:(kj + 1) * 128], rq, start=True, stop=False, rd=[ksT, qT], wr=[bk])
                        MM(V3(bk[:, :], 4), Eall[sp_, kj * 128:(kj + 1) * 128], selbT[sp_], start=False, stop=True, rd=[selbT], wr=[bk])
                        pt = next_pe()
                        ACT(pt[:], bk[:, :], AF.Exp, rd=[bk], wr=[pt], scale=0.125)
                        if kj == qi:
                            TT(pt[:], pt[:], maskD4[:], ALU.mult, rd=[pt], wr=[pt])
                        for hh in range(4):
                            MM(ACCA[:, hh * 65:(hh + 1) * 65], pt[:, hh * 128:(hh + 1) * 128], vs_aug[:, kj, g, :],
                               start=(kj == 0 and hh == 0), stop=(kj == qi), rd=[pt, vs_aug], wr=[ACCA])
                    ACOPY(osel[:].rearrange("p a b -> p (a b)"), ACCA[:, 0:260], rd=[ACCA], wr=[osel])
                    k0 = max(0, qi - 4)
                    for kj in range(k0, qi + 1):
                        bk = PS()
                        MM(V3(bk[:, :], 4), kwT[gp, kj * 128:(kj + 1) * 128], rq, rd=[kwT, qT], wr=[bk])
                        pt = next_pe()
                        ACT(pt[:], bk[:, :], AF.Exp, rd=[bk], wr=[pt], scale=0.125)
                        if kj == qi:
                            TT(pt[:], pt[:], maskD4[:], ALU.mult, rd=[pt], wr=[pt])
                        elif kj == qi - 4:
                            TT(pt[:], pt[:], maskW4[:], ALU.mult, rd=[pt], wr=[pt])
                        for hh in range(4):
                            MM(ACCB[:, hh * 65:(hh + 1) * 65], pt[:, hh * 128:(hh + 1) * 128], vw_aug[:, kj, g, :],
                               start=(kj == k0 and hh == 0), stop=(kj == qi), rd=[pt, vw_aug], wr=[ACCB])
                    ACOPY(owin[:].rearrange("p a b -> p (a b)"), ACCB[:, 0:260], rd=[ACCB], wr=[owin])
                    den, coef, o1, o2 = T['den'], T['coef'], T['o1'], T['o2']
                    CP(den[:, 0, :], ocr[:, :, 64], rd=[ocr], wr=[den])
                    CP(den[:, 1, :], osel[:, :, 64], rd=[osel], wr=[den])
                    CP(den[:, 2, :], owin[:, :, 64], rd=[owin], wr=[den])
                    TS(den[:], den[:], 1e-30, None, ALU.max, rd=[den], wr=[den])
                    P.emit('dve', lambda e: e.reciprocal(out=den[:], in_=den[:]), [den], [den])
                    gview = bg[:, qi, :].rearrange("p (r h) -> p r h", r=3)[:, :, 4 * g:4 * g + 4]
                    TT(coef[:], den[:], gview, ALU.mult, rd=[den, bg], wr=[coef])

                    def bc(r):
                        return coef[:, r, :].unsqueeze(2).broadcast_to([128, 4, 64])
                    TT(o1[:], ocr[:, :, 0:64], bc(0), ALU.mult, rd=[ocr, coef], wr=[o1])
                    TT(o2[:], osel[:, :, 0:64], bc(1), ALU.mult, rd=[osel, coef], wr=[o2])
                    TT(o1[:], o1[:], o2[:], ALU.add, rd=[o1, o2], wr=[o1])
                    TT(o2[:], owin[:, :, 0:64], bc(2), ALU.mult, rd=[owin, coef], wr=[o2])
                    TT(V3(oattn[:, g * 256:(g + 1) * 256], 4), o1[:], o2[:], ALU.add, rd=[o1, o2], wr=[oattn])
                bkt = PS()
                bktv = bkt[:, :].bitcast(BF16)
                for c in range(4):
                    TR(bktv[:, c * 128:(c + 1) * 128], oattn[:, c * 128:(c + 1) * 128], identb[:], rd=[oattn], wr=[bkt])
                CP(oattnT[:, :, i * 128:(i + 1) * 128], V3(bktv[:, 0:512], 4), rd=[bkt], wr=[oattnT])
            if CUT < 50:
                return
            sig, t2, mixT, tmp = T['sig'], T['t2'], T['mixT'], T['tmp']
            for side in range(2):
                cu, wu_ = R.get(wchunk((wup_d if side == 0 else wua_d)[:, :], 1024, nk=4))
                wuv = V3(wu_[:, :], 4)
                srcT = ypoolT if side == 0 else oattnT
                for mh in range(2):
                    c0 = 1816 + side * 1024 + mh * 512
                    cm, wm = R.get(wchunk(win_d[:, c0:c0 + 512], 512))
                    wmv = V3(wm[:, :], 8)
                    for dq in range(4):
                        dc = mh * 4 + dq
                        bka = PS()
                        for kc in range(4):
                            MM(bka[:, :], wuv[:, kc, dc * 128:(dc + 1) * 128], srcT[:, kc, :], start=(kc == 0), stop=(kc == 3),
                               rd=[wu_, srcT], wr=[bka])
                        bkg = PS()
                        for k in range(8):
                            MM(bkg[:, :], wmv[:, k, dq * 128:(dq + 1) * 128], h1T[:, k, :], start=(k == 0), stop=(k == 7),
                               rd=[wm, h1T], wr=[bkg])
                        ACT(sig[:], bkg[:, :], AF.Sigmoid, rd=[bkg], wr=[sig])
                        if side == 0:
                            TT(mixT[:, dc, :], sig[:], bka[:, :], ALU.mult, rd=[sig, bka], wr=[mixT])
                        else:
                            TT(t2[:], sig[:], bka[:, :], ALU.mult, rd=[sig, bka], wr=[t2])
                            TT(mixT[:, dc, :], t2[:], mixT[:, dc, :], ALU.add, rd=[t2, mixT], wr=[mixT])
                    R.done(cm)
                R.done(cu)
            if CUT < 60:
                return
            for dh in range(2):
                co, wo = R.get(wchunk(wout_d[:, dh * 512:(dh + 1) * 512], 512))
                wov = V3(wo[:, :], 8)
                for i in range(4):
                    bk = PS()
                    for k in range(8):
                        MM(bk[:, :], mixT[:, k, i * 128:(i + 1) * 128], wov[:, k, :], start=(k == 0), stop=(k == 7), rd=[mixT, wo], wr=[bk])
                    TT(tmp[:], bk[:, :], g1bc[:, dh * 512:(dh + 1) * 512], ALU.mult, rd=[bk, g1bc], wr=[tmp])
                    a_ = acc[:, jb + i, dh * 512:(dh + 1) * 512]
                    TT(a_, a_, tmp[:], ALU.add, rd=[tmp, ('acc', jb + i)], wr=[('acc', jb + i)])
                R.done(co)

        def alloc_moe(ph, tag):
            T = {}

            def a(name, shape, dt):
                T[name] = sb(f"{name}_{tag}", shape, dt, ph)
            a('sqj', [128, D], BF16)
            a('ss', [128, 8], F32)
            a('lnv', [128, 8], F32)
            a('rstd', [128, 8], F32)
            a('xn2', [128, D], F32)
            a('h2f', [128, 8, 128], F32)
            a('h2T', [128, 8, 1024], BF16)
            a('lg', [128, 36], F32)
            a('sm', [128, 16], F32)
            a('goh', [128, 4], F32)
            a('eg', [128, 4], F32)
            a('lesel', [128, 8], F32)
            a('m8', [128, 8], F32)
            a('c8', [128, 8], F32)
            a('c8b', [128, 8], F32)
            a('sg0', [128, 512], BF16)
            a('sg1', [128, 512], BF16)
            a('he0', [128, 4, 512], BF16)
            a('he1', [128, 4, 512], BF16)
            a('ot0', [128, D], F32)
            a('ot1', [128, D], F32)
            return T

        def moe_group(T, b, grp):
            ss, lnv, rstd, xn2, h2f, h2T = T['ss'], T['lnv'], T['rstd'], T['xn2'], T['h2f'], T['h2T']
            lg, sm, goh, eg, lesel, m8, c8, c8b = T['lg'], T['sm'], T['goh'], T['eg'], T['lesel'], T['m8'], T['c8'], T['c8b']
            for j in range(8):
                ACT(T['sqj'][:], acc[:, j, :], AF.Square, rd=[('acc', j)], wr=[T['sqj'], (ss.name, j)], accum_out=ss[:, j:j + 1])
            ACT(lnv[:], ss[:], AF.Ln, rd=[(ss.name, j) for j in range(8)], wr=[lnv], scale=1.0 / D, bias=EPS)
            ACT(rstd[:], lnv[:], AF.Exp, rd=[lnv], wr=[rstd], scale=-0.5)
            for j in range(8):
                TS(xn2[:], acc[:, j, :], rstd[:, j:j + 1], None, ALU.mult, rd=[('acc', j), rstd], wr=[xn2])
                tb = [PS(), PS()]
                for c in range(8):
                    TR(tb[c // 4][:, (c % 4) * 128:(c % 4 + 1) * 128], xn2[:, c * 128:(c + 1) * 128], identf[:], rd=[xn2], wr=[tb[c // 4]])
                for c in range(8):
                    TS(h2f[:, c, :], tb[c // 4][:, (c % 4) * 128:(c % 4 + 1) * 128], s2T[:, c, b:b + 1], modT[:, 16 + c, b:b + 1],
                       ALU.mult, ALU.add, rd=[tb[c // 4], s2T, modT], wr=[h2f])
                ACOPY(h2T[:, :, j * 128:(j + 1) * 128], h2f[:], rd=[h2f], wr=[h2T])
                bk = PS()
                for c in range(8):
                    MM(bk[:, 0:36], h2f[:, c, :], rw[:, c, :], start=(c == 0), stop=(c == 7), rd=[h2f, rw], wr=[bk])
                TT(lg[:], bk[:, 0:36], rb_bc[:], ALU.add, rd=[bk, rb_bc], wr=[lg])
                P.emit('dve', lambda e: e.tensor_reduce(out=sm[:, 0:1], in_=lg[:, 0:4], axis=AX.X, op=ALU.max), [lg], [sm])
                TS(goh[:], lg[:, 0:4], sm[:, 0:1], None, ALU.is_equal, rd=[lg, sm], wr=[goh])
                TS(sm[:, 1:2], sm[:, 0:1], -1.0, None, ALU.mult, rd=[sm], wr=[sm])
                ACT(eg[:], lg[:, 0:4], AF.Exp, rd=[lg, sm], wr=[eg, sm], bias=sm[:, 1:2], accum_out=sm[:, 2:3])
                P.emit('dve', lambda e: e.reciprocal(out=sm[:, 3:4], in_=sm[:, 2:3]), [sm], [sm])
                TS(lesel[:], lg[:, 4:12], goh[:, 0:1], None, ALU.mult, rd=[lg, goh], wr=[lesel])
                for g in range(1, 4):
                    STT(lesel[:], lg[:, 4 + 8 * g:12 + 8 * g], goh[:, g:g + 1], lesel[:], ALU.mult, ALU.add, rd=[lg, goh, lesel], wr=[lesel])
                P.emit('dve', lambda e: e.max(out=m8[:], in_=lesel[:]), [lesel], [m8])
                TT(sm[:, 4:5], m8[:, 1:2], m8[:, 0:1], ALU.subtract, rd=[m8, sm], wr=[sm])
                ACT(sm[:, 5:6], sm[:, 4:5], AF.Exp, rd=[sm], wr=[sm])
                TS(sm[:, 6:7], sm[:, 5:6], 1.0, None, ALU.add, rd=[sm], wr=[sm])
                P.emit('dve', lambda e: e.reciprocal(out=sm[:, 6:7], in_=sm[:, 6:7]), [sm], [sm])
                TT(sm[:, 7:8], sm[:, 5:6], sm[:, 6:7], ALU.mult, rd=[sm], wr=[sm])
                TT(sm[:, 8:9], sm[:, 6:7], sm[:, 3:4], ALU.mult, rd=[sm], wr=[sm])
                TT(sm[:, 9:10], sm[:, 7:8], sm[:, 3:4], ALU.mult, rd=[sm], wr=[sm])
                TS(c8[:], lesel[:], m8[:, 0:1], sm[:, 8:9], ALU.is_equal, ALU.mult, rd=[lesel, m8, sm], wr=[c8])
                TS(c8b[:], lesel[:], m8[:, 1:2], sm[:, 9:10], ALU.is_equal, ALU.mult, rd=[lesel, m8, sm], wr=[c8b])
                TT(c8[:], c8[:], c8b[:], ALU.add, rd=[c8, c8b], wr=[c8])
                for g in range(4):
                    TS(comb[:, j, 8 * g:8 * g + 8], c8[:], goh[:, g:g + 1], None, ALU.mult, rd=[c8, goh], wr=[comb])
            for e_ in range(NEXP if CUT >= 80 else 0):
                cg, wg = R.get(wchunk(eg_d[e_], 512))
                cu, wu_ = R.get(wchunk(eu_d[e_], 512))
                cd, wd = R.get(wchunk(ed_d[e_], 1024, nk=4))
                wgv, wuv, wdv = V3(wg[:, :], 8), V3(wu_[:, :], 8), V3(wd[:, :], 4)
                TT(wdv, wdv, g2bc[:].unsqueeze(1).broadcast_to([128, 4, D]), ALU.mult, rd=[wd, g2bc], wr=[wd])
                for half in range(2):
                    he = T[f'he{half}']
                    ts_ = slice(half * 512, (half + 1) * 512)
                    for fc in range(4):
                        bg_ = PS()
                        for k in range(8):
                            MM(bg_[:, :], wgv[:, k, fc * 128:(fc + 1) * 128], h2T[:, k, ts_], start=(k == 0), stop=(k == 7), rd=[wg, h2T], wr=[bg_])
                        bu_ = PS()
                        for k in range(8):
                            MM(bu_[:, :], wuv[:, k, fc * 128:(fc + 1) * 128], h2T[:, k, ts_], start=(k == 0), stop=(k == 7), rd=[wu_, h2T], wr=[bu_])
                        sg = T[f'sg{fc % 2}']
                        ACT(sg[:], bg_[:, :], AF.Silu, rd=[bg_], wr=[sg])
                        TT(he[:, fc, :], sg[:], bu_[:, :], ALU.mult, rd=[sg, bu_], wr=[he])
                    if half == 1:
                        R.done(cg)
                        R.done(cu)
                    for i in range(4):
                        j = half * 4 + i
                        for dh in range(2):
                            by = PS()
                            for fc in range(4):
                                MM(by[:, :], he[:, fc, i * 128:(i + 1) * 128], wdv[:, fc, dh * 512:(dh + 1) * 512],
                                   start=(fc == 0), stop=(fc == 3), rd=[he, wd], wr=[by])
                            a_ = acc[:, j, dh * 512:(dh + 1) * 512]
                            STT(a_, by[:, :], comb[:, j, e_:e_ + 1], a_, ALU.mult, ALU.add, rd=[by, comb, ('acc', j)], wr=[('acc', j)])
                R.done(cd)
            for j in range(8):
                ACT(T['sqj'][:], acc[:, j, :], AF.Square, rd=[('acc', j)], wr=[T['sqj'], (ss.name, j)], accum_out=ss[:, j:j + 1])
            ACT(lnv[:], ss[:], AF.Ln, rd=[(ss.name, j) for j in range(8)], wr=[lnv], scale=1.0 / D, bias=EPS)
            ACT(rstd[:], lnv[:], AF.Exp, rd=[lnv], wr=[rstd], scale=-0.5)
            for j in range(8):
                ot = T[f'ot{j % 2}']
                STT(ot[:], acc[:, j, :], rstd[:, j:j + 1], fg_bc[:], ALU.mult, ALU.mult, rd=[('acc', j), rstd, fg_bc], wr=[ot])
                r0 = b * S + grp * 1024 + j * 128
                DMA('sp', y_d[r0:r0 + 128, :], ot[:], f'st{j % 2}', [ot], [('yout', j % 2)])

        P.plan = True
        gen()
        P.plan = False
        gen()
        P.run()
    return nc


def _consts():
    f = np.float32
    c = {}
    c['identf'] = np.eye(128, dtype=f)
    k = np.arange(128)[:, None]
    q = np.arange(128)[None, :]
    c['maskD4'] = np.tile((k <= q).astype(f), (1, 4))
    c['maskW4'] = np.tile((k > q).astype(f), (1, 4))
    mc = np.zeros((128, 16, 128), f)
    n = np.arange(128)[:, None]
    for qi in range(16):
        t = qi * 128 + np.arange(128)[None, :]
        mc[:, qi, :] = ((16 * n + 31 <= t) & (n < 127)).astype(f)
    c['maskC'] = mc
    ea = np.zeros((32, 2048), f)
    ea[np.arange(2048) // 64, np.arange(2048)] = 1.0
    c['Eall'] = ea
    ptcur = np.zeros((128, 4, 128), f)
    ptfirst = np.zeros((128, 4, 128), f)
    ptprev = np.zeros((128, 4, 128), f)
    for g, w in enumerate((2, 4, 8, 16)):
        for t in range(128):
            for tp in range(max(0, t - w + 1), t + 1):
                ptcur[tp, g, t] += 1.0 / w
                ptfirst[tp, g, t] += 1.0 / min(t + 1, w)
            ptcur[t, g, t] -= 1.0
            ptfirst[t, g, t] -= 1.0
            for tp in range(t - w + 1, 0):
                ptprev[128 + tp, g, t] += 1.0 / w
    c['ptcur'], c['ptfirst'], c['ptprev'] = ptcur, ptfirst, ptprev
    s1 = np.arange(127)[:, None] * 16
    s2 = np.arange(32)[None, :] * 64
    ov = np.clip(np.minimum(s1 + 32, s2 + 64) - np.maximum(s1, s2), 0, None) / 32.0
    ovl = np.zeros((128, 33), f)
    ovl[:, 0] = 1.0
    ovl[:127, 1:] = ov
    c['ovl1'] = ovl
    sv = np.zeros((128, 16, 32), f)
    sbias = np.zeros((128, 16, 32), f)
    blk = np.arange(32)[None, :]
    for qi in range(16):
        t = qi * 128 + np.arange(128)
        cur = (t // 64)[:, None]
        valid = blk <= cur
        forced = (blk == 0) | (blk == cur) | (blk == cur - 1)
        sv[:, qi, :] = (valid & ~forced).astype(f)
        sbias[:, qi, :] = np.where(forced, 1e4, np.where(valid, 0.0, -1e30)).astype(f)
    c['selvalid'], c['selbias'] = sv, sbias
    return c


def _fm(v, nch):
    return np.ascontiguousarray(np.asarray(v, np.float32).reshape(nch, 128).T)


def prep_shared(inp):
    f = np.float32
    m = dict(_consts())
    m['ada_w'] = np.ascontiguousarray(inp['ada_w'][0], f)
    adab = np.asarray(inp['ada_b'][0], f)
    m['adabT'] = _fm(adab, 48)
    m['adab_bc'] = np.ascontiguousarray(np.broadcast_to(
        np.stack([adab[2048:3072], adab[5120:6144]], 0)[None], (128, 2, D)), f)
    m['n1g'] = _fm(inp['norm1_g'][0], 8)
    m['n2g'] = _fm(inp['norm2_g'][0], 8)
    m['fg_bc'] = np.ascontiguousarray(np.broadcast_to(np.asarray(inp['final_g'], f)[None], (128, D)), f)
    w_in = np.asarray(inp['w_in'][0], f)
    qperm = np.array([512 + (g * 4 + hh) * 64 + d for hh in range(4) for g in range(2) for d in range(64)])
    cols = np.concatenate([np.arange(0, 512), np.arange(1408, 1536), np.arange(1664, 1792), np.arange(1792, 1816),
                           qperm, np.arange(1024, 1152), np.arange(1152, 1280), np.arange(1280, 1408),
                           np.arange(1536, 1664), np.arange(1816, 3864)])
    m['w_in_p'] = np.ascontiguousarray(w_in[:, cols])
    m['pool_w_r'] = np.ascontiguousarray(np.transpose(np.asarray(inp['pool_w'][0], f), (1, 0, 2)))
    m['pscT'] = _fm(inp['pool_scale'][0], 4)
    pos = np.asarray(inp['cmp_pos'][0], f)
    pt = np.transpose(pos, (2, 0, 1))
    m['posT'] = np.ascontiguousarray(np.concatenate([pt, pt], 0))
    m['cmp_w1'] = np.ascontiguousarray(inp['cmp_w1'][0], f)
    b1 = np.asarray(inp['cmp_b1'][0], f)
    m['b1T'] = np.ascontiguousarray(np.transpose(b1.reshape(2, 2, 128), (2, 0, 1)))
    w2 = np.asarray(inp['cmp_w2'][0], f)
    m['w2r'] = np.ascontiguousarray(np.transpose(w2.reshape(2, 2, 128, 64), (2, 0, 1, 3)))
    m['w_up_pool'] = np.ascontiguousarray(inp['w_up_pool'][0], f)
    m['w_up_attn'] = np.ascontiguousarray(inp['w_up_attn'][0], f)
    m['w_out'] = np.ascontiguousarray(inp['w_out'][0], f)
    rwf = np.concatenate([np.asarray(inp['router_g_w'][0], f), np.asarray(inp['router_e_w'][0], f)], 1)
    m['rw'] = np.ascontiguousarray(np.transpose(rwf.reshape(8, 128, 36), (1, 0, 2)))
    rb = np.concatenate([np.asarray(inp['router_g_b'][0], f), np.asarray(inp['router_e_b'][0], f)], 0)
    m['rb_bc'] = np.ascontiguousarray(np.broadcast_to(rb[None], (128, 36)), f)
    m['exp_w_gate'] = np.ascontiguousarray(inp['exp_w_gate'][0], f)
    m['exp_w_up'] = np.ascontiguousarray(inp['exp_w_up'][0], f)
    m['exp_w_down'] = np.ascontiguousarray(inp['exp_w_down'][0], f)
    return m


def prep_core(inp, shared, b0, nseq):
    f = np.float32
    m = dict(shared)
    m['x'] = np.ascontiguousarray(np.asarray(inp['x'][b0:b0 + nseq], f).reshape(nseq * S, D))
    c = np.asarray(inp['c'][b0:b0 + nseq], f)
    ck = np.transpose(c.reshape(nseq, 8, 128), (2, 1, 0))
    m['cT'] = np.ascontiguousarray(ck)
    m['crep'] = np.ascontiguousarray(np.broadcast_to(np.transpose(c.reshape(nseq, 8, 128), (0, 2, 1))[:, :, :, None],
                                                     (nseq, 128, 8, 128)), f)
    return m


_NC_CACHE = {}


def kernel(**inputs):
    nseq = 32 // NCORES
    if nseq not in _NC_CACHE:
        _NC_CACHE[nseq] = build(nseq)
    nc = _NC_CACHE[nseq]
    shared = prep_shared(inputs)
    in_maps = [prep_core(inputs, shared, core * nseq, nseq) for core in range(NCORES)]
    res = run_bass_kernel_spmd(nc, in_maps, core_ids=list(range(NCORES)))
    out = np.concatenate([np.asarray(r["y"], np.float32).reshape(nseq, S, D) for r in res.results], axis=0)
    return out
```

```python
import numpy as np
from contextlib import ExitStack
import concourse.bass as bass
import concourse.mybir as mybir
from concourse.bass_utils import run_bass_kernel_spmd

F32 = mybir.dt.float32
BF16 = mybir.dt.bfloat16
AF = mybir.ActivationFunctionType
ALU = mybir.AluOpType
AX = mybir.AxisListType

D = 1024
S = 2048
NCORES = 8
NR = 4
NEXP = 32
EPS = 1e-6
CUT = 100


class Prog:
    ENGS = ['pe', 'act', 'dve', 'pool', 'sp']

    def __init__(self, nc, es):
        self.nc = nc
        self.es = es
        self.plan = False
        self.ops = {e: [] for e in self.ENGS}
        self.sems = {}
        self.semcount = {}
        self.known = {e: {} for e in self.ENGS}
        self.lastw = {}
        self.readers = {}
        self.nbank = 0
        for e in ['pe', 'act', 'dve', 'pool']:
            self.sem('c_' + e)

    def sem(self, name):
        if name not in self.sems:
            self.sems[name] = self.es.enter_context(self.nc.semaphore(name))
            self.semcount[name] = 0
        return name

    @staticmethod
    def _key(r):
        if isinstance(r, (str, tuple)):
            return r
        t = getattr(r, 'tensor', r)
        return t.name

    def emit(self, eng, fn, reads=(), writes=(), sem=None):
        if self.plan:
            return
        is_dma = sem is not None
        if not is_dma:
            sem = 'c_' + eng
            inc = 1
        else:
            self.sem(sem)
            inc = 16
        waits = {}

        def need(dep, raw):
            s, v, e, d = dep
            if (not d) and e == eng and eng == 'pe':
                return
            if d and is_dma and (not raw) and s == sem:
                return
            if self.known[eng].get(s, 0) >= v:
                return
            waits[s] = max(waits.get(s, 0), v)

        for r in reads:
            k = self._key(r)
            if k in self.lastw:
                need(self.lastw[k], True)
        for w in writes:
            k = self._key(w)
            if k in self.lastw:
                need(self.lastw[k], False)
            for dep in self.readers.get(k, {}).values():
                need(dep, False)
        for s, v in waits.items():
            self.known[eng][s] = v
        self.semcount[sem] += inc
        tick = self.semcount[sem]
        me = (sem, tick, eng, is_dma)
        self.ops[eng].append((list(waits.items()), fn, sem, inc))
        for r in reads:
            k = self._key(r)
            self.readers.setdefault(k, {})[(eng, sem)] = me
        for w in writes:
            k = self._key(w)
            self.lastw[k] = me
            self.readers[k] = {}

    def barrier(self):
        if self.plan:
            return
        for eng in self.ENGS:
            waits = []
            for s, c in self.semcount.items():
                if c > 0 and self.known[eng].get(s, 0) < c:
                    waits.append((s, c))
                    self.known[eng][s] = c
            if waits:
                self.ops[eng].append((waits, None, None, 0))

    def run(self):
        nc = self.nc
        P = self

        def replay(e, eng):
            for (waits, fn, sem, inc) in P.ops[e]:
                for (s, v) in waits:
                    eng.wait_ge(P.sems[s], v)
                if fn is not None:
                    ins = fn(eng)
                    ins.then_inc(P.sems[sem], inc)

        with nc.Block() as block:
            @block.tensor
            def _(eng):
                replay('pe', eng)

            @block.scalar
            def _(eng):
                replay('act', eng)

            @block.vector
            def _(eng):
                replay('dve', eng)

            @block.gpsimd
            def _(eng):
                replay('pool', eng)

            @block.sync
            def _(eng):
                replay('sp', eng)


class Ring:
    def __init__(self, P, slots):
        self.P = P
        self.slots = slots
        self.seq = []
        self.reset()

    def reset(self):
        self.idx = 0
        self.loaded = 0
        self.dead = set()

    def get(self, loader):
        i = self.idx
        self.idx += 1
        if self.P.plan:
            self.seq.append(loader)
            return i, self.slots[i % NR]
        self.top_up()
        assert self.loaded > i, ("ring too many live chunks", i)
        return i, self.slots[i % NR]

    def done(self, i):
        if self.P.plan:
            return
        self.dead.add(i)
        self.top_up()

    def top_up(self):
        while self.loaded < len(self.seq) and (self.loaded < NR or (self.loaded - NR) in self.dead):
            c = self.loaded
            self.seq[c](self.slots[c % NR], c % NR)
            self.loaded += 1


def build(nseq, dbg=False):
    nc = bass.Bass("TRN2", target_bir_lowering=False)
    ntok = nseq * S

    def din(name, shape, dt=F32):
        return nc.dram_tensor(name, list(shape), dt, kind="ExternalInput").ap()

    x_d = din("x", [ntok, D])
    crep_d = din("crep", [nseq, 128, 8, 128])
    cT_d = din("cT", [128, 8, nseq])
    adaw_d = din("ada_w", [D, 6 * D])
    adabT_d = din("adabT", [128, 48])
    adabbc_d = din("adab_bc", [128, 2, D])
    n1g_d = din("n1g", [128, 8])
    n2g_d = din("n2g", [128, 8])
    fgbc_d = din("fg_bc", [128, D])
    win_d = din("w_in_p", [D, 3864])
    poolw_d = din("pool_w_r", [128, 4, 128])
    psc_d = din("pscT", [128, 4])
    posT_d = din("posT", [128, 2, 32])
    w1_d = din("cmp_w1", [2, 2048, 256])
    b1T_d = din("b1T", [128, 2, 2])
    w2r_d = din("w2r", [128, 2, 2, 64])
    wup_d = din("w_up_pool", [512, D])
    wua_d = din("w_up_attn", [512, D])
    wout_d = din("w_out", [D, D])
    rw_d = din("rw", [128, 8, 36])
    rbbc_d = din("rb_bc", [128, 36])
    eg_d = din("exp_w_gate", [NEXP, D, 512])
    eu_d = din("exp_w_up", [NEXP, D, 512])
    ed_d = din("exp_w_down", [NEXP, 512, D])
    identf_d = din("identf", [128, 128])
    maskD_d = din("maskD4", [128, 512])
    maskW_d = din("maskW4", [128, 512])
    maskC_d = din("maskC", [128, 16, 128])
    eall_d = din("Eall", [32, 2048])
    ptcur_d = din("ptcur", [128, 4, 128])
    ptfirst_d = din("ptfirst", [128, 4, 128])
    ptprev_d = din("ptprev", [128, 4, 128])
    ovl_d = din("ovl1", [128, 33])
    selv_d = din("selvalid", [128, 16, 32])
    selb_d = din("selbias", [128, 16, 32])
    y_d = nc.dram_tensor("y", [ntok, D], F32, kind="ExternalOutput").ap()
    if dbg:
        dbg_d = nc.dram_tensor("dbg_x1", [ntok, D], F32, kind="ExternalOutput").ap()

    with ExitStack() as es:
        P = Prog(nc, es)

        uniq = [0]

        def sb(name, shape, dt, stack=es):
            uniq[0] += 1
            return stack.enter_context(nc.sbuf_tensor(f"{name}_u{uniq[0]}", list(shape), dt))

        identb = sb("identb", [128, 128], BF16)
        identf = sb("identf_s", [128, 128], F32)
        maskD4 = sb("maskD4_s", [128, 512], BF16)
        maskW4 = sb("maskW4_s", [128, 512], BF16)
        maskC = sb("maskC_s", [128, 16, 128], BF16)
        Eall = sb("Eall_s", [128, 2048], BF16)
        ptcur = sb("ptcur_s", [128, 4, 128], BF16)
        ptfirst = sb("ptfirst_s", [128, 4, 128], BF16)
        ptprev = sb("ptprev_s", [128, 4, 128], BF16)
        selvalid = sb("selvalid_s", [128, 16, 32], F32)
        selbiasc = sb("selbias_s", [128, 16, 32], F32)
        modT = sb("modT", [128, 32, nseq], F32)
        s1T = sb("s1T", [128, 8, nseq], F32)
        s2T = sb("s2T", [128, 8, nseq], F32)
        adabT = sb("adabT_s", [128, 48], F32)
        n1g = sb("n1g_s", [128, 8], F32)
        n2g = sb("n2g_s", [128, 8], F32)
        fg_bc = sb("fg_bc_s", [128, D], F32)
        g1bc = sb("g1bc", [128, D], F32)
        g2bc = sb("g2bc", [128, D], F32)
        poolw = sb("poolw_s", [128, 4, 128], BF16)
        pscT = sb("pscT_s", [128, 4], F32)
        posT = sb("posT_s", [128, 2, 32], BF16)
        b1T = sb("b1T_s", [128, 2, 2], F32)
        biasc = sb("biasc", [128, 2, 2], F32)
        w2sb = sb("w2sb", [128, 2, 2, 64], BF16)
        rw = sb("rw_s", [128, 8, 36], F32)
        rb_bc = sb("rb_bc_s", [128, 36], F32)
        crep = sb("crep_s", [128, 8, 128], BF16)
        cT = sb("cT_s", [128, 8, nseq], BF16)
        acc = sb("acc", [128, 8, D], F32)
        comb = sb("comb", [128, 8, 32], F32)
        kcT = sb("kcT", [128, S], BF16)
        vcT = sb("vcT", [128, S], BF16)
        ksT = sb("ksT", [128, S], BF16)
        kwT = sb("kwT", [128, S], BF16)
        kcmpT = sb("kcmpT", [128, 128], BF16)
        vcaug = sb("vcaug", [128, 2, 97], BF16)
        hidv = sb("hidv", [128, 2, 2, 128], BF16)
        vs_aug = sb("vs_aug", [128, 16, 2, 65], BF16)
        vw_aug = sb("vw_aug", [128, 16, 2, 65], BF16)
        bg = sb("bg", [128, 16, 24], F32)
        ucarry = sb("ucarry", [128, 512], BF16)
        ring = [sb(f"ring{i}", [128, 4096], BF16) for i in range(NR)]
        R = Ring(P, ring)

        banks = [es.enter_context(nc.psum_tensor(f"bank{i}", [128, 512], F32)) for i in range(6)]
        ACCA = es.enter_context(nc.psum_tensor("ACCA", [128, 512], F32))
        ACCB = es.enter_context(nc.psum_tensor("ACCB", [128, 512], F32))

        def PS():
            b = banks[P.nbank % 6]
            P.nbank += 1
            return b

        def MM(out, lhsT, rhs, start=True, stop=True, rd=(), wr=()):
            P.emit('pe', lambda e: e.matmul(out, lhsT, rhs, start=start, stop=stop, skip_group_check=True), rd, wr)

        def TR(out, in_, ident, rd=(), wr=()):
            P.emit('pe', lambda e: e.transpose(out, in_, ident), rd, wr)

        def ACT(out, in_, func, rd=(), wr=(), **kw):
            P.emit('act', lambda e: e.activation(out=out, in_=in_, func=func, **kw), rd, wr)

        def ACOPY(out, in_, rd=(), wr=()):
            P.emit('act', lambda e: e.copy(out=out, in_=in_), rd, wr)

        def TS(out, in0, s1, s2, op0, op1=None, rd=(), wr=(), eng='dve'):
            if op1 is None:
                P.emit(eng, lambda e: e.tensor_scalar(out=out, in0=in0, scalar1=s1, scalar2=None, op0=op0), rd, wr)
            else:
                P.emit(eng, lambda e: e.tensor_scalar(out=out, in0=in0, scalar1=s1, scalar2=s2, op0=op0, op1=op1), rd, wr)

        def TT(out, in0, in1, op, rd=(), wr=(), eng='dve'):
            P.emit(eng, lambda e: e.tensor_tensor(out=out, in0=in0, in1=in1, op=op), rd, wr)

        def STT(out, in0, scalar, in1, op0, op1, rd=(), wr=()):
            P.emit('dve', lambda e: e.scalar_tensor_tensor(out=out, in0=in0, scalar=scalar, in1=in1, op0=op0, op1=op1), rd, wr)

        def CP(out, in_, rd=(), wr=(), eng='dve'):
            P.emit(eng, lambda e: e.tensor_copy(out=out, in_=in_), rd, wr)

        def DMA(eng, out, in_, sem, rd=(), wr=()):
            P.emit(eng, lambda e: e.dma_start(out=out, in_=in_), rd, wr, sem=sem)

        def wchunk(src2d, ncols, nk=8):
            def loader(slot, si):
                dst = slot[:, 0:nk * ncols].rearrange("p (k n) -> p k n", k=nk)
                DMA('pool', dst, src2d.rearrange("(k p) n -> p k n", p=128), f'ring{si}', (), [slot])
            return loader

        def w1chunk(kv, half):
            def loader(slot, si):
                src = w1_d[kv].rearrange("(l d) c -> d l c", d=64)[:, 16 * half:16 * half + 16, :]
                for hp in range(2):
                    dst = slot[64 * hp:64 * hp + 64, :].rearrange("p (l c) -> p l c", l=16)
                    DMA('pool', dst, src, f'ring{si}', (), [slot])
            return loader

        def V3(ap2d, a):
            return ap2d.rearrange("p (a b) -> p a b", a=a)

        def gen():
            P.nbank = 0
            R.reset()
            for (dst, src) in [(identf, identf_d), (selvalid, selv_d), (selbiasc, selb_d), (adabT, adabT_d),
                               (n1g, n1g_d), (n2g, n2g_d), (fg_bc, fgbc_d), (pscT, psc_d), (b1T, b1T_d),
                               (rw, rw_d), (rb_bc, rbbc_d)]:
                DMA('sp', dst[:], src, 'cst_sp', (), [dst])
            P.emit('dve', lambda e: e.memset(Eall[:], 0.0), (), [Eall])
            DMA('pool', Eall[0:32, :], eall_d, 'cst_pool', (), [Eall])
            DMA('pool', Eall[64:96, :], eall_d, 'cst_pool', (), [Eall])
            for (dst, src) in [(maskD4, maskD_d), (maskW4, maskW_d), (maskC, maskC_d),
                               (ptcur, ptcur_d), (ptfirst, ptfirst_d), (ptprev, ptprev_d),
                               (poolw, poolw_d), (posT, posT_d), (w2sb, w2r_d), (cT, cT_d)]:
                DMA('pool', dst[:], src, 'cst_pool', (), [dst])
            DMA('pool', identb[:], identf_d, 'cst_pool', (), [identb])
            for g in range(2):
                DMA('pool', vcaug[:, g, 64:97], ovl_d, 'cst_pool', (), [vcaug])
            P.emit('dve', lambda e: e.memset(hidv[:], 0.0), (), [hidv])
            P.emit('dve', lambda e: e.memset(kcmpT[:], 0.0), (), [kcmpT])
            P.emit('dve', lambda e: e.memset(vs_aug[:], 1.0), (), [vs_aug])
            P.emit('dve', lambda e: e.memset(vw_aug[:], 1.0), (), [vw_aug])
            P.emit('dve', lambda e: e.memset(ucarry[:], 0.0), (), [ucarry])
            for g in range(2):
                P.emit('dve', lambda e, g=g: e.memset(vcaug[:, g, 0:64], 0.0), (), [vcaug])
            P.barrier()

            for mi, c0 in enumerate([0, 1024, 3072, 4096]):
                for hf in range(2):
                    ci, w = R.get(wchunk(adaw_d[:, c0 + 512 * hf:c0 + 512 * hf + 512], 512))
                    wv = V3(w[:, :], 8)
                    bk = PS()
                    for fc in range(4):
                        for k in range(8):
                            MM(bk[:, fc * nseq:(fc + 1) * nseq], wv[:, k, fc * 128:(fc + 1) * 128], cT[:, k, :],
                               start=(k == 0), stop=(k == 7), rd=[w, cT], wr=[bk])
                    R.done(ci)
                    j0 = mi * 8 + hf * 4
                    cb = (c0 // 128) + hf * 4
                    TT(modT[:, j0:j0 + 4, :], V3(bk[:, 0:4 * nseq], 4),
                       adabT[:, cb:cb + 4].unsqueeze(2).broadcast_to([128, 4, nseq]), ALU.add, rd=[bk, adabT], wr=[modT])
            TS(s1T[:], modT[:, 8:16, :], 1.0, None, ALU.add, rd=[modT], wr=[s1T])
            TT(s1T[:], s1T[:], n1g[:].unsqueeze(2).broadcast_to([128, 8, nseq]), ALU.mult, rd=[s1T, n1g], wr=[s1T])
            TS(s2T[:], modT[:, 24:32, :], 1.0, None, ALU.add, rd=[modT], wr=[s2T])
            TT(s2T[:], s2T[:], n2g[:].unsqueeze(2).broadcast_to([128, 8, nseq]), ALU.mult, rd=[s2T, n2g], wr=[s2T])

            for kv in range(2):
                ca, wa = R.get(w1chunk(kv, 0))
                cb_, wb = R.get(w1chunk(kv, 1))
                for cc in range(2):
                    bk = PS()
                    for l in range(32):
                        w = wa if l < 16 else wb
                        wv = V3(w[:, :], 16)
                        MM(bk[:, 0:1], wv[0:64, l % 16, cc * 128:(cc + 1) * 128], posT[0:64, kv, l:l + 1],
                           start=(l == 0), stop=(l == 31), rd=[w, posT], wr=[bk])
                    TT(biasc[:, kv, cc:cc + 1], bk[:, 0:1], b1T[:, kv, cc:cc + 1], ALU.add, rd=[bk, b1T], wr=[biasc])
                R.done(ca)
                R.done(cb_)

            for b in range(nseq):
                seq_prologue(b)
                for grp in range(2):
                    with ExitStack() as ph:
                        T = alloc_mixer(ph, f"{b}_{grp}")
                        for sth in range(2):
                            mixer_supertile(T, b, grp * 2 + sth)
                    P.barrier()
                    if dbg:
                        for j in range(8):
                            r0 = b * S + grp * 1024 + j * 128
                            DMA('sp', dbg_d[r0:r0 + 128, :], acc[:, j, :], f'dbgst', [('acc', j)], ['dbgout'])
                        P.barrier()
                    with ExitStack() as ph:
                        T = alloc_moe(ph, f"{b}_{grp}")
                        moe_group(T, b, grp)
                    P.barrier()
            P.barrier()

        def seq_prologue(b):
            DMA('pool', crep[:], crep_d[b], 'crep', (), [crep])
            DMA('sp', g1bc[:], adabbc_d[:, 0, :], 'g1l', (), [g1bc])
            DMA('sp', g2bc[:], adabbc_d[:, 1, :], 'g2l', (), [g2bc])
            for (dst, c0) in [(g1bc, 2048), (g2bc, 5120)]:
                for hf in range(2):
                    ci, w = R.get(wchunk(adaw_d[:, c0 + 512 * hf:c0 + 512 * hf + 512], 512))
                    wv = V3(w[:, :], 8)
                    bk = PS()
                    for k in range(8):
                        MM(bk[:, :], crep[:, k, :], wv[:, k, :], start=(k == 0), stop=(k == 7), rd=[w, crep], wr=[bk])
                    R.done(ci)
                    TT(dst[:, hf * 512:(hf + 1) * 512], bk[:, :], dst[:, hf * 512:(hf + 1) * 512], ALU.add,
                       rd=[bk, dst], wr=[dst])

        def alloc_mixer(ph, tag):
            T = {}

            def a(name, shape, dt):
                T[name] = sb(f"{name}_{tag}", shape, dt, ph)
            a('sqj', [128, D], BF16)
            a('xn0', [128, D], BF16)
            a('xn1', [128, D], BF16)
            a('ss', [128, 4], F32)
            a('lnv', [128, 4], F32)
            a('rstd', [128, 4], F32)
            a('h1T', [128, 8, 512], BF16)
            a('qz0', [128, 4, 512], BF16)
            a('qz1', [128, 4, 512], BF16)
            a('selbT0', [128, 4, 128], BF16)
            a('selbT1', [128, 4, 128], BF16)
            a('u_tm', [128, 4, 512], BF16)
            a('pooledT', [128, 4, 512], BF16)
            a('ypoolT', [128, 4, 512], BF16)
            a('hidk', [128, 2, 32], BF16)
            for i in range(6):
                a(f'pe{i}', [128, 512], BF16)
            for g in range(2):
                a(f'pc{g}', [128, 512], BF16)
                a(f'pcm{g}', [128, 512], BF16)
                a(f'ocr{g}', [128, 4, 97], F32)
                a(f'osel{g}', [128, 4, 65], F32)
                a(f'owin{g}', [128, 4, 65], F32)
            a('rs4', [128, 4], F32)
            a('psc', [128, 32], F32)
            a('score', [128, 32], F32)
            a('m8', [128, 8], F32)
            a('selb', [128, 32], BF16)
            a('den', [128, 3, 4], F32)
            a('coef', [128, 3, 4], F32)
            a('o1', [128, 4, 64], F32)
            a('o2', [128, 4, 64], F32)
            a('oattn0', [128, 512], BF16)
            a('oattn1', [128, 512], BF16)
            a('oattnT', [128, 4, 512], BF16)
            a('sig', [128, 512], F32)
            a('t2', [128, 512], F32)
            a('mixT', [128, 8, 512], BF16)
            a('tmp', [128, 512], F32)
            return T

        def mixer_supertile(T, b, st):
            h1T, u_tm = T['h1T'], T['u_tm']
            qz = [T['qz0'], T['qz1']]
            if 'zeroed' not in T:
                T['zeroed'] = True
                P.emit('dve', lambda e: e.memset(T['qz0'][:], 0.0), (), [T['qz0']])
                P.emit('dve', lambda e: e.memset(T['qz1'][:], 0.0), (), [T['qz1']])
                P.emit('dve', lambda e: e.memset(T['selbT0'][:], 0.0), (), [T['selbT0']])
                P.emit('dve', lambda e: e.memset(T['selbT1'][:], 0.0), (), [T['selbT1']])
            tok0 = st * 512
            jb = (st % 2) * 4
            for i in range(4):
                r0 = b * S + tok0 + i * 128
                DMA('sp', acc[:, jb + i, :], x_d[r0:r0 + 128, :], f'xl{jb + i}', (), [('acc', jb + i)])
            ss, lnv, rstd = T['ss'], T['lnv'], T['rstd']
            for i in range(4):
                ACT(T['sqj'][:], acc[:, jb + i, :], AF.Square, rd=[('acc', jb + i)], wr=[T['sqj'], (ss.name, i)],
                    accum_out=ss[:, i:i + 1])
            ACT(lnv[:], ss[:], AF.Ln, rd=[(ss.name, i) for i in range(4)], wr=[lnv], scale=1.0 / D, bias=EPS)
            ACT(rstd[:], lnv[:], AF.Exp, rd=[lnv], wr=[rstd], scale=-0.5)
            tb = [PS() for _ in range(4)]
            tbv = [t[:, :].bitcast(BF16) for t in tb]
            for i in range(4):
                xn = T[f'xn{i % 2}']
                TS(xn[:], acc[:, jb + i, :], rstd[:, i:i + 1], None, ALU.mult, rd=[('acc', jb + i), rstd], wr=[xn])
                for c in range(8):
                    o = tbv[c // 2][:, (c % 2) * 512 + i * 128:(c % 2) * 512 + (i + 1) * 128]
                    TR(o, xn[:, c * 128:(c + 1) * 128], identb[:], rd=[xn], wr=[tb[c // 2]])
            for c in range(8):
                TS(h1T[:, c, :], tbv[c // 2][:, (c % 2) * 512:(c % 2 + 1) * 512], s1T[:, c, b:b + 1], modT[:, c, b:b + 1],
                   ALU.mult, ALU.add, rd=[tb[c // 2], s1T, modT], wr=[h1T])
            if CUT < 10:
                return
            ci, w = R.get(wchunk(win_d[:, 0:512], 512))
            wv = V3(w[:, :], 8)
            for i in range(4):
                bk = PS()
                for k in range(8):
                    MM(bk[:, :], h1T[:, k, i * 128:(i + 1) * 128], wv[:, k, :], start=(k == 0), stop=(k == 7), rd=[h1T, w], wr=[bk])
                ACOPY(u_tm[:, i, :], bk[:, :], rd=[bk], wr=[u_tm])
            R.done(ci)
            ci, w = R.get(wchunk(win_d[:, 512:792], 280))
            wv = w[:, 0:8 * 280].rearrange("p (k n) -> p k n", k=8)
            for i in range(4):
                tt = st * 4 + i
                bk = PS()
                for k in range(8):
                    MM(bk[:, 0:280], h1T[:, k, i * 128:(i + 1) * 128], wv[:, k, :], start=(k == 0), stop=(k == 7), rd=[h1T, w], wr=[bk])
                ACOPY(vs_aug[:, tt, :, 0:64], V3(bk[:, 0:128], 2), rd=[bk], wr=[vs_aug])
                ACOPY(vw_aug[:, tt, :, 0:64], V3(bk[:, 128:256], 2), rd=[bk], wr=[vw_aug])
                ACT(bg[:, tt, :], bk[:, 256:280], AF.Sigmoid, rd=[bk], wr=[bg])
            R.done(ci)
            ci, w = R.get(wchunk(win_d[:, 792:1304], 512))
            wv = V3(w[:, :], 8)
            for hh in range(4):
                bk = PS()
                for k in range(8):
                    MM(bk[:, :], wv[:, k, hh * 128:(hh + 1) * 128], h1T[:, k, :], start=(k == 0), stop=(k == 7), rd=[h1T, w], wr=[bk])
                if hh % 2 == 0:
                    ACOPY(qz[0][0:64, hh, :], bk[0:64, :], rd=[bk], wr=[qz[0]])
                    ACOPY(qz[1][64:128, hh, :], bk[64:128, :], rd=[bk], wr=[qz[1]])
                else:
                    CP(qz[0][0:64, hh, :], bk[0:64, :], rd=[bk], wr=[qz[0]])
                    CP(qz[1][64:128, hh, :], bk[64:128, :], rd=[bk], wr=[qz[1]])
            R.done(ci)
            ci, w = R.get(wchunk(win_d[:, 1304:1816], 512))
            wv = V3(w[:, :], 8)
            for n, dst in enumerate([kcT, vcT, ksT, kwT]):
                bk = PS()
                for k in range(8):
                    MM(bk[:, :], wv[:, k, n * 128:(n + 1) * 128], h1T[:, k, :], start=(k == 0), stop=(k == 7), rd=[h1T, w], wr=[bk])
                if n % 2 == 0:
                    ACOPY(dst[:, tok0:tok0 + 512], bk[:, :], rd=[bk], wr=[dst])
                else:
                    CP(dst[:, tok0:tok0 + 512], bk[:, :], rd=[bk], wr=[dst])
            R.done(ci)
            if CUT < 20:
                return
            pooledT, ypoolT = T['pooledT'], T['ypoolT']
            for g in range(4):
                bk = PS()
                gs = slice(g * 128, (g + 1) * 128)
                for i in range(4):
                    tt = st * 4 + i
                    o = bk[:, i * 128:(i + 1) * 128]
                    if tt == 0:
                        MM(o, u_tm[:, 0, gs], ptfirst[:, g, :], rd=[u_tm, ptfirst], wr=[bk])
                    else:
                        MM(o, u_tm[:, i, gs], ptcur[:, g, :], start=True, stop=False, rd=[u_tm, ptcur], wr=[bk])
                        if i > 0:
                            MM(o, u_tm[64:128, i - 1, gs], ptprev[64:128, g, :], start=False, stop=True, rd=[u_tm, ptprev], wr=[bk])
                        else:
                            MM(o, ucarry[64:128, gs], ptprev[64:128, g, :], start=False, stop=True, rd=[ucarry, ptprev], wr=[bk])
                ACOPY(pooledT[:, g, :], bk[:, :], rd=[bk], wr=[pooledT])
                bk2 = PS()
                MM(bk2[:, :], poolw[:, g, :], pooledT[:, g, :], rd=[poolw, pooledT], wr=[bk2])
                TS(ypoolT[:, g, :], bk2[:, :], pscT[:, g:g + 1], None, ALU.mult, rd=[bk2, pscT], wr=[ypoolT])
            CP(ucarry[:], u_tm[:, 3, :], rd=[u_tm], wr=[ucarry])
            if CUT < 30:
                return
            i0 = max(0, 32 * st - 1)
            i1 = 32 * (st + 1) - 1
            nb = i1 - i0
            hidk = T['hidk']
            for kv in range(2):
                src = kcT if kv == 0 else vcT
                ca, wa = R.get(w1chunk(kv, 0))
                cb_, wb = R.get(w1chunk(kv, 1))
                for g in range(2):
                    for cc in range(2):
                        bk = PS()
                        for l in range(32):
                            w = wa if l < 16 else wb
                            wv = V3(w[:, :], 16)
                            t0 = 16 * i0 + l
                            MM(bk[:, 0:nb], wv[64 * g:64 * g + 64, l % 16, cc * 128:(cc + 1) * 128],
                               src[64 * g:64 * g + 64, t0:t0 + 16 * (nb - 1) + 1:16],
                               start=(l == 0), stop=(l == 31), rd=[w, src], wr=[bk])
                        if kv == 0:
                            ACT(hidk[:, cc, 0:nb], bk[:, 0:nb], AF.Gelu_apprx_tanh, rd=[bk, biasc], wr=[hidk],
                                bias=biasc[:, kv, cc:cc + 1])
                        else:
                            ACT(hidv[:, g, cc, i0:i1], bk[:, 0:nb], AF.Gelu_apprx_tanh, rd=[bk, biasc], wr=[hidv],
                                bias=biasc[:, kv, cc:cc + 1])
                    bk = PS()
                    if kv == 0:
                        for cc in range(2):
                            MM(bk[64 * g:64 * g + 64, 0:nb], w2sb[:, 0, cc, :], hidk[:, cc, 0:nb],
                               start=(cc == 0), stop=(cc == 1), rd=[w2sb, hidk], wr=[bk])
                        CP(kcmpT[64 * g:64 * g + 64, i0:i1], bk[64 * g:64 * g + 64, 0:nb], rd=[bk], wr=[kcmpT])
                    else:
                        for cc in range(2):
                            MM(bk[:, 0:64], hidv[:, g, cc, :], w2sb[:, 1, cc, :],
                               start=(cc == 0), stop=(cc == 1), rd=[w2sb, hidv], wr=[bk])
                        CP(vcaug[:, g, 0:64], bk[:, 0:64], rd=[bk], wr=[vcaug])
                R.done(ca)
                R.done(cb_)
            if CUT < 40:
                return
            oattnT = T['oattnT']
            npe = [0]

            def next_pe():
                t = T[f'pe{npe[0] % 6}']
                npe[0] += 1
                return t

            def rq_(g, i):
                return qz[g][:, :, i * 128:(i + 1) * 128]

            def attn_cmp(i, qi, g):
                gp = slice(64 * g, 64 * g + 64)
                ocr = T[f'ocr{g}']
                pc, pcm = T[f'pc{g}'], T[f'pcm{g}']
                bk = PS()
                MM(V3(bk[:, :], 4), kcmpT[:, :], rq_(g, i), rd=[kcmpT, qz[g]], wr=[bk])
                ACT(pc[:], bk[:, :], AF.Exp, rd=[bk], wr=[pc], scale=0.125)
                TT(V3(pcm[:, :], 4), V3(pc[:, :], 4), maskC[:, qi, :].unsqueeze(1).broadcast_to([128, 4, 128]), ALU.mult,
                   rd=[pc, maskC], wr=[pcm])
                bk2 = PS()
                for hh in range(4):
                    MM(bk2[:, hh * 97:(hh + 1) * 97], pcm[:, hh * 128:(hh + 1) * 128], vcaug[:, g, :], rd=[pcm, vcaug], wr=[bk2])
                ACOPY(ocr[:].rearrange("p a b -> p (a b)"), bk2[:, 0:388], rd=[bk2], wr=[ocr])

            def attn_select(i, qi, g):
                ocr = T[f'ocr{g}']
                rs4, psc, score, m8, selb, selbT = T['rs4'], T['psc'], T['score'], T['m8'], T['selb'], T[f'selbT{g}']
                TS(rs4[:], ocr[:, :, 64], 1e-30, None, ALU.max, rd=[ocr], wr=[rs4])
                P.emit('dve', lambda e: e.reciprocal(out=rs4[:], in_=rs4[:]), [rs4], [rs4])
                TS(psc[:], ocr[:, 0, 65:97], rs4[:, 0:1], None, ALU.mult, rd=[ocr, rs4], wr=[psc])
                for hh in range(1, 4):
                    STT(psc[:], ocr[:, hh, 65:97], rs4[:, hh:hh + 1], psc[:], ALU.mult, ALU.add, rd=[ocr, rs4, psc], wr=[psc])
                TT(score[:], psc[:], selvalid[:, qi, :], ALU.mult, rd=[psc, selvalid], wr=[score])
                TT(score[:], score[:], selbiasc[:, qi, :], ALU.add, rd=[score, selbiasc], wr=[score])
                P.emit('dve', lambda e: e.max(out=m8[:], in_=score[:]), [score], [m8])
                TS(score[:], score[:], m8[:, 7:8], None, ALU.is_ge, rd=[score, m8], wr=[score])
                TS(selb[:], score[:], 1.0, 30000.0, ALU.subtract, ALU.mult, rd=[score], wr=[selb])
                bkt = PS()
                bktv = bkt[:, :].bitcast(BF16)
                sp_ = slice(64 * g, 64 * g + 32)
                TR(bktv[sp_, 0:128], selb[:, :], identb[:], rd=[selb], wr=[bkt])
                CP(selbT[sp_], bktv[sp_, 0:128].unsqueeze(1).broadcast_to([32, 4, 128]), rd=[bkt], wr=[selbT])

            def attn_combine(i, qi, g, oattn):
                ocr, osel, owin = T[f'ocr{g}'], T[f'osel{g}'], T[f'owin{g}']
                den, coef, o1, o2 = T['den'], T['coef'], T['o1'], T['o2']
                CP(den[:, 0, :], ocr[:, :, 64], rd=[ocr], wr=[den])
                CP(den[:, 1, :], osel[:, :, 64], rd=[osel], wr=[den])
                CP(den[:, 2, :], owin[:, :, 64], rd=[owin], wr=[den])
                TS(den[:], den[:], 1e-30, None, ALU.max, rd=[den], wr=[den])
                P.emit('dve', lambda e: e.reciprocal(out=den[:], in_=den[:]), [den], [den])
                gview = bg[:, qi, :].rearrange("p (r h) -> p r h", r=3)[:, :, 4 * g:4 * g + 4]
                TT(coef[:], den[:], gview, ALU.mult, rd=[den, bg], wr=[coef])

                def bc(r):
                    return coef[:, r, :].unsqueeze(2).broadcast_to([128, 4, 64])
                TT(o1[:], ocr[:, :, 0:64], bc(0), ALU.mult, rd=[ocr, coef], wr=[o1])
                TT(o2[:], osel[:, :, 0:64], bc(1), ALU.mult, rd=[osel, coef], wr=[o2])
                TT(o1[:], o1[:], o2[:], ALU.add, rd=[o1, o2], wr=[o1])
                TT(o2[:], owin[:, :, 0:64], bc(2), ALU.mult, rd=[owin, coef], wr=[o2])
                TT(V3(oattn[:, g * 256:(g + 1) * 256], 4), o1[:], o2[:], ALU.add, rd=[o1, o2], wr=[oattn])

            def attn_transposes(i, oattn):
                bkt = PS()
                bktv = bkt[:, :].bitcast(BF16)
                for c in range(4):
                    TR(bktv[:, c * 128:(c + 1) * 128], oattn[:, c * 128:(c + 1) * 128], identb[:], rd=[oattn], wr=[bkt])
                CP(oattnT[:, :, i * 128:(i + 1) * 128], V3(bktv[:, 0:512], 4), rd=[bkt], wr=[oattnT])

            pending_tr = None
            for i in range(4):
                qi = st * 4 + i
                oattn = T[f'oattn{i % 2}']
                for g in range(2):
                    attn_cmp(i, qi, g)
                for g in range(2):
                    attn_select(i, qi, g)
                if pending_tr is not None:
                    attn_transposes(*pending_tr)
                    pending_tr = None
                tasks = []
                k0 = max(0, qi - 4)
                for g in range(2):
                    for kj in range(k0, qi + 1):
                        tasks.append(('win', g, kj, kj == k0, kj == qi))
                    for kj in range(qi + 1):
                        tasks.append(('sel', g, kj, kj == 0, kj == qi))
                nt = len(tasks)
                sbank = [None] * nt
                ptile = [None] * nt

                def S_(t):
                    br, g, kj, first, last = tasks[t]
                    gp = slice(64 * g, 64 * g + 64)
                    bk = PS()
                    sbank[t] = bk
                    if br == 'win':
                        MM(V3(bk[:, :], 4), kwT[:, kj * 128:(kj + 1) * 128], rq_(g, i), rd=[kwT, qz[g]], wr=[bk])
                    else:
                        sp_ = slice(64 * g, 64 * g + 32)
                        MM(V3(bk[:, :], 4), ksT[:, kj * 128:(kj + 1) * 128], rq_(g, i), start=True, stop=False, rd=[ksT, qz[g]], wr=[bk])
                        MM(V3(bk[:, :], 4), Eall[:, kj * 128:(kj + 1) * 128], T[f'selbT{g}'][:], start=False, stop=True,
                           rd=[T[f'selbT{g}']], wr=[bk])

                def E_(t):
                    br, g, kj, first, last = tasks[t]
                    bk = sbank[t]
                    pt = next_pe()
                    ptile[t] = pt
                    ACT(pt[:], bk[:, :], AF.Exp, rd=[bk], wr=[pt], scale=0.125)
                    if kj == qi:
                        TT(pt[:], pt[:], maskD4[:], ALU.mult, rd=[pt], wr=[pt])
                    elif br == 'win' and kj == qi - 4:
                        TT(pt[:], pt[:], maskW4[:], ALU.mult, rd=[pt], wr=[pt])

                def V_(t):
                    br, g, kj, first, last = tasks[t]
                    pt = ptile[t]
                    accb = ACCB if br == 'win' else ACCA
                    vaug = vw_aug if br == 'win' else vs_aug
                    for hh in range(4):
                        MM(accb[:, hh * 65:(hh + 1) * 65], pt[:, hh * 128:(hh + 1) * 128], vaug[:, kj, g, :],
                           start=(first and hh == 0), stop=last, rd=[pt, vaug], wr=[accb])
                    if last:
                        dst = T[f'owin{g}'] if br == 'win' else T[f'osel{g}']
                        ACOPY(dst[:].rearrange("p a b -> p (a b)"), accb[:, 0:260], rd=[accb], wr=[dst])

                for t in range(nt + 2):
                    if t < nt:
                        S_(t)
                    if 0 <= t - 1 < nt:
                        E_(t - 1)
                    if 0 <= t - 2 < nt:
                        V_(t - 2)
                for g in range(2):
                    attn_combine(i, qi, g, oattn)
                pending_tr = (i, oattn)
            attn_transposes(*pending_tr)
            if CUT < 50:
                return
            sig, t2, mixT, tmp = T['sig'], T['t2'], T['mixT'], T['tmp']
            for side in range(2):
                cu, wu_ = R.get(wchunk((wup_d if side == 0 else wua_d)[:, :], 1024, nk=4))
                wuv = V3(wu_[:, :], 4)
                srcT = ypoolT if side == 0 else oattnT
                for mh in range(2):
                    c0 = 1816 + side * 1024 + mh * 512
                    cm, wm = R.get(wchunk(win_d[:, c0:c0 + 512], 512))
                    wmv = V3(wm[:, :], 8)
                    for dq in range(4):
                        dc = mh * 4 + dq
                        bka = PS()
                        for kc in range(4):
                            MM(bka[:, :], wuv[:, kc, dc * 128:(dc + 1) * 128], srcT[:, kc, :], start=(kc == 0), stop=(kc == 3),
                               rd=[wu_, srcT], wr=[bka])
                        bkg = PS()
                        for k in range(8):
                            MM(bkg[:, :], wmv[:, k, dq * 128:(dq + 1) * 128], h1T[:, k, :], start=(k == 0), stop=(k == 7),
                               rd=[wm, h1T], wr=[bkg])
                        ACT(sig[:], bkg[:, :], AF.Sigmoid, rd=[bkg], wr=[sig])
                        if side == 0:
                            TT(mixT[:, dc, :], sig[:], bka[:, :], ALU.mult, rd=[sig, bka], wr=[mixT])
                        else:
                            TT(t2[:], sig[:], bka[:, :], ALU.mult, rd=[sig, bka], wr=[t2])
                            TT(mixT[:, dc, :], t2[:], mixT[:, dc, :], ALU.add, rd=[t2, mixT], wr=[mixT])
                    R.done(cm)
                R.done(cu)
            if CUT < 60:
                return
            for dh in range(2):
                co, wo = R.get(wchunk(wout_d[:, dh * 512:(dh + 1) * 512], 512))
                wov = V3(wo[:, :], 8)
                for i in range(4):
                    bk = PS()
                    for k in range(8):
                        MM(bk[:, :], mixT[:, k, i * 128:(i + 1) * 128], wov[:, k, :], start=(k == 0), stop=(k == 7), rd=[mixT, wo], wr=[bk])
                    TT(tmp[:], bk[:, :], g1bc[:, dh * 512:(dh + 1) * 512], ALU.mult, rd=[bk, g1bc], wr=[tmp])
                    a_ = acc[:, jb + i, dh * 512:(dh + 1) * 512]
                    TT(a_, a_, tmp[:], ALU.add, rd=[tmp, ('acc', jb + i)], wr=[('acc', jb + i)])
                R.done(co)

        def alloc_moe(ph, tag):
            T = {}

            def a(name, shape, dt):
                T[name] = sb(f"{name}_{tag}", shape, dt, ph)
            a('sqj', [128, D], BF16)
            a('ss', [128, 8], F32)
            a('lnv', [128, 8], F32)
            a('rstd', [128, 8], F32)
            a('xn2_0', [128, D], F32)
            a('xn2_1', [128, D], F32)
            a('h2f_0', [128, 8, 128], F32)
            a('h2f_1', [128, 8, 128], F32)
            a('h2T', [128, 8, 1024], BF16)
            a('lg', [128, 8, 36], F32)
            a('sm', [128, 8, 8], F32)
            a('goh', [128, 8, 4], F32)
            a('eg', [128, 8, 4], F32)
            a('lesel', [128, 8, 8], F32)
            a('m8', [128, 8, 8], F32)
            a('c8', [128, 8, 8], F32)
            a('c8b', [128, 8, 8], F32)
            a('sg0', [128, 512], BF16)
            a('sg1', [128, 512], BF16)
            a('he0', [128, 4, 512], BF16)
            a('he1', [128, 4, 512], BF16)
            a('ot0', [128, D], F32)
            a('ot1', [128, D], F32)
            return T

        def moe_group(T, b, grp):
            ss, lnv, rstd, h2T = T['ss'], T['lnv'], T['rstd'], T['h2T']
            lg, sm, goh, eg, lesel, m8, c8, c8b = T['lg'], T['sm'], T['goh'], T['eg'], T['lesel'], T['m8'], T['c8'], T['c8b']
            for j in range(8):
                ACT(T['sqj'][:], acc[:, j, :], AF.Square, rd=[('acc', j)], wr=[T['sqj'], (ss.name, j)], accum_out=ss[:, j:j + 1])
            ACT(lnv[:], ss[:], AF.Ln, rd=[(ss.name, j) for j in range(8)], wr=[lnv], scale=1.0 / D, bias=EPS)
            ACT(rstd[:], lnv[:], AF.Exp, rd=[lnv], wr=[rstd], scale=-0.5)
            for j in range(8):
                xn2 = T[f'xn2_{j % 2}']
                h2f = T[f'h2f_{j % 2}']
                ACT(xn2[:], acc[:, j, :], AF.Identity, rd=[('acc', j), rstd], wr=[xn2], scale=rstd[:, j:j + 1])
                tb = [PS(), PS()]
                for c in range(8):
                    TR(tb[c // 4][:, (c % 4) * 128:(c % 4 + 1) * 128], xn2[:, c * 128:(c + 1) * 128], identf[:], rd=[xn2], wr=[tb[c // 4]])
                for c in range(8):
                    TS(h2f[:, c, :], tb[c // 4][:, (c % 4) * 128:(c % 4 + 1) * 128], s2T[:, c, b:b + 1], modT[:, 16 + c, b:b + 1],
                       ALU.mult, ALU.add, rd=[tb[c // 4], s2T, modT], wr=[h2f])
                ACOPY(h2T[:, :, j * 128:(j + 1) * 128], h2f[:], rd=[h2f], wr=[h2T])
                bk = PS()
                for c in range(8):
                    MM(bk[:, 0:36], h2f[:, c, :], rw[:, c, :], start=(c == 0), stop=(c == 7), rd=[h2f, rw], wr=[bk])
                TT(lg[:, j, :], bk[:, 0:36], rb_bc[:], ALU.add, rd=[bk, rb_bc], wr=[lg])
            def b3(ap2, n):
                return ap2.unsqueeze(2).broadcast_to([128, 8, n])
            L4 = lg[:, :, 0:4]
            gmax, gsum, gp_ = sm[:, 0, :], sm[:, 1, :], sm[:, 2, :]
            dd, ed, w0, w1 = sm[:, 3, :], sm[:, 4, :], sm[:, 5, :], sm[:, 6, :]
            P.emit('dve', lambda e: e.tensor_reduce(out=gmax, in_=L4, axis=AX.X, op=ALU.max), [lg], [sm])
            TT(goh[:], L4, b3(gmax, 4), ALU.is_equal, rd=[lg, sm], wr=[goh])
            TT(eg[:], L4, b3(gmax, 4), ALU.subtract, rd=[lg, sm], wr=[eg])
            ACT(eg[:], eg[:], AF.Exp, rd=[eg], wr=[eg])
            P.emit('dve', lambda e: e.tensor_reduce(out=gsum, in_=eg[:], axis=AX.X, op=ALU.add), [eg], [sm])
            P.emit('dve', lambda e: e.reciprocal(out=gp_, in_=gsum), [sm], [sm])
            TT(lesel[:], lg[:, :, 4:12], b3(goh[:, :, 0], 8), ALU.mult, rd=[lg, goh], wr=[lesel])
            for g in range(1, 4):
                TT(c8b[:], lg[:, :, 4 + 8 * g:12 + 8 * g], b3(goh[:, :, g], 8), ALU.mult, rd=[lg, goh], wr=[c8b])
                TT(lesel[:], lesel[:], c8b[:], ALU.add, rd=[lesel, c8b], wr=[lesel])
            for j in range(8):
                P.emit('dve', lambda e, j=j: e.max(out=m8[:, j, :], in_=lesel[:, j, :]), [lesel], [m8])
            TT(dd, m8[:, :, 1], m8[:, :, 0], ALU.subtract, rd=[m8], wr=[sm])
            ACT(ed, dd, AF.Exp, rd=[sm], wr=[sm])
            TS(w0, ed, 1.0, None, ALU.add, rd=[sm], wr=[sm])
            P.emit('dve', lambda e: e.reciprocal(out=w0, in_=w0), [sm], [sm])
            TT(w1, ed, w0, ALU.mult, rd=[sm], wr=[sm])
            TT(w0, w0, gp_, ALU.mult, rd=[sm], wr=[sm])
            TT(w1, w1, gp_, ALU.mult, rd=[sm], wr=[sm])
            TT(c8[:], lesel[:], b3(m8[:, :, 0], 8), ALU.is_equal, rd=[lesel, m8], wr=[c8])
            TT(c8[:], c8[:], b3(w0, 8), ALU.mult, rd=[c8, sm], wr=[c8])
            TT(c8b[:], lesel[:], b3(m8[:, :, 1], 8), ALU.is_equal, rd=[lesel, m8], wr=[c8b])
            TT(c8b[:], c8b[:], b3(w1, 8), ALU.mult, rd=[c8b, sm], wr=[c8b])
            TT(c8[:], c8[:], c8b[:], ALU.add, rd=[c8, c8b], wr=[c8])
            for g in range(4):
                TT(comb[:, :, 8 * g:8 * g + 8], c8[:], b3(goh[:, :, g], 8), ALU.mult, rd=[c8, goh], wr=[comb])
            for e_ in range(NEXP if CUT >= 80 else 0):
                cg, wg = R.get(wchunk(eg_d[e_], 512))
                cu, wu_ = R.get(wchunk(eu_d[e_], 512))
                cd, wd = R.get(wchunk(ed_d[e_], 1024, nk=4))
                wgv, wuv, wdv = V3(wg[:, :], 8), V3(wu_[:, :], 8), V3(wd[:, :], 4)
                TT(wdv, wdv, g2bc[:].unsqueeze(1).broadcast_to([128, 4, D]), ALU.mult, rd=[wd, g2bc], wr=[wd])
                for half in range(2):
                    he = T[f'he{half}']
                    ts_ = slice(half * 512, (half + 1) * 512)
                    for fc in range(4):
                        bg_ = PS()
                        for k in range(8):
                            MM(bg_[:, :], wgv[:, k, fc * 128:(fc + 1) * 128], h2T[:, k, ts_], start=(k == 0), stop=(k == 7), rd=[wg, h2T], wr=[bg_])
                        bu_ = PS()
                        for k in range(8):
                            MM(bu_[:, :], wuv[:, k, fc * 128:(fc + 1) * 128], h2T[:, k, ts_], start=(k == 0), stop=(k == 7), rd=[wu_, h2T], wr=[bu_])
                        sg = T[f'sg{fc % 2}']
                        ACT(sg[:], bg_[:, :], AF.Silu, rd=[bg_], wr=[sg])
                        TT(he[:, fc, :], sg[:], bu_[:, :], ALU.mult, rd=[sg, bu_], wr=[he])
                    if half == 1:
                        R.done(cg)
                        R.done(cu)
                    for i in range(4):
                        j = half * 4 + i
                        for dh in range(2):
                            by = PS()
                            for fc in range(4):
                                MM(by[:, :], he[:, fc, i * 128:(i + 1) * 128], wdv[:, fc, dh * 512:(dh + 1) * 512],
                                   start=(fc == 0), stop=(fc == 3), rd=[he, wd], wr=[by])
                            a_ = acc[:, j, dh * 512:(dh + 1) * 512]
                            STT(a_, by[:, :], comb[:, j, e_:e_ + 1], a_, ALU.mult, ALU.add, rd=[by, comb, ('acc', j)], wr=[('acc', j)])
                R.done(cd)
            for j in range(8):
                ACT(T['sqj'][:], acc[:, j, :], AF.Square, rd=[('acc', j)], wr=[T['sqj'], (ss.name, j)], accum_out=ss[:, j:j + 1])
            ACT(lnv[:], ss[:], AF.Ln, rd=[(ss.name, j) for j in range(8)], wr=[lnv], scale=1.0 / D, bias=EPS)
            ACT(rstd[:], lnv[:], AF.Exp, rd=[lnv], wr=[rstd], scale=-0.5)
            for j in range(8):
                ot = T[f'ot{j % 2}']
                STT(ot[:], acc[:, j, :], rstd[:, j:j + 1], fg_bc[:], ALU.mult, ALU.mult, rd=[('acc', j), rstd, fg_bc], wr=[ot])
                r0 = b * S + grp * 1024 + j * 128
                DMA('sp', y_d[r0:r0 + 128, :], ot[:], f'st{j % 2}', [ot], [('yout', j % 2)])

        P.plan = True
        gen()
        P.plan = False
        gen()
        P.run()
    return nc


def _consts():
    f = np.float32
    c = {}
    c['identf'] = np.eye(128, dtype=f)
    k = np.arange(128)[:, None]
    q = np.arange(128)[None, :]
    c['maskD4'] = np.tile((k <= q).astype(f), (1, 4))
    c['maskW4'] = np.tile((k > q).astype(f), (1, 4))
    mc = np.zeros((128, 16, 128), f)
    n = np.arange(128)[:, None]
    for qi in range(16):
        t = qi * 128 + np.arange(128)[None, :]
        mc[:, qi, :] = ((16 * n + 31 <= t) & (n < 127)).astype(f)
    c['maskC'] = mc
    ea = np.zeros((32, 2048), f)
    ea[np.arange(2048) // 64, np.arange(2048)] = 1.0
    c['Eall'] = ea
    ptcur = np.zeros((128, 4, 128), f)
    ptfirst = np.zeros((128, 4, 128), f)
    ptprev = np.zeros((128, 4, 128), f)
    for g, w in enumerate((2, 4, 8, 16)):
        for t in range(128):
            for tp in range(max(0, t - w + 1), t + 1):
                ptcur[tp, g, t] += 1.0 / w
                ptfirst[tp, g, t] += 1.0 / min(t + 1, w)
            ptcur[t, g, t] -= 1.0
            ptfirst[t, g, t] -= 1.0
            for tp in range(t - w + 1, 0):
                ptprev[128 + tp, g, t] += 1.0 / w
    c['ptcur'], c['ptfirst'], c['ptprev'] = ptcur, ptfirst, ptprev
    s1 = np.arange(127)[:, None] * 16
    s2 = np.arange(32)[None, :] * 64
    ov = np.clip(np.minimum(s1 + 32, s2 + 64) - np.maximum(s1, s2), 0, None) / 32.0
    ovl = np.zeros((128, 33), f)
    ovl[:, 0] = 1.0
    ovl[:127, 1:] = ov
    c['ovl1'] = ovl
    sv = np.zeros((128, 16, 32), f)
    sbias = np.zeros((128, 16, 32), f)
    blk = np.arange(32)[None, :]
    for qi in range(16):
        t = qi * 128 + np.arange(128)
        cur = (t // 64)[:, None]
        valid = blk <= cur
        forced = (blk == 0) | (blk == cur) | (blk == cur - 1)
        sv[:, qi, :] = (valid & ~forced).astype(f)
        sbias[:, qi, :] = np.where(forced, 1e4, np.where(valid, 0.0, -1e30)).astype(f)
    c['selvalid'], c['selbias'] = sv, sbias
    return c


def _fm(v, nch):
    return np.ascontiguousarray(np.asarray(v, np.float32).reshape(nch, 128).T)


def prep_shared(inp):
    f = np.float32
    m = dict(_consts())
    m['ada_w'] = np.ascontiguousarray(inp['ada_w'][0], f)
    adab = np.asarray(inp['ada_b'][0], f)
    m['adabT'] = _fm(adab, 48)
    m['adab_bc'] = np.ascontiguousarray(np.broadcast_to(
        np.stack([adab[2048:3072], adab[5120:6144]], 0)[None], (128, 2, D)), f)
    m['n1g'] = _fm(inp['norm1_g'][0], 8)
    m['n2g'] = _fm(inp['norm2_g'][0], 8)
    m['fg_bc'] = np.ascontiguousarray(np.broadcast_to(np.asarray(inp['final_g'], f)[None], (128, D)), f)
    w_in = np.asarray(inp['w_in'][0], f)
    qperm = np.array([512 + (g * 4 + hh) * 64 + d for hh in range(4) for g in range(2) for d in range(64)])
    cols = np.concatenate([np.arange(0, 512), np.arange(1408, 1536), np.arange(1664, 1792), np.arange(1792, 1816),
                           qperm, np.arange(1024, 1152), np.arange(1152, 1280), np.arange(1280, 1408),
                           np.arange(1536, 1664), np.arange(1816, 3864)])
    m['w_in_p'] = np.ascontiguousarray(w_in[:, cols])
    m['pool_w_r'] = np.ascontiguousarray(np.transpose(np.asarray(inp['pool_w'][0], f), (1, 0, 2)))
    m['pscT'] = _fm(inp['pool_scale'][0], 4)
    pos = np.asarray(inp['cmp_pos'][0], f)
    pt = np.transpose(pos, (2, 0, 1))
    m['posT'] = np.ascontiguousarray(np.concatenate([pt, pt], 0))
    m['cmp_w1'] = np.ascontiguousarray(inp['cmp_w1'][0], f)
    b1 = np.asarray(inp['cmp_b1'][0], f)
    m['b1T'] = np.ascontiguousarray(np.transpose(b1.reshape(2, 2, 128), (2, 0, 1)))
    w2 = np.asarray(inp['cmp_w2'][0], f)
    m['w2r'] = np.ascontiguousarray(np.transpose(w2.reshape(2, 2, 128, 64), (2, 0, 1, 3)))
    m['w_up_pool'] = np.ascontiguousarray(inp['w_up_pool'][0], f)
    m['w_up_attn'] = np.ascontiguousarray(inp['w_up_attn'][0], f)
    m['w_out'] = np.ascontiguousarray(inp['w_out'][0], f)
    rwf = np.concatenate([np.asarray(inp['router_g_w'][0], f), np.asarray(inp['router_e_w'][0], f)], 1)
    m['rw'] = np.ascontiguousarray(np.transpose(rwf.reshape(8, 128, 36), (1, 0, 2)))
    rb = np.concatenate([np.asarray(inp['router_g_b'][0], f), np.asarray(inp['router_e_b'][0], f)], 0)
    m['rb_bc'] = np.ascontiguousarray(np.broadcast_to(rb[None], (128, 36)), f)
    m['exp_w_gate'] = np.ascontiguousarray(inp['exp_w_gate'][0], f)
    m['exp_w_up'] = np.ascontiguousarray(inp['exp_w_up'][0], f)
    m['exp_w_down'] = np.ascontiguousarray(inp['exp_w_down'][0], f)
    return m


def prep_core(inp, shared, b0, nseq):
    f = np.float32
    m = dict(shared)
    m['x'] = np.ascontiguousarray(np.asarray(inp['x'][b0:b0 + nseq], f).reshape(nseq * S, D))
    c = np.asarray(inp['c'][b0:b0 + nseq], f)
    ck = np.transpose(c.reshape(nseq, 8, 128), (2, 1, 0))
    m['cT'] = np.ascontiguousarray(ck)
    m['crep'] = np.ascontiguousarray(np.broadcast_to(np.transpose(c.reshape(nseq, 8, 128), (0, 2, 1))[:, :, :, None],
                                                     (nseq, 128, 8, 128)), f)
    return m


_NC_CACHE = {}


def kernel(**inputs):
    nseq = 32 // NCORES
    if nseq not in _NC_CACHE:
        _NC_CACHE[nseq] = build(nseq)
    nc = _NC_CACHE[nseq]
    shared = prep_shared(inputs)
    in_maps = [prep_core(inputs, shared, core * nseq, nseq) for core in range(NCORES)]
    res = run_bass_kernel_spmd(nc, in_maps, core_ids=list(range(NCORES)))
    out = np.concatenate([np.asarray(r["y"], np.float32).reshape(nseq, S, D) for r in res.results], axis=0)
    return out
```

```python
import numpy as np
from contextlib import ExitStack
import concourse.bass as bass
import concourse.mybir as mybir
from concourse.bass_utils import run_bass_kernel_spmd

F32 = mybir.dt.float32
BF16 = mybir.dt.bfloat16
AF = mybir.ActivationFunctionType
ALU = mybir.AluOpType
AX = mybir.AxisListType

D = 1024
S = 2048
NCORES = 8
NR = 4
NEXP = 32
EPS = 1e-6
CUT = 100


class Prog:
    ENGS = ['pe', 'act', 'dve', 'pool', 'sp']

    def __init__(self, nc, es):
        self.nc = nc
        self.es = es
        self.plan = False
        self.ops = {e: [] for e in self.ENGS}
        self.sems = {}
        self.semcount = {}
        self.known = {e: {} for e in self.ENGS}
        self.lastw = {}
        self.readers = {}
        self.nbank = 0
        for e in ['pe', 'act', 'dve', 'pool']:
            self.sem('c_' + e)

    def sem(self, name):
        if name not in self.sems:
            self.sems[name] = self.es.enter_context(self.nc.semaphore(name))
            self.semcount[name] = 0
        return name

    @staticmethod
    def _key(r):
        if isinstance(r, (str, tuple)):
            return r
        t = getattr(r, 'tensor', r)
        return t.name

    def emit(self, eng, fn, reads=(), writes=(), sem=None):
        if self.plan:
            return
        is_dma = sem is not None
        if not is_dma:
            sem = 'c_' + eng
            inc = 1
        else:
            self.sem(sem)
            inc = 16
        waits = {}

        def need(dep, raw):
            s, v, e, d = dep
            if (not d) and e == eng and eng == 'pe':
                return
            if d and is_dma and (not raw) and s == sem:
                return
            if self.known[eng].get(s, 0) >= v:
                return
            waits[s] = max(waits.get(s, 0), v)

        for r in reads:
            k = self._key(r)
            if k in self.lastw:
                need(self.lastw[k], True)
        for w in writes:
            k = self._key(w)
            if k in self.lastw:
                need(self.lastw[k], False)
            for dep in self.readers.get(k, {}).values():
                need(dep, False)
        for s, v in waits.items():
            self.known[eng][s] = v
        self.semcount[sem] += inc
        tick = self.semcount[sem]
        me = (sem, tick, eng, is_dma)
        self.ops[eng].append((list(waits.items()), fn, sem, inc))
        for r in reads:
            k = self._key(r)
            self.readers.setdefault(k, {})[(eng, sem)] = me
        for w in writes:
            k = self._key(w)
            self.lastw[k] = me
            self.readers[k] = {}

    def barrier(self):
        if self.plan:
            return
        for eng in self.ENGS:
            waits = []
            for s, c in self.semcount.items():
                if c > 0 and self.known[eng].get(s, 0) < c:
                    waits.append((s, c))
                    self.known[eng][s] = c
            if waits:
                self.ops[eng].append((waits, None, None, 0))

    def run(self):
        nc = self.nc
        P = self

        def replay(e, eng):
            for (waits, fn, sem, inc) in P.ops[e]:
                for (s, v) in waits:
                    eng.wait_ge(P.sems[s], v)
                if fn is not None:
                    ins = fn(eng)
                    ins.then_inc(P.sems[sem], inc)

        with nc.Block() as block:
            @block.tensor
            def _(eng):
                replay('pe', eng)

            @block.scalar
            def _(eng):
                replay('act', eng)

            @block.vector
            def _(eng):
                replay('dve', eng)

            @block.gpsimd
            def _(eng):
                replay('pool', eng)

            @block.sync
            def _(eng):
                replay('sp', eng)


class Ring:
    def __init__(self, P, slots):
        self.P = P
        self.slots = slots
        self.seq = []
        self.reset()

    def reset(self):
        self.idx = 0
        self.loaded = 0
        self.dead = set()

    def get(self, loader):
        i = self.idx
        self.idx += 1
        if self.P.plan:
            self.seq.append(loader)
            return i, self.slots[i % NR]
        self.top_up()
        assert self.loaded > i, ("ring too many live chunks", i)
        return i, self.slots[i % NR]

    def done(self, i):
        if self.P.plan:
            return
        self.dead.add(i)
        self.top_up()

    def top_up(self):
        while self.loaded < len(self.seq) and (self.loaded < NR or (self.loaded - NR) in self.dead):
            c = self.loaded
            self.seq[c](self.slots[c % NR], c % NR)
            self.loaded += 1


def build(nseq, dbg=False):
    nc = bass.Bass("TRN2", target_bir_lowering=False)
    ntok = nseq * S

    def din(name, shape, dt=F32):
        return nc.dram_tensor(name, list(shape), dt, kind="ExternalInput").ap()

    x_d = din("x", [ntok, D])
    crep_d = din("crep", [nseq, 128, 8, 128])
    cT_d = din("cT", [128, 8, nseq])
    adaw_d = din("ada_w", [D, 6 * D])
    adabT_d = din("adabT", [128, 48])
    adabbc_d = din("adab_bc", [128, 2, D])
    n1g_d = din("n1g", [128, 8])
    n2g_d = din("n2g", [128, 8])
    fgbc_d = din("fg_bc", [128, D])
    win_d = din("w_in_p", [D, 3864])
    poolw_d = din("pool_w_r", [128, 4, 128])
    psc_d = din("pscT", [128, 4])
    posT_d = din("posT", [128, 2, 32])
    w1_d = din("cmp_w1", [2, 2048, 256])
    b1T_d = din("b1T", [128, 2, 2])
    w2r_d = din("w2r", [128, 2, 2, 64])
    wup_d = din("w_up_pool", [512, D])
    wua_d = din("w_up_attn", [512, D])
    wout_d = din("w_out", [D, D])
    rw_d = din("rw", [128, 8, 36])
    rbbc_d = din("rb_bc", [128, 36])
    eg_d = din("exp_w_gate", [NEXP, D, 512])
    eu_d = din("exp_w_up", [NEXP, D, 512])
    ed_d = din("exp_w_down", [NEXP, 512, D])
    identf_d = din("identf", [128, 128])
    maskD_d = din("maskD4", [128, 512])
    maskW_d = din("maskW4", [128, 512])
    maskC_d = din("maskC", [128, 16, 128])
    eall_d = din("Eall", [32, 2048])
    ptcur_d = din("ptcur", [128, 4, 128])
    ptfirst_d = din("ptfirst", [128, 4, 128])
    ptprev_d = din("ptprev", [128, 4, 128])
    ovl_d = din("ovl1", [128, 33])
    selv_d = din("selvalid", [128, 16, 32])
    selb_d = din("selbias", [128, 16, 32])
    y_d = nc.dram_tensor("y", [ntok, D], F32, kind="ExternalOutput").ap()
    if dbg:
        dbg_d = nc.dram_tensor("dbg_x1", [ntok, D], F32, kind="ExternalOutput").ap()

    with ExitStack() as es:
        P = Prog(nc, es)

        uniq = [0]

        def sb(name, shape, dt, stack=es):
            uniq[0] += 1
            return stack.enter_context(nc.sbuf_tensor(f"{name}_u{uniq[0]}", list(shape), dt))

        identb = sb("identb", [128, 128], BF16)
        identf = sb("identf_s", [128, 128], F32)
        maskD4 = sb("maskD4_s", [128, 512], BF16)
        maskW4 = sb("maskW4_s", [128, 512], BF16)
        maskC = sb("maskC_s", [128, 16, 128], BF16)
        Eall = sb("Eall_s", [128, 2048], BF16)
        ptcur = sb("ptcur_s", [128, 4, 128], BF16)
        ptfirst = sb("ptfirst_s", [128, 4, 128], BF16)
        ptprev = sb("ptprev_s", [128, 4, 128], BF16)
        selvalid = sb("selvalid_s", [128, 16, 32], F32)
        selbiasc = sb("selbias_s", [128, 16, 32], F32)
        modT = sb("modT", [128, 32, nseq], F32)
        s1T = sb("s1T", [128, 8, nseq], F32)
        s2T = sb("s2T", [128, 8, nseq], F32)
        adabT = sb("adabT_s", [128, 48], F32)
        n1g = sb("n1g_s", [128, 8], F32)
        n2g = sb("n2g_s", [128, 8], F32)
        fg_bc = sb("fg_bc_s", [128, D], F32)
        g1bc = sb("g1bc", [128, D], F32)
        g2bc = sb("g2bc", [128, D], F32)
        poolw = sb("poolw_s", [128, 4, 128], BF16)
        pscT = sb("pscT_s", [128, 4], F32)
        posT = sb("posT_s", [128, 2, 32], BF16)
        b1T = sb("b1T_s", [128, 2, 2], F32)
        biasc = sb("biasc", [128, 2, 2], F32)
        w2sb = sb("w2sb", [128, 2, 2, 64], BF16)
        rw = sb("rw_s", [128, 8, 36], F32)
        rb_bc = sb("rb_bc_s", [128, 36], F32)
        crep = sb("crep_s", [128, 8, 128], BF16)
        cT = sb("cT_s", [128, 8, nseq], BF16)
        acc = sb("acc", [128, 8, D], F32)
        comb = sb("comb", [128, 8, 32], F32)
        kcT = sb("kcT", [128, S], BF16)
        vcT = sb("vcT", [128, S], BF16)
        ksT = sb("ksT", [128, S], BF16)
        kwT = sb("kwT", [128, S], BF16)
        kcmpT = sb("kcmpT", [128, 128], BF16)
        vcaug = sb("vcaug", [128, 2, 97], BF16)
        hidv = sb("hidv", [128, 2, 2, 128], BF16)
        vs_aug = sb("vs_aug", [128, 16, 2, 65], BF16)
        vw_aug = sb("vw_aug", [128, 16, 2, 65], BF16)
        bg = sb("bg", [128, 16, 24], F32)
        ucarry = sb("ucarry", [128, 512], BF16)
        ring = [sb(f"ring{i}", [128, 4096], BF16) for i in range(NR)]
        R = Ring(P, ring)

        banks = [es.enter_context(nc.psum_tensor(f"bank{i}", [128, 512], F32)) for i in range(6)]
        ACCA = es.enter_context(nc.psum_tensor("ACCA", [128, 512], F32))
        ACCB = es.enter_context(nc.psum_tensor("ACCB", [128, 512], F32))

        def PS():
            b = banks[P.nbank % 6]
            P.nbank += 1
            return b

        def MM(out, lhsT, rhs, start=True, stop=True, rd=(), wr=()):
            P.emit('pe', lambda e: e.matmul(out, lhsT, rhs, start=start, stop=stop, skip_group_check=True), rd, wr)

        def TR(out, in_, ident, rd=(), wr=()):
            P.emit('pe', lambda e: e.transpose(out, in_, ident), rd, wr)

        def ACT(out, in_, func, rd=(), wr=(), **kw):
            P.emit('act', lambda e: e.activation(out=out, in_=in_, func=func, **kw), rd, wr)

        def ACOPY(out, in_, rd=(), wr=()):
            P.emit('act', lambda e: e.copy(out=out, in_=in_), rd, wr)

        def TS(out, in0, s1, s2, op0, op1=None, rd=(), wr=(), eng='dve'):
            if op1 is None:
                P.emit(eng, lambda e: e.tensor_scalar(out=out, in0=in0, scalar1=s1, scalar2=None, op0=op0), rd, wr)
            else:
                P.emit(eng, lambda e: e.tensor_scalar(out=out, in0=in0, scalar1=s1, scalar2=s2, op0=op0, op1=op1), rd, wr)

        def TT(out, in0, in1, op, rd=(), wr=(), eng='dve'):
            P.emit(eng, lambda e: e.tensor_tensor(out=out, in0=in0, in1=in1, op=op), rd, wr)

        def STT(out, in0, scalar, in1, op0, op1, rd=(), wr=()):
            P.emit('dve', lambda e: e.scalar_tensor_tensor(out=out, in0=in0, scalar=scalar, in1=in1, op0=op0, op1=op1), rd, wr)

        def CP(out, in_, rd=(), wr=(), eng='dve'):
            P.emit(eng, lambda e: e.tensor_copy(out=out, in_=in_), rd, wr)

        def DMA(eng, out, in_, sem, rd=(), wr=()):
            P.emit(eng, lambda e: e.dma_start(out=out, in_=in_), rd, wr, sem=sem)

        def wchunk(src2d, ncols, nk=8):
            def loader(slot, si):
                dst = slot[:, 0:nk * ncols].rearrange("p (k n) -> p k n", k=nk)
                DMA('pool', dst, src2d.rearrange("(k p) n -> p k n", p=128), f'ring{si}', (), [slot])
            return loader

        def w1chunk(kv, half):
            def loader(slot, si):
                src = w1_d[kv].rearrange("(l d) c -> d l c", d=64)[:, 16 * half:16 * half + 16, :]
                for hp in range(2):
                    dst = slot[64 * hp:64 * hp + 64, :].rearrange("p (l c) -> p l c", l=16)
                    DMA('pool', dst, src, f'ring{si}', (), [slot])
            return loader

        def V3(ap2d, a):
            return ap2d.rearrange("p (a b) -> p a b", a=a)

        def gen():
            P.nbank = 0
            R.reset()
            for (dst, src) in [(identf, identf_d), (selvalid, selv_d), (selbiasc, selb_d), (adabT, adabT_d),
                               (n1g, n1g_d), (n2g, n2g_d), (fg_bc, fgbc_d), (pscT, psc_d), (b1T, b1T_d),
                               (rw, rw_d), (rb_bc, rbbc_d)]:
                DMA('sp', dst[:], src, 'cst_sp', (), [dst])
            P.emit('dve', lambda e: e.memset(Eall[:], 0.0), (), [Eall])
            DMA('pool', Eall[0:32, :], eall_d, 'cst_pool', (), [Eall])
            DMA('pool', Eall[64:96, :], eall_d, 'cst_pool', (), [Eall])
            for (dst, src) in [(maskD4, maskD_d), (maskW4, maskW_d), (maskC, maskC_d),
                               (ptcur, ptcur_d), (ptfirst, ptfirst_d), (ptprev, ptprev_d),
                               (poolw, poolw_d), (posT, posT_d), (w2sb, w2r_d), (cT, cT_d)]:
                DMA('pool', dst[:], src, 'cst_pool', (), [dst])
            DMA('pool', identb[:], identf_d, 'cst_pool', (), [identb])
            for g in range(2):
                DMA('pool', vcaug[:, g, 64:97], ovl_d, 'cst_pool', (), [vcaug])
            P.emit('dve', lambda e: e.memset(hidv[:], 0.0), (), [hidv])
            P.emit('dve', lambda e: e.memset(kcmpT[:], 0.0), (), [kcmpT])
            P.emit('dve', lambda e: e.memset(vs_aug[:], 1.0), (), [vs_aug])
            P.emit('dve', lambda e: e.memset(vw_aug[:], 1.0), (), [vw_aug])
            P.emit('dve', lambda e: e.memset(ucarry[:], 0.0), (), [ucarry])
            for g in range(2):
                P.emit('dve', lambda e, g=g: e.memset(vcaug[:, g, 0:64], 0.0), (), [vcaug])
            P.barrier()

            for mi, c0 in enumerate([0, 1024, 3072, 4096]):
                for hf in range(2):
                    ci, w = R.get(wchunk(adaw_d[:, c0 + 512 * hf:c0 + 512 * hf + 512], 512))
                    wv = V3(w[:, :], 8)
                    bk = PS()
                    for fc in range(4):
                        for k in range(8):
                            MM(bk[:, fc * nseq:(fc + 1) * nseq], wv[:, k, fc * 128:(fc + 1) * 128], cT[:, k, :],
                               start=(k == 0), stop=(k == 7), rd=[w, cT], wr=[bk])
                    R.done(ci)
                    j0 = mi * 8 + hf * 4
                    cb = (c0 // 128) + hf * 4
                    TT(modT[:, j0:j0 + 4, :], V3(bk[:, 0:4 * nseq], 4),
                       adabT[:, cb:cb + 4].unsqueeze(2).broadcast_to([128, 4, nseq]), ALU.add, rd=[bk, adabT], wr=[modT])
            TS(s1T[:], modT[:, 8:16, :], 1.0, None, ALU.add, rd=[modT], wr=[s1T])
            TT(s1T[:], s1T[:], n1g[:].unsqueeze(2).broadcast_to([128, 8, nseq]), ALU.mult, rd=[s1T, n1g], wr=[s1T])
            TS(s2T[:], modT[:, 24:32, :], 1.0, None, ALU.add, rd=[modT], wr=[s2T])
            TT(s2T[:], s2T[:], n2g[:].unsqueeze(2).broadcast_to([128, 8, nseq]), ALU.mult, rd=[s2T, n2g], wr=[s2T])

            for kv in range(2):
                ca, wa = R.get(w1chunk(kv, 0))
                cb_, wb = R.get(w1chunk(kv, 1))
                for cc in range(2):
                    bk = PS()
                    for l in range(32):
                        w = wa if l < 16 else wb
                        wv = V3(w[:, :], 16)
                        MM(bk[:, 0:1], wv[0:64, l % 16, cc * 128:(cc + 1) * 128], posT[0:64, kv, l:l + 1],
                           start=(l == 0), stop=(l == 31), rd=[w, posT], wr=[bk])
                    TT(biasc[:, kv, cc:cc + 1], bk[:, 0:1], b1T[:, kv, cc:cc + 1], ALU.add, rd=[bk, b1T], wr=[biasc])
                R.done(ca)
                R.done(cb_)

            for b in range(nseq):
                seq_prologue(b)
                for grp in range(2):
                    with ExitStack() as ph:
                        T = alloc_mixer(ph, f"{b}_{grp}")
                        for sth in range(2):
                            mixer_supertile(T, b, grp * 2 + sth)
                    P.barrier()
                    if dbg:
                        for j in range(8):
                            r0 = b * S + grp * 1024 + j * 128
                            DMA('sp', dbg_d[r0:r0 + 128, :], acc[:, j, :], f'dbgst', [('acc', j)], ['dbgout'])
                        P.barrier()
                    with ExitStack() as ph:
                        T = alloc_moe(ph, f"{b}_{grp}")
                        moe_group(T, b, grp)
                    P.barrier()
            P.barrier()

        def seq_prologue(b):
            DMA('pool', crep[:], crep_d[b], 'crep', (), [crep])
            DMA('sp', g1bc[:], adabbc_d[:, 0, :], 'g1l', (), [g1bc])
            DMA('sp', g2bc[:], adabbc_d[:, 1, :], 'g2l', (), [g2bc])
            for (dst, c0) in [(g1bc, 2048), (g2bc, 5120)]:
                for hf in range(2):
                    ci, w = R.get(wchunk(adaw_d[:, c0 + 512 * hf:c0 + 512 * hf + 512], 512))
                    wv = V3(w[:, :], 8)
                    bk = PS()
                    for k in range(8):
                        MM(bk[:, :], crep[:, k, :], wv[:, k, :], start=(k == 0), stop=(k == 7), rd=[w, crep], wr=[bk])
                    R.done(ci)
                    TT(dst[:, hf * 512:(hf + 1) * 512], bk[:, :], dst[:, hf * 512:(hf + 1) * 512], ALU.add,
                       rd=[bk, dst], wr=[dst])

        def alloc_mixer(ph, tag):
            T = {}

            def a(name, shape, dt):
                T[name] = sb(f"{name}_{tag}", shape, dt, ph)
            a('xn0', [128, D], BF16)
            a('xn1', [128, D], BF16)
            a('ss', [128, 4], F32)
            a('lnv', [128, 4], F32)
            a('rstd', [128, 4], F32)
            a('h1T', [128, 8, 512], BF16)
            a('qz0', [128, 4, 512], BF16)
            a('qz1', [128, 4, 512], BF16)
            for pp in range(2):
                for g in range(2):
                    a(f'selbT{pp}{g}', [128, 4, 128], BF16)
            a('u_tm', [128, 4, 512], BF16)
            a('pooledT', [128, 4, 512], BF16)
            a('ypoolT', [128, 4, 512], BF16)
            a('hidk', [128, 2, 32], BF16)
            for i in range(6):
                a(f'pe{i}', [128, 512], BF16)
            for g in range(2):
                a(f'pc{g}', [128, 512], BF16)
            a('ocrp0', [128, 2, 4, 97], F32)
            a('ocrp1', [128, 2, 4, 97], F32)
            a('osel', [128, 2, 4, 65], F32)
            a('owin', [128, 2, 4, 65], F32)
            a('rs', [128, 2, 4], F32)
            a('tmp4', [128, 2, 4, 32], F32)
            a('psc', [128, 2, 32], F32)
            a('score', [128, 2, 32], F32)
            a('m8', [128, 2, 8], F32)
            a('selb', [128, 2, 32], BF16)
            a('den', [128, 3, 2, 4], F32)
            a('coef', [128, 3, 2, 4], F32)
            a('o1', [128, 2, 4, 64], F32)
            a('o2', [128, 2, 4, 64], F32)
            a('oattn0', [128, 512], BF16)
            a('oattn1', [128, 512], BF16)
            a('oattnT', [128, 4, 512], BF16)
            a('sig', [128, 512], F32)
            a('mixT', [128, 8, 512], BF16)
            a('tmp', [128, 512], F32)
            return T

        def mixer_supertile(T, b, st):
            h1T, u_tm = T['h1T'], T['u_tm']
            qz = [T['qz0'], T['qz1']]
            if 'zeroed' not in T:
                T['zeroed'] = True
                P.emit('dve', lambda e: e.memset(T['qz0'][:], 0.0), (), [T['qz0']])
                P.emit('dve', lambda e: e.memset(T['qz1'][:], 0.0), (), [T['qz1']])
                for nm in ['selbT00', 'selbT01', 'selbT10', 'selbT11']:
                    P.emit('dve', lambda e, nm=nm: e.memset(T[nm][:], 0.0), (), [T[nm]])
            tok0 = st * 512
            jb = (st % 2) * 4
            for i in range(4):
                r0 = b * S + tok0 + i * 128
                DMA('sp', acc[:, jb + i, :], x_d[r0:r0 + 128, :], f'xl{jb + i}', (), [('acc', jb + i)])
            ss, lnv, rstd = T['ss'], T['lnv'], T['rstd']
            for i in range(4):
                ACT(T['xn0'][:], acc[:, jb + i, :], AF.Square, rd=[('acc', jb + i)], wr=[T['xn0'], (ss.name, i)],
                    accum_out=ss[:, i:i + 1])
            ACT(lnv[:], ss[:], AF.Ln, rd=[(ss.name, i) for i in range(4)], wr=[lnv], scale=1.0 / D, bias=EPS)
            ACT(rstd[:], lnv[:], AF.Exp, rd=[lnv], wr=[rstd], scale=-0.5)
            tb = [PS() for _ in range(4)]
            tbv = [t[:, :].bitcast(BF16) for t in tb]
            for i in range(4):
                xn = T[f'xn{i % 2}']
                TS(xn[:], acc[:, jb + i, :], rstd[:, i:i + 1], None, ALU.mult, rd=[('acc', jb + i), rstd], wr=[xn])
                for c in range(8):
                    o = tbv[c // 2][:, (c % 2) * 512 + i * 128:(c % 2) * 512 + (i + 1) * 128]
                    TR(o, xn[:, c * 128:(c + 1) * 128], identb[:], rd=[xn], wr=[tb[c // 2]])
            for c in range(8):
                TS(h1T[:, c, :], tbv[c // 2][:, (c % 2) * 512:(c % 2 + 1) * 512], s1T[:, c, b:b + 1], modT[:, c, b:b + 1],
                   ALU.mult, ALU.add, rd=[tb[c // 2], s1T, modT], wr=[h1T])
            if CUT < 10:
                return
            ci, w = R.get(wchunk(win_d[:, 0:512], 512))
            wv = V3(w[:, :], 8)
            for i in range(4):
                bk = PS()
                for k in range(8):
                    MM(bk[:, :], h1T[:, k, i * 128:(i + 1) * 128], wv[:, k, :], start=(k == 0), stop=(k == 7), rd=[h1T, w], wr=[bk])
                ACOPY(u_tm[:, i, :], bk[:, :], rd=[bk], wr=[u_tm])
            R.done(ci)
            ci, w = R.get(wchunk(win_d[:, 512:792], 280))
            wv = w[:, 0:8 * 280].rearrange("p (k n) -> p k n", k=8)
            for i in range(4):
                tt = st * 4 + i
                bk = PS()
                for k in range(8):
                    MM(bk[:, 0:280], h1T[:, k, i * 128:(i + 1) * 128], wv[:, k, :], start=(k == 0), stop=(k == 7), rd=[h1T, w], wr=[bk])
                ACOPY(vs_aug[:, tt, :, 0:64], V3(bk[:, 0:128], 2), rd=[bk], wr=[vs_aug])
                ACOPY(vw_aug[:, tt, :, 0:64], V3(bk[:, 128:256], 2), rd=[bk], wr=[vw_aug])
                ACT(bg[:, tt, :], bk[:, 256:280], AF.Sigmoid, rd=[bk], wr=[bg])
            R.done(ci)
            ci, w = R.get(wchunk(win_d[:, 792:1304], 512))
            wv = V3(w[:, :], 8)
            for hh in range(4):
                bk = PS()
                for k in range(8):
                    MM(bk[:, :], wv[:, k, hh * 128:(hh + 1) * 128], h1T[:, k, :], start=(k == 0), stop=(k == 7), rd=[h1T, w], wr=[bk])
                if hh % 2 == 0:
                    ACOPY(qz[0][0:64, hh, :], bk[0:64, :], rd=[bk], wr=[qz[0]])
                    ACOPY(qz[1][64:128, hh, :], bk[64:128, :], rd=[bk], wr=[qz[1]])
                else:
                    CP(qz[0][0:64, hh, :], bk[0:64, :], rd=[bk], wr=[qz[0]])
                    CP(qz[1][64:128, hh, :], bk[64:128, :], rd=[bk], wr=[qz[1]])
            R.done(ci)
            ci, w = R.get(wchunk(win_d[:, 1304:1816], 512))
            wv = V3(w[:, :], 8)
            for n, dst in enumerate([kcT, vcT, ksT, kwT]):
                bk = PS()
                for k in range(8):
                    MM(bk[:, :], wv[:, k, n * 128:(n + 1) * 128], h1T[:, k, :], start=(k == 0), stop=(k == 7), rd=[h1T, w], wr=[bk])
                if n % 2 == 0:
                    ACOPY(dst[:, tok0:tok0 + 512], bk[:, :], rd=[bk], wr=[dst])
                else:
                    CP(dst[:, tok0:tok0 + 512], bk[:, :], rd=[bk], wr=[dst])
            R.done(ci)
            if CUT < 20:
                return
            pooledT, ypoolT = T['pooledT'], T['ypoolT']
            for g in range(4):
                bk = PS()
                gs = slice(g * 128, (g + 1) * 128)
                for i in range(4):
                    tt = st * 4 + i
                    o = bk[:, i * 128:(i + 1) * 128]
                    if tt == 0:
                        MM(o, u_tm[:, 0, gs], ptfirst[:, g, :], rd=[u_tm, ptfirst], wr=[bk])
                    else:
                        MM(o, u_tm[:, i, gs], ptcur[:, g, :], start=True, stop=False, rd=[u_tm, ptcur], wr=[bk])
                        if i > 0:
                            MM(o, u_tm[64:128, i - 1, gs], ptprev[64:128, g, :], start=False, stop=True, rd=[u_tm, ptprev], wr=[bk])
                        else:
                            MM(o, ucarry[64:128, gs], ptprev[64:128, g, :], start=False, stop=True, rd=[ucarry, ptprev], wr=[bk])
                ACOPY(pooledT[:, g, :], bk[:, :], rd=[bk], wr=[pooledT])
                bk2 = PS()
                MM(bk2[:, :], poolw[:, g, :], pooledT[:, g, :], rd=[poolw, pooledT], wr=[bk2])
                TS(ypoolT[:, g, :], bk2[:, :], pscT[:, g:g + 1], None, ALU.mult, rd=[bk2, pscT], wr=[ypoolT])
            CP(ucarry[:], u_tm[:, 3, :], rd=[u_tm], wr=[ucarry])
            if CUT < 30:
                return
            i0 = max(0, 32 * st - 1)
            i1 = 32 * (st + 1) - 1
            nb = i1 - i0
            hidk = T['hidk']
            for kv in range(2):
                src = kcT if kv == 0 else vcT
                ca, wa = R.get(w1chunk(kv, 0))
                cb_, wb = R.get(w1chunk(kv, 1))
                for g in range(2):
                    for cc in range(2):
                        bk = PS()
                        for l in range(32):
                            w = wa if l < 16 else wb
                            wv = V3(w[:, :], 16)
                            t0 = 16 * i0 + l
                            MM(bk[:, 0:nb], wv[64 * g:64 * g + 64, l % 16, cc * 128:(cc + 1) * 128],
                               src[64 * g:64 * g + 64, t0:t0 + 16 * (nb - 1) + 1:16],
                               start=(l == 0), stop=(l == 31), rd=[w, src], wr=[bk])
                        if kv == 0:
                            ACT(hidk[:, cc, 0:nb], bk[:, 0:nb], AF.Gelu_apprx_tanh, rd=[bk, biasc], wr=[hidk],
                                bias=biasc[:, kv, cc:cc + 1])
                        else:
                            ACT(hidv[:, g, cc, i0:i1], bk[:, 0:nb], AF.Gelu_apprx_tanh, rd=[bk, biasc], wr=[hidv],
                                bias=biasc[:, kv, cc:cc + 1])
                    bk = PS()
                    if kv == 0:
                        for cc in range(2):
                            MM(bk[64 * g:64 * g + 64, 0:nb], w2sb[:, 0, cc, :], hidk[:, cc, 0:nb],
                               start=(cc == 0), stop=(cc == 1), rd=[w2sb, hidk], wr=[bk])
                        CP(kcmpT[64 * g:64 * g + 64, i0:i1], bk[64 * g:64 * g + 64, 0:nb], rd=[bk], wr=[kcmpT])
                    else:
                        for cc in range(2):
                            MM(bk[:, 0:64], hidv[:, g, cc, :], w2sb[:, 1, cc, :],
                               start=(cc == 0), stop=(cc == 1), rd=[w2sb, hidv], wr=[bk])
                        CP(vcaug[:, g, 0:64], bk[:, 0:64], rd=[bk], wr=[vcaug])
                R.done(ca)
                R.done(cb_)
            if CUT < 40:
                return
            oattnT = T['oattnT']
            npe = [0]

            def next_pe():
                t = T[f'pe{npe[0] % 6}']
                npe[0] += 1
                return t

            def rq_(g, i):
                return qz[g][:, :, i * 128:(i + 1) * 128]

            def attn_cmp(i, qi, g, par):
                ocr = T[f'ocrp{par}']
                pc, pcm = T[f'pc{g}'], T[f'pc{g}']
                bk = PS()
                MM(V3(bk[:, :], 4), kcmpT[:, :], rq_(g, i), rd=[kcmpT, qz[g]], wr=[bk])
                ACT(pc[:], bk[:, :], AF.Exp, rd=[bk], wr=[pc], scale=0.125)
                TT(V3(pcm[:, :], 4), V3(pc[:, :], 4), maskC[:, qi, :].unsqueeze(1).broadcast_to([128, 4, 128]), ALU.mult,
                   rd=[pc, maskC], wr=[pcm])
                bk2 = PS()
                for hh in range(4):
                    MM(bk2[:, hh * 97:(hh + 1) * 97], pcm[:, hh * 128:(hh + 1) * 128], vcaug[:, g, :], rd=[pcm, vcaug], wr=[bk2])
                ACOPY(ocr[:, g].rearrange("p a b -> p (a b)"), bk2[:, 0:388], rd=[bk2], wr=[ocr])

            def attn_select(i, qi, par):
                ocr = T[f'ocrp{par}']
                rs, tmp4, psc, score, m8, selb = T['rs'], T['tmp4'], T['psc'], T['score'], T['m8'], T['selb']
                TS(rs[:], ocr[:, :, :, 64], 1e-30, None, ALU.max, rd=[ocr], wr=[rs])
                P.emit('dve', lambda e: e.reciprocal(out=rs[:], in_=rs[:]), [rs], [rs])
                TT(tmp4[:], ocr[:, :, :, 65:97], rs[:].unsqueeze(3).broadcast_to([128, 2, 4, 32]), ALU.mult, rd=[ocr, rs], wr=[tmp4])
                P.emit('dve', lambda e: e.tensor_reduce(out=psc[:], in_=tmp4[:].rearrange("p g h j -> p g j h"), axis=AX.X, op=ALU.add),
                       [tmp4], [psc])
                TT(score[:], psc[:], selvalid[:, qi, :].unsqueeze(1).broadcast_to([128, 2, 32]), ALU.mult, rd=[psc, selvalid], wr=[score])
                TT(score[:], score[:], selbiasc[:, qi, :].unsqueeze(1).broadcast_to([128, 2, 32]), ALU.add, rd=[score, selbiasc], wr=[score])
                for g in range(2):
                    P.emit('dve', lambda e, g=g: e.max(out=m8[:, g, :], in_=score[:, g, :]), [score], [m8])
                TT(score[:], score[:], m8[:, :, 7].unsqueeze(2).broadcast_to([128, 2, 32]), ALU.is_ge, rd=[score, m8], wr=[score])
                TS(selb[:], score[:], 1.0, 30000.0, ALU.subtract, ALU.mult, rd=[score], wr=[selb])
                bkt = PS()
                bktv = bkt[:, :].bitcast(BF16)
                for g in range(2):
                    sp_ = slice(64 * g, 64 * g + 32)
                    TR(bktv[sp_, 0:128], selb[:, g, :], identb[:], rd=[selb], wr=[bkt])
                for g in range(2):
                    sp_ = slice(64 * g, 64 * g + 32)
                    selbT = T[f'selbT{par}{g}']
                    CP(selbT[sp_], bktv[sp_, 0:128].unsqueeze(1).broadcast_to([32, 4, 128]), rd=[bkt], wr=[selbT])

            def attn_combine(i, qi, par, oattn):
                ocr, osel, owin = T[f'ocrp{par}'], T['osel'], T['owin']
                den, coef, o1, o2 = T['den'], T['coef'], T['o1'], T['o2']
                CP(den[:, 0], ocr[:, :, :, 64], rd=[ocr], wr=[den])
                CP(den[:, 1], osel[:, :, :, 64], rd=[osel], wr=[den])
                CP(den[:, 2], owin[:, :, :, 64], rd=[owin], wr=[den])
                TS(den[:], den[:], 1e-30, None, ALU.max, rd=[den], wr=[den])
                P.emit('dve', lambda e: e.reciprocal(out=den[:], in_=den[:]), [den], [den])
                gview = bg[:, qi, :].rearrange("p (r g h) -> p r g h", r=3, g=2)
                TT(coef[:], den[:], gview, ALU.mult, rd=[den, bg], wr=[coef])

                def bc(r):
                    return coef[:, r].unsqueeze(3).broadcast_to([128, 2, 4, 64])
                TT(o1[:], ocr[:, :, :, 0:64], bc(0), ALU.mult, rd=[ocr, coef], wr=[o1])
                TT(o2[:], osel[:, :, :, 0:64], bc(1), ALU.mult, rd=[osel, coef], wr=[o2])
                TT(o1[:], o1[:], o2[:], ALU.add, rd=[o1, o2], wr=[o1])
                TT(o2[:], owin[:, :, :, 0:64], bc(2), ALU.mult, rd=[owin, coef], wr=[o2])
                TT(oattn[:, :].rearrange("p (g h d) -> p g h d", g=2, h=4), o1[:], o2[:], ALU.add, rd=[o1, o2], wr=[oattn])

            def attn_transposes(i, oattn):
                bkt = PS()
                bktv = bkt[:, :].bitcast(BF16)
                for c in range(4):
                    TR(bktv[:, c * 128:(c + 1) * 128], oattn[:, c * 128:(c + 1) * 128], identb[:], rd=[oattn], wr=[bkt])
                CP(oattnT[:, :, i * 128:(i + 1) * 128], V3(bktv[:, 0:512], 4), rd=[bkt], wr=[oattnT])

            def attn_prep(i):
                qi_ = st * 4 + i
                for g in range(2):
                    attn_cmp(i, qi_, g, i % 2)
                attn_select(i, qi_, i % 2)

            pending_tr = None
            attn_prep(0)
            for i in range(4):
                qi = st * 4 + i
                par = i % 2
                oattn = T[f'oattn{i % 2}']
                if i + 1 < 4:
                    attn_prep(i + 1)
                if pending_tr is not None:
                    attn_transposes(*pending_tr)
                    pending_tr = None
                tasks = []
                k0 = max(0, qi - 4)
                for g in range(2):
                    for kj in range(k0, qi + 1):
                        tasks.append(('win', g, kj, kj == k0, kj == qi))
                    for kj in range(qi + 1):
                        tasks.append(('sel', g, kj, kj == 0, kj == qi))
                nt = len(tasks)
                sbank = [None] * nt
                ptile = [None] * nt

                def S_(t):
                    br, g, kj, first, last = tasks[t]
                    bk = PS()
                    sbank[t] = bk
                    if br == 'win':
                        MM(V3(bk[:, :], 4), kwT[:, kj * 128:(kj + 1) * 128], rq_(g, i), rd=[kwT, qz[g]], wr=[bk])
                    else:
                        selbT = T[f'selbT{par}{g}']
                        MM(V3(bk[:, :], 4), ksT[:, kj * 128:(kj + 1) * 128], rq_(g, i), start=True, stop=False, rd=[ksT, qz[g]], wr=[bk])
                        MM(V3(bk[:, :], 4), Eall[:, kj * 128:(kj + 1) * 128], selbT[:], start=False, stop=True,
                           rd=[selbT], wr=[bk])

                def E_(t):
                    br, g, kj, first, last = tasks[t]
                    bk = sbank[t]
                    pt = next_pe()
                    ptile[t] = pt
                    ACT(pt[:], bk[:, :], AF.Exp, rd=[bk], wr=[pt], scale=0.125)
                    if kj == qi:
                        TT(pt[:], pt[:], maskD4[:], ALU.mult, rd=[pt], wr=[pt])
                    elif br == 'win' and kj == qi - 4:
                        TT(pt[:], pt[:], maskW4[:], ALU.mult, rd=[pt], wr=[pt])

                def V_(t):
                    br, g, kj, first, last = tasks[t]
                    pt = ptile[t]
                    accb = ACCB if br == 'win' else ACCA
                    vaug = vw_aug if br == 'win' else vs_aug
                    for hh in range(4):
                        MM(accb[:, hh * 65:(hh + 1) * 65], pt[:, hh * 128:(hh + 1) * 128], vaug[:, kj, g, :],
                           start=(first and hh == 0), stop=last, rd=[pt, vaug], wr=[accb])
                    if last:
                        dst = T['owin'] if br == 'win' else T['osel']
                        ACOPY(dst[:, g].rearrange("p a b -> p (a b)"), accb[:, 0:260], rd=[accb], wr=[dst])

                for t in range(nt + 2):
                    if t < nt:
                        S_(t)
                    if 0 <= t - 1 < nt:
                        E_(t - 1)
                    if 0 <= t - 2 < nt:
                        V_(t - 2)
                attn_combine(i, qi, par, oattn)
                pending_tr = (i, oattn)
            attn_transposes(*pending_tr)
            if CUT < 50:
                return
            sig, t2, mixT, tmp = T['sig'], T['tmp'], T['mixT'], T['tmp']
            for side in range(2):
                cu, wu_ = R.get(wchunk((wup_d if side == 0 else wua_d)[:, :], 1024, nk=4))
                wuv = V3(wu_[:, :], 4)
                srcT = ypoolT if side == 0 else oattnT
                for mh in range(2):
                    c0 = 1816 + side * 1024 + mh * 512
                    cm, wm = R.get(wchunk(win_d[:, c0:c0 + 512], 512))
                    wmv = V3(wm[:, :], 8)
                    for dq in range(4):
                        dc = mh * 4 + dq
                        bka = PS()
                        for kc in range(4):
                            MM(bka[:, :], wuv[:, kc, dc * 128:(dc + 1) * 128], srcT[:, kc, :], start=(kc == 0), stop=(kc == 3),
                               rd=[wu_, srcT], wr=[bka])
                        bkg = PS()
                        for k in range(8):
                            MM(bkg[:, :], wmv[:, k, dq * 128:(dq + 1) * 128], h1T[:, k, :], start=(k == 0), stop=(k == 7),
                               rd=[wm, h1T], wr=[bkg])
                        ACT(sig[:], bkg[:, :], AF.Sigmoid, rd=[bkg], wr=[sig])
                        if side == 0:
                            TT(mixT[:, dc, :], sig[:], bka[:, :], ALU.mult, rd=[sig, bka], wr=[mixT])
                        else:
                            TT(t2[:], sig[:], bka[:, :], ALU.mult, rd=[sig, bka], wr=[t2])
                            TT(mixT[:, dc, :], t2[:], mixT[:, dc, :], ALU.add, rd=[t2, mixT], wr=[mixT])
                    R.done(cm)
                R.done(cu)
            if CUT < 60:
                return
            for dh in range(2):
                co, wo = R.get(wchunk(wout_d[:, dh * 512:(dh + 1) * 512], 512))
                wov = V3(wo[:, :], 8)
                for i in range(4):
                    bk = PS()
                    for k in range(8):
                        MM(bk[:, :], mixT[:, k, i * 128:(i + 1) * 128], wov[:, k, :], start=(k == 0), stop=(k == 7), rd=[mixT, wo], wr=[bk])
                    TT(tmp[:], bk[:, :], g1bc[:, dh * 512:(dh + 1) * 512], ALU.mult, rd=[bk, g1bc], wr=[tmp])
                    a_ = acc[:, jb + i, dh * 512:(dh + 1) * 512]
                    TT(a_, a_, tmp[:], ALU.add, rd=[tmp, ('acc', jb + i)], wr=[('acc', jb + i)])
                R.done(co)

        def alloc_moe(ph, tag):
            T = {}

            def a(name, shape, dt):
                T[name] = sb(f"{name}_{tag}", shape, dt, ph)
            a('sqj', [128, D], BF16)
            a('ss', [128, 8], F32)
            a('lnv', [128, 8], F32)
            a('rstd', [128, 8], F32)
            a('xn2_0', [128, D], F32)
            a('xn2_1', [128, D], F32)
            a('h2f_0', [128, 8, 128], F32)
            a('h2f_1', [128, 8, 128], F32)
            a('h2T', [128, 8, 1024], BF16)
            a('lg', [128, 8, 36], F32)
            a('sm', [128, 8, 8], F32)
            a('goh', [128, 8, 4], F32)
            a('eg', [128, 8, 4], F32)
            a('lesel', [128, 8, 8], F32)
            a('m8', [128, 8, 8], F32)
            a('c8', [128, 8, 8], F32)
            a('c8b', [128, 8, 8], F32)
            a('sg0', [128, 512], BF16)
            a('sg1', [128, 512], BF16)
            a('he0', [128, 4, 512], BF16)
            a('he1', [128, 4, 512], BF16)
            a('ot0', [128, D], F32)
            a('ot1', [128, D], F32)
            return T

        def moe_group(T, b, grp):
            ss, lnv, rstd, h2T = T['ss'], T['lnv'], T['rstd'], T['h2T']
            lg, sm, goh, eg, lesel, m8, c8, c8b = T['lg'], T['sm'], T['goh'], T['eg'], T['lesel'], T['m8'], T['c8'], T['c8b']
            for j in range(8):
                ACT(T['sqj'][:], acc[:, j, :], AF.Square, rd=[('acc', j)], wr=[T['sqj'], (ss.name, j)], accum_out=ss[:, j:j + 1])
            ACT(lnv[:], ss[:], AF.Ln, rd=[(ss.name, j) for j in range(8)], wr=[lnv], scale=1.0 / D, bias=EPS)
            ACT(rstd[:], lnv[:], AF.Exp, rd=[lnv], wr=[rstd], scale=-0.5)
            def pre_a(j):
                xn2 = T[f'xn2_{j % 2}']
                h2f = T[f'h2f_{j % 2}']
                ACT(xn2[:], acc[:, j, :], AF.Identity, rd=[('acc', j), rstd], wr=[xn2], scale=rstd[:, j:j + 1])
                tb = [PS(), PS()]
                for c in range(8):
                    TR(tb[c // 4][:, (c % 4) * 128:(c % 4 + 1) * 128], xn2[:, c * 128:(c + 1) * 128], identf[:], rd=[xn2], wr=[tb[c // 4]])
                for c in range(8):
                    src_ = tb[c // 4][:, (c % 4) * 128:(c % 4 + 1) * 128]
                    if c < 4:
                        TS(h2f[:, c, :], src_, s2T[:, c, b:b + 1], modT[:, 16 + c, b:b + 1],
                           ALU.mult, ALU.add, rd=[tb[c // 4], s2T, modT], wr=[(h2f.name, 0)])
                    else:
                        ACT(h2f[:, c, :], src_, AF.Identity, rd=[tb[c // 4], s2T, modT], wr=[(h2f.name, 1)],
                            scale=s2T[:, c, b:b + 1], bias=modT[:, 16 + c, b:b + 1])

            def pre_b(j):
                h2f = T[f'h2f_{j % 2}']
                hk = [(h2f.name, 0), (h2f.name, 1)]
                ACOPY(h2T[:, :, j * 128:(j + 1) * 128], h2f[:], rd=hk, wr=[h2T])
                bk = PS()
                for c in range(8):
                    MM(bk[:, 0:36], h2f[:, c, :], rw[:, c, :], start=(c == 0), stop=(c == 7), rd=hk + [rw], wr=[bk])
                TT(lg[:, j, :], bk[:, 0:36], rb_bc[:], ALU.add, rd=[bk, rb_bc], wr=[lg])

            pre_a(0)
            for j in range(8):
                if j + 1 < 8:
                    pre_a(j + 1)
                pre_b(j)
            def b3(ap2, n):
                return ap2.unsqueeze(2).broadcast_to([128, 8, n])
            L4 = lg[:, :, 0:4]
            gmax, gsum, gp_ = sm[:, 0, :], sm[:, 1, :], sm[:, 2, :]
            dd, ed, w0, w1 = sm[:, 3, :], sm[:, 4, :], sm[:, 5, :], sm[:, 6, :]
            P.emit('dve', lambda e: e.tensor_reduce(out=gmax, in_=L4, axis=AX.X, op=ALU.max), [lg], [sm])
            TT(goh[:], L4, b3(gmax, 4), ALU.is_equal, rd=[lg, sm], wr=[goh])
            TT(eg[:], L4, b3(gmax, 4), ALU.subtract, rd=[lg, sm], wr=[eg])
            ACT(eg[:], eg[:], AF.Exp, rd=[eg], wr=[eg])
            P.emit('dve', lambda e: e.tensor_reduce(out=gsum, in_=eg[:], axis=AX.X, op=ALU.add), [eg], [sm])
            P.emit('dve', lambda e: e.reciprocal(out=gp_, in_=gsum), [sm], [sm])
            TT(lesel[:], lg[:, :, 4:12], b3(goh[:, :, 0], 8), ALU.mult, rd=[lg, goh], wr=[lesel])
            for g in range(1, 4):
                TT(c8b[:], lg[:, :, 4 + 8 * g:12 + 8 * g], b3(goh[:, :, g], 8), ALU.mult, rd=[lg, goh], wr=[c8b])
                TT(lesel[:], lesel[:], c8b[:], ALU.add, rd=[lesel, c8b], wr=[lesel])
            for j in range(8):
                P.emit('dve', lambda e, j=j: e.max(out=m8[:, j, :], in_=lesel[:, j, :]), [lesel], [m8])
            TT(dd, m8[:, :, 1], m8[:, :, 0], ALU.subtract, rd=[m8], wr=[sm])
            ACT(ed, dd, AF.Exp, rd=[sm], wr=[sm])
            TS(w0, ed, 1.0, None, ALU.add, rd=[sm], wr=[sm])
            P.emit('dve', lambda e: e.reciprocal(out=w0, in_=w0), [sm], [sm])
            TT(w1, ed, w0, ALU.mult, rd=[sm], wr=[sm])
            TT(w0, w0, gp_, ALU.mult, rd=[sm], wr=[sm])
            TT(w1, w1, gp_, ALU.mult, rd=[sm], wr=[sm])
            TT(c8[:], lesel[:], b3(m8[:, :, 0], 8), ALU.is_equal, rd=[lesel, m8], wr=[c8])
            TT(c8[:], c8[:], b3(w0, 8), ALU.mult, rd=[c8, sm], wr=[c8])
            TT(c8b[:], lesel[:], b3(m8[:, :, 1], 8), ALU.is_equal, rd=[lesel, m8], wr=[c8b])
            TT(c8b[:], c8b[:], b3(w1, 8), ALU.mult, rd=[c8b, sm], wr=[c8b])
            TT(c8[:], c8[:], c8b[:], ALU.add, rd=[c8, c8b], wr=[c8])
            for g in range(4):
                TT(comb[:, :, 8 * g:8 * g + 8], c8[:], b3(goh[:, :, g], 8), ALU.mult, rd=[c8, goh], wr=[comb])
            for e_ in range(NEXP if CUT >= 80 else 0):
                cg, wg = R.get(wchunk(eg_d[e_], 512))
                cu, wu_ = R.get(wchunk(eu_d[e_], 512))
                cd, wd = R.get(wchunk(ed_d[e_], 1024, nk=4))
                wgv, wuv, wdv = V3(wg[:, :], 8), V3(wu_[:, :], 8), V3(wd[:, :], 4)
                TT(wdv, wdv, g2bc[:].unsqueeze(1).broadcast_to([128, 4, D]), ALU.mult, rd=[wd, g2bc], wr=[wd])
                for half in range(2):
                    he = T[f'he{half}']
                    ts_ = slice(half * 512, (half + 1) * 512)
                    for fc in range(4):
                        bg_ = PS()
                        for k in range(8):
                            MM(bg_[:, :], wgv[:, k, fc * 128:(fc + 1) * 128], h2T[:, k, ts_], start=(k == 0), stop=(k == 7), rd=[wg, h2T], wr=[bg_])
                        bu_ = PS()
                        for k in range(8):
                            MM(bu_[:, :], wuv[:, k, fc * 128:(fc + 1) * 128], h2T[:, k, ts_], start=(k == 0), stop=(k == 7), rd=[wu_, h2T], wr=[bu_])
                        sg = T[f'sg{fc % 2}']
                        ACT(sg[:], bg_[:, :], AF.Silu, rd=[bg_], wr=[sg])
                        TT(he[:, fc, :], sg[:], bu_[:, :], ALU.mult, rd=[sg, bu_], wr=[he])
                    if half == 1:
                        R.done(cg)
                        R.done(cu)
                    for i in range(4):
                        j = half * 4 + i
                        for dh in range(2):
                            by = PS()
                            for fc in range(4):
                                MM(by[:, :], he[:, fc, i * 128:(i + 1) * 128], wdv[:, fc, dh * 512:(dh + 1) * 512],
                                   start=(fc == 0), stop=(fc == 3), rd=[he, wd], wr=[by])
                            a_ = acc[:, j, dh * 512:(dh + 1) * 512]
                            STT(a_, by[:, :], comb[:, j, e_:e_ + 1], a_, ALU.mult, ALU.add, rd=[by, comb, ('acc', j)], wr=[('acc', j)])
                R.done(cd)
            for j in range(8):
                ACT(T['sqj'][:], acc[:, j, :], AF.Square, rd=[('acc', j)], wr=[T['sqj'], (ss.name, j)], accum_out=ss[:, j:j + 1])
            ACT(lnv[:], ss[:], AF.Ln, rd=[(ss.name, j) for j in range(8)], wr=[lnv], scale=1.0 / D, bias=EPS)
            ACT(rstd[:], lnv[:], AF.Exp, rd=[lnv], wr=[rstd], scale=-0.5)
            for j in range(8):
                ot = T[f'ot{j % 2}']
                STT(ot[:], acc[:, j, :], rstd[:, j:j + 1], fg_bc[:], ALU.mult, ALU.mult, rd=[('acc', j), rstd, fg_bc], wr=[ot])
                r0 = b * S + grp * 1024 + j * 128
                DMA('sp', y_d[r0:r0 + 128, :], ot[:], f'st{j % 2}', [ot], [('yout', j % 2)])

        P.plan = True
        gen()
        P.plan = False
        gen()
        P.run()
    return nc


def _consts():
    f = np.float32
    c = {}
    c['identf'] = np.eye(128, dtype=f)
    k = np.arange(128)[:, None]
    q = np.arange(128)[None, :]
    c['maskD4'] = np.tile((k <= q).astype(f), (1, 4))
    c['maskW4'] = np.tile((k > q).astype(f), (1, 4))
    mc = np.zeros((128, 16, 128), f)
    n = np.arange(128)[:, None]
    for qi in range(16):
        t = qi * 128 + np.arange(128)[None, :]
        mc[:, qi, :] = ((16 * n + 31 <= t) & (n < 127)).astype(f)
    c['maskC'] = mc
    ea = np.zeros((32, 2048), f)
    ea[np.arange(2048) // 64, np.arange(2048)] = 1.0
    c['Eall'] = ea
    ptcur = np.zeros((128, 4, 128), f)
    ptfirst = np.zeros((128, 4, 128), f)
    ptprev = np.zeros((128, 4, 128), f)
    for g, w in enumerate((2, 4, 8, 16)):
        for t in range(128):
            for tp in range(max(0, t - w + 1), t + 1):
                ptcur[tp, g, t] += 1.0 / w
                ptfirst[tp, g, t] += 1.0 / min(t + 1, w)
            ptcur[t, g, t] -= 1.0
            ptfirst[t, g, t] -= 1.0
            for tp in range(t - w + 1, 0):
                ptprev[128 + tp, g, t] += 1.0 / w
    c['ptcur'], c['ptfirst'], c['ptprev'] = ptcur, ptfirst, ptprev
    s1 = np.arange(127)[:, None] * 16
    s2 = np.arange(32)[None, :] * 64
    ov = np.clip(np.minimum(s1 + 32, s2 + 64) - np.maximum(s1, s2), 0, None) / 32.0
    ovl = np.zeros((128, 33), f)
    ovl[:, 0] = 1.0
    ovl[:127, 1:] = ov
    c['ovl1'] = ovl
    sv = np.zeros((128, 16, 32), f)
    sbias = np.zeros((128, 16, 32), f)
    blk = np.arange(32)[None, :]
    for qi in range(16):
        t = qi * 128 + np.arange(128)
        cur = (t // 64)[:, None]
        valid = blk <= cur
        forced = (blk == 0) | (blk == cur) | (blk == cur - 1)
        sv[:, qi, :] = (valid & ~forced).astype(f)
        sbias[:, qi, :] = np.where(forced, 1e4, np.where(valid, 0.0, -1e30)).astype(f)
    c['selvalid'], c['selbias'] = sv, sbias
    return c


def _fm(v, nch):
    return np.ascontiguousarray(np.asarray(v, np.float32).reshape(nch, 128).T)


def prep_shared(inp):
    f = np.float32
    m = dict(_consts())
    m['ada_w'] = np.ascontiguousarray(inp['ada_w'][0], f)
    adab = np.asarray(inp['ada_b'][0], f)
    m['adabT'] = _fm(adab, 48)
    m['adab_bc'] = np.ascontiguousarray(np.broadcast_to(
        np.stack([adab[2048:3072], adab[5120:6144]], 0)[None], (128, 2, D)), f)
    m['n1g'] = _fm(inp['norm1_g'][0], 8)
    m['n2g'] = _fm(inp['norm2_g'][0], 8)
    m['fg_bc'] = np.ascontiguousarray(np.broadcast_to(np.asarray(inp['final_g'], f)[None], (128, D)), f)
    w_in = np.asarray(inp['w_in'][0], f)
    qperm = np.array([512 + (g * 4 + hh) * 64 + d for hh in range(4) for g in range(2) for d in range(64)])
    cols = np.concatenate([np.arange(0, 512), np.arange(1408, 1536), np.arange(1664, 1792), np.arange(1792, 1816),
                           qperm, np.arange(1024, 1152), np.arange(1152, 1280), np.arange(1280, 1408),
                           np.arange(1536, 1664), np.arange(1816, 3864)])
    m['w_in_p'] = np.ascontiguousarray(w_in[:, cols])
    m['pool_w_r'] = np.ascontiguousarray(np.transpose(np.asarray(inp['pool_w'][0], f), (1, 0, 2)))
    m['pscT'] = _fm(inp['pool_scale'][0], 4)
    pos = np.asarray(inp['cmp_pos'][0], f)
    pt = np.transpose(pos, (2, 0, 1))
    m['posT'] = np.ascontiguousarray(np.concatenate([pt, pt], 0))
    m['cmp_w1'] = np.ascontiguousarray(inp['cmp_w1'][0], f)
    b1 = np.asarray(inp['cmp_b1'][0], f)
    m['b1T'] = np.ascontiguousarray(np.transpose(b1.reshape(2, 2, 128), (2, 0, 1)))
    w2 = np.asarray(inp['cmp_w2'][0], f)
    m['w2r'] = np.ascontiguousarray(np.transpose(w2.reshape(2, 2, 128, 64), (2, 0, 1, 3)))
    m['w_up_pool'] = np.ascontiguousarray(inp['w_up_pool'][0], f)
    m['w_up_attn'] = np.ascontiguousarray(inp['w_up_attn'][0], f)
    m['w_out'] = np.ascontiguousarray(inp['w_out'][0], f)
    rwf = np.concatenate([np.asarray(inp['router_g_w'][0], f), np.asarray(inp['router_e_w'][0], f)], 1)
    m['rw'] = np.ascontiguousarray(np.transpose(rwf.reshape(8, 128, 36), (1, 0, 2)))
    rb = np.concatenate([np.asarray(inp['router_g_b'][0], f), np.asarray(inp['router_e_b'][0], f)], 0)
    m['rb_bc'] = np.ascontiguousarray(np.broadcast_to(rb[None], (128, 36)), f)
    m['exp_w_gate'] = np.ascontiguousarray(inp['exp_w_gate'][0], f)
    m['exp_w_up'] = np.ascontiguousarray(inp['exp_w_up'][0], f)
    m['exp_w_down'] = np.ascontiguousarray(inp['exp_w_down'][0], f)
    return m


def prep_core(inp, shared, b0, nseq):
    f = np.float32
    m = dict(shared)
    m['x'] = np.ascontiguousarray(np.asarray(inp['x'][b0:b0 + nseq], f).reshape(nseq * S, D))
    c = np.asarray(inp['c'][b0:b0 + nseq], f)
    ck = np.transpose(c.reshape(nseq, 8, 128), (2, 1, 0))
    m['cT'] = np.ascontiguousarray(ck)
    m['crep'] = np.ascontiguousarray(np.broadcast_to(np.transpose(c.reshape(nseq, 8, 128), (0, 2, 1))[:, :, :, None],
                                                     (nseq, 128, 8, 128)), f)
    return m


_NC_CACHE = {}


def kernel(**inputs):
    nseq = 32 // NCORES
    if nseq not in _NC_CACHE:
        _NC_CACHE[nseq] = build(nseq)
    nc = _NC_CACHE[nseq]
    shared = prep_shared(inputs)
    in_maps = [prep_core(inputs, shared, core * nseq, nseq) for core in range(NCORES)]
    res = run_bass_kernel_spmd(nc, in_maps, core_ids=list(range(NCORES)))
    out = np.concatenate([np.asarray(r["y"], np.float32).reshape(nseq, S, D) for r in res.results], axis=0)
    return out
```

```python
import numpy as np
from contextlib import ExitStack
import concourse.bass as bass
import concourse.mybir as mybir
from concourse.bass_utils import run_bass_kernel_spmd

F32 = mybir.dt.float32
BF16 = mybir.dt.bfloat16
AF = mybir.ActivationFunctionType
ALU = mybir.AluOpType
AX = mybir.AxisListType

D = 1024
S = 2048
NCORES = 8
NR = 4
NEXP = 32
EPS = 1e-6
CUT = 100


class Prog:
    ENGS = ['pe', 'act', 'dve', 'pool', 'sp']

    def __init__(self, nc, es):
        self.nc = nc
        self.es = es
        self.plan = False
        self.ops = {e: [] for e in self.ENGS}
        self.sems = {}
        self.semcount = {}
        self.known = {e: {} for e in self.ENGS}
        self.lastw = {}
        self.readers = {}
        self.nbank = 0
        for e in ['pe', 'act', 'dve', 'pool']:
            self.sem('c_' + e)

    def sem(self, name):
        if name not in self.sems:
            self.sems[name] = self.es.enter_context(self.nc.semaphore(name))
            self.semcount[name] = 0
        return name

    @staticmethod
    def _key(r):
        if isinstance(r, (str, tuple)):
            return r
        t = getattr(r, 'tensor', r)
        return t.name

    def emit(self, eng, fn, reads=(), writes=(), sem=None):
        if self.plan:
            return
        is_dma = sem is not None
        if not is_dma:
            sem = 'c_' + eng
            inc = 1
        else:
            self.sem(sem)
            inc = 16
        waits = {}

        def need(dep, raw):
            s, v, e, d = dep
            if (not d) and e == eng and eng == 'pe':
                return
            if d and is_dma and (not raw) and s == sem:
                return
            if self.known[eng].get(s, 0) >= v:
                return
            waits[s] = max(waits.get(s, 0), v)

        for r in reads:
            k = self._key(r)
            if k in self.lastw:
                need(self.lastw[k], True)
        for w in writes:
            k = self._key(w)
            if k in self.lastw:
                need(self.lastw[k], False)
            for dep in self.readers.get(k, {}).values():
                need(dep, False)
        for s, v in waits.items():
            self.known[eng][s] = v
        self.semcount[sem] += inc
        tick = self.semcount[sem]
        me = (sem, tick, eng, is_dma)
        self.ops[eng].append((list(waits.items()), fn, sem, inc))
        for r in reads:
            k = self._key(r)
            self.readers.setdefault(k, {})[(eng, sem)] = me
        for w in writes:
            k = self._key(w)
            self.lastw[k] = me
            self.readers[k] = {}

    def barrier(self):
        if self.plan:
            return
        for eng in self.ENGS:
            waits = []
            for s, c in self.semcount.items():
                if c > 0 and self.known[eng].get(s, 0) < c:
                    waits.append((s, c))
                    self.known[eng][s] = c
            if waits:
                self.ops[eng].append((waits, None, None, 0))

    def run(self):
        nc = self.nc
        P = self

        def replay(e, eng):
            for (waits, fn, sem, inc) in P.ops[e]:
                for (s, v) in waits:
                    eng.wait_ge(P.sems[s], v)
                if fn is not None:
                    ins = fn(eng)
                    ins.then_inc(P.sems[sem], inc)

        with nc.Block() as block:
            @block.tensor
            def _(eng):
                replay('pe', eng)

            @block.scalar
            def _(eng):
                replay('act', eng)

            @block.vector
            def _(eng):
                replay('dve', eng)

            @block.gpsimd
            def _(eng):
                replay('pool', eng)

            @block.sync
            def _(eng):
                replay('sp', eng)


class Ring:
    def __init__(self, P, slots):
        self.P = P
        self.slots = slots
        self.seq = []
        self.reset()

    def reset(self):
        self.idx = 0
        self.loaded = 0
        self.dead = set()

    def get(self, loader):
        i = self.idx
        self.idx += 1
        if self.P.plan:
            self.seq.append(loader)
            return i, self.slots[i % NR]
        self.top_up()
        assert self.loaded > i, ("ring too many live chunks", i)
        return i, self.slots[i % NR]

    def done(self, i):
        if self.P.plan:
            return
        self.dead.add(i)
        self.top_up()

    def top_up(self):
        while self.loaded < len(self.seq) and (self.loaded < NR or (self.loaded - NR) in self.dead):
            c = self.loaded
            self.seq[c](self.slots[c % NR], c % NR)
            self.loaded += 1


def build(nseq, dbg=False):
    nc = bass.Bass("TRN2", target_bir_lowering=False)
    ntok = nseq * S

    def din(name, shape, dt=F32):
        return nc.dram_tensor(name, list(shape), dt, kind="ExternalInput").ap()

    x_d = din("x", [ntok, D])
    crep_d = din("crep", [nseq, 128, 8, 128])
    cT_d = din("cT", [128, 8, nseq])
    adaw_d = din("ada_w", [D, 6 * D])
    adabT_d = din("adabT", [128, 48])
    adabbc_d = din("adab_bc", [128, 2, D])
    n1g_d = din("n1g", [128, 8])
    n2g_d = din("n2g", [128, 8])
    fgbc_d = din("fg_bc", [128, D])
    win_d = din("w_in_p", [D, 3864])
    poolw_d = din("pool_w_r", [128, 4, 128])
    psc_d = din("pscT", [128, 4])
    posT_d = din("posT", [128, 2, 32])
    w1_d = din("cmp_w1", [2, 2048, 256])
    b1T_d = din("b1T", [128, 2, 2])
    w2r_d = din("w2r", [128, 2, 2, 64])
    wup_d = din("w_up_pool", [512, D])
    wua_d = din("w_up_attn", [512, D])
    wout_d = din("w_out", [D, D])
    rw_d = din("rw", [128, 8, 36])
    rbbc_d = din("rb_bc", [128, 36])
    eg_d = din("exp_w_gate", [NEXP, D, 512])
    eu_d = din("exp_w_up", [NEXP, D, 512])
    ed_d = din("exp_w_down", [NEXP, 512, D])
    identf_d = din("identf", [128, 128])
    maskD_d = din("maskD4", [128, 512])
    maskW_d = din("maskW4", [128, 512])
    maskC_d = din("maskC", [128, 16, 128])
    eall_d = din("Eall", [32, 2048])
    ptcur_d = din("ptcur", [128, 4, 128])
    ptfirst_d = din("ptfirst", [128, 4, 128])
    ptprev_d = din("ptprev", [128, 4, 128])
    ovl_d = din("ovl1", [128, 33])
    selv_d = din("selvalid", [128, 16, 32])
    selb_d = din("selbias", [128, 16, 32])
    y_d = nc.dram_tensor("y", [ntok, D], F32, kind="ExternalOutput").ap()
    if dbg:
        dbg_d = nc.dram_tensor("dbg_x1", [ntok, D], F32, kind="ExternalOutput").ap()

    with ExitStack() as es:
        P = Prog(nc, es)

        uniq = [0]

        def sb(name, shape, dt, stack=es):
            uniq[0] += 1
            return stack.enter_context(nc.sbuf_tensor(f"{name}_u{uniq[0]}", list(shape), dt))

        identb = sb("identb", [128, 128], BF16)
        identf = sb("identf_s", [128, 128], F32)
        maskD4 = sb("maskD4_s", [128, 512], BF16)
        maskW4 = sb("maskW4_s", [128, 512], BF16)
        maskC = sb("maskC_s", [128, 16, 128], BF16)
        Eall = sb("Eall_s", [128, 2048], BF16)
        ptcur = sb("ptcur_s", [128, 4, 128], BF16)
        ptfirst = sb("ptfirst_s", [128, 4, 128], BF16)
        ptprev = sb("ptprev_s", [128, 4, 128], BF16)
        selvalid = sb("selvalid_s", [128, 16, 32], F32)
        selbiasc = sb("selbias_s", [128, 16, 32], F32)
        modT = sb("modT", [128, 32, nseq], F32)
        s1T = sb("s1T", [128, 8, nseq], F32)
        s2T = sb("s2T", [128, 8, nseq], F32)
        adabT = sb("adabT_s", [128, 48], F32)
        n1g = sb("n1g_s", [128, 8], F32)
        n2g = sb("n2g_s", [128, 8], F32)
        fg_bc = sb("fg_bc_s", [128, D], F32)
        g1bc = sb("g1bc", [128, D], F32)
        g2bc = sb("g2bc", [128, D], F32)
        poolw = sb("poolw_s", [128, 4, 128], BF16)
        pscT = sb("pscT_s", [128, 4], F32)
        posT = sb("posT_s", [128, 2, 32], BF16)
        b1T = sb("b1T_s", [128, 2, 2], F32)
        biasc = sb("biasc", [128, 2, 2], F32)
        w2sb = sb("w2sb", [128, 2, 2, 64], BF16)
        rw = sb("rw_s", [128, 8, 36], F32)
        rb_bc = sb("rb_bc_s", [128, 36], F32)
        crep = sb("crep_s", [128, 8, 128], BF16)
        cT = sb("cT_s", [128, 8, nseq], BF16)
        acc = sb("acc", [128, 8, D], F32)
        comb = sb("comb", [128, 8, 32], F32)
        kcT = sb("kcT", [128, S], BF16)
        vcT = sb("vcT", [128, S], BF16)
        ksT = sb("ksT", [128, S], BF16)
        kwT = sb("kwT", [128, S], BF16)
        kcmpT = sb("kcmpT", [128, 128], BF16)
        vcaug = sb("vcaug", [128, 2, 97], BF16)
        hidv = sb("hidv", [128, 2, 2, 128], BF16)
        vs_aug = sb("vs_aug", [128, 16, 2, 65], BF16)
        vw_aug = sb("vw_aug", [128, 16, 2, 65], BF16)
        bg = sb("bg", [128, 16, 24], F32)
        ucarry = sb("ucarry", [128, 512], BF16)
        ring = [sb(f"ring{i}", [128, 4096], BF16) for i in range(NR)]
        R = Ring(P, ring)

        banks = [es.enter_context(nc.psum_tensor(f"bank{i}", [128, 512], F32)) for i in range(6)]
        ACCA = es.enter_context(nc.psum_tensor("ACCA", [128, 512], F32))
        ACCB = es.enter_context(nc.psum_tensor("ACCB", [128, 512], F32))

        def PS():
            b = banks[P.nbank % 6]
            P.nbank += 1
            return b

        def MM(out, lhsT, rhs, start=True, stop=True, rd=(), wr=()):
            P.emit('pe', lambda e: e.matmul(out, lhsT, rhs, start=start, stop=stop, skip_group_check=True), rd, wr)

        def TR(out, in_, ident, rd=(), wr=()):
            P.emit('pe', lambda e: e.transpose(out, in_, ident), rd, wr)

        def ACT(out, in_, func, rd=(), wr=(), **kw):
            P.emit('act', lambda e: e.activation(out=out, in_=in_, func=func, **kw), rd, wr)

        def ACOPY(out, in_, rd=(), wr=()):
            P.emit('act', lambda e: e.copy(out=out, in_=in_), rd, wr)

        def TS(out, in0, s1, s2, op0, op1=None, rd=(), wr=(), eng='dve'):
            if op1 is None:
                P.emit(eng, lambda e: e.tensor_scalar(out=out, in0=in0, scalar1=s1, scalar2=None, op0=op0), rd, wr)
            else:
                P.emit(eng, lambda e: e.tensor_scalar(out=out, in0=in0, scalar1=s1, scalar2=s2, op0=op0, op1=op1), rd, wr)

        def TT(out, in0, in1, op, rd=(), wr=(), eng='dve'):
            P.emit(eng, lambda e: e.tensor_tensor(out=out, in0=in0, in1=in1, op=op), rd, wr)

        def STT(out, in0, scalar, in1, op0, op1, rd=(), wr=()):
            P.emit('dve', lambda e: e.scalar_tensor_tensor(out=out, in0=in0, scalar=scalar, in1=in1, op0=op0, op1=op1), rd, wr)

        def CP(out, in_, rd=(), wr=(), eng='dve'):
            P.emit(eng, lambda e: e.tensor_copy(out=out, in_=in_), rd, wr)

        def DMA(eng, out, in_, sem, rd=(), wr=()):
            P.emit(eng, lambda e: e.dma_start(out=out, in_=in_), rd, wr, sem=sem)

        def wchunk(src2d, ncols, nk=8):
            def loader(slot, si):
                dst = slot[:, 0:nk * ncols].rearrange("p (k n) -> p k n", k=nk)
                DMA('pool', dst, src2d.rearrange("(k p) n -> p k n", p=128), f'ring{si}', (), [slot])
            return loader

        def w1chunk(kv, half):
            def loader(slot, si):
                src = w1_d[kv].rearrange("(l d) c -> d l c", d=64)[:, 16 * half:16 * half + 16, :]
                for hp in range(2):
                    dst = slot[64 * hp:64 * hp + 64, :].rearrange("p (l c) -> p l c", l=16)
                    DMA('pool', dst, src, f'ring{si}', (), [slot])
            return loader

        def V3(ap2d, a):
            return ap2d.rearrange("p (a b) -> p a b", a=a)

        def gen():
            P.nbank = 0
            R.reset()
            for (dst, src) in [(identf, identf_d), (selvalid, selv_d), (selbiasc, selb_d), (adabT, adabT_d),
                               (n1g, n1g_d), (n2g, n2g_d), (fg_bc, fgbc_d), (pscT, psc_d), (b1T, b1T_d),
                               (rw, rw_d), (rb_bc, rbbc_d)]:
                DMA('sp', dst[:], src, 'cst_sp', (), [dst])
            P.emit('dve', lambda e: e.memset(Eall[:], 0.0), (), [Eall])
            DMA('pool', Eall[0:32, :], eall_d, 'cst_pool', (), [Eall])
            DMA('pool', Eall[64:96, :], eall_d, 'cst_pool', (), [Eall])
            for (dst, src) in [(maskD4, maskD_d), (maskW4, maskW_d), (maskC, maskC_d),
                               (ptcur, ptcur_d), (ptfirst, ptfirst_d), (ptprev, ptprev_d),
                               (poolw, poolw_d), (posT, posT_d), (w2sb, w2r_d), (cT, cT_d)]:
                DMA('pool', dst[:], src, 'cst_pool', (), [dst])
            DMA('pool', identb[:], identf_d, 'cst_pool', (), [identb])
            for g in range(2):
                DMA('pool', vcaug[:, g, 64:97], ovl_d, 'cst_pool', (), [vcaug])
            P.emit('dve', lambda e: e.memset(hidv[:], 0.0), (), [hidv])
            P.emit('dve', lambda e: e.memset(kcmpT[:], 0.0), (), [kcmpT])
            P.emit('dve', lambda e: e.memset(vs_aug[:], 1.0), (), [vs_aug])
            P.emit('dve', lambda e: e.memset(vw_aug[:], 1.0), (), [vw_aug])
            P.emit('dve', lambda e: e.memset(ucarry[:], 0.0), (), [ucarry])
            for g in range(2):
                P.emit('dve', lambda e, g=g: e.memset(vcaug[:, g, 0:64], 0.0), (), [vcaug])
            P.barrier()

            for mi, c0 in enumerate([0, 1024, 3072, 4096]):
                for hf in range(2):
                    ci, w = R.get(wchunk(adaw_d[:, c0 + 512 * hf:c0 + 512 * hf + 512], 512))
                    wv = V3(w[:, :], 8)
                    bk = PS()
                    for fc in range(4):
                        for k in range(8):
                            MM(bk[:, fc * nseq:(fc + 1) * nseq], wv[:, k, fc * 128:(fc + 1) * 128], cT[:, k, :],
                               start=(k == 0), stop=(k == 7), rd=[w, cT], wr=[bk])
                    R.done(ci)
                    j0 = mi * 8 + hf * 4
                    cb = (c0 // 128) + hf * 4
                    TT(modT[:, j0:j0 + 4, :], V3(bk[:, 0:4 * nseq], 4),
                       adabT[:, cb:cb + 4].unsqueeze(2).broadcast_to([128, 4, nseq]), ALU.add, rd=[bk, adabT], wr=[modT])
            TS(s1T[:], modT[:, 8:16, :], 1.0, None, ALU.add, rd=[modT], wr=[s1T])
            TT(s1T[:], s1T[:], n1g[:].unsqueeze(2).broadcast_to([128, 8, nseq]), ALU.mult, rd=[s1T, n1g], wr=[s1T])
            TS(s2T[:], modT[:, 24:32, :], 1.0, None, ALU.add, rd=[modT], wr=[s2T])
            TT(s2T[:], s2T[:], n2g[:].unsqueeze(2).broadcast_to([128, 8, nseq]), ALU.mult, rd=[s2T, n2g], wr=[s2T])

            for kv in range(2):
                ca, wa = R.get(w1chunk(kv, 0))
                cb_, wb = R.get(w1chunk(kv, 1))
                for cc in range(2):
                    bk = PS()
                    for l in range(32):
                        w = wa if l < 16 else wb
                        wv = V3(w[:, :], 16)
                        MM(bk[:, 0:1], wv[0:64, l % 16, cc * 128:(cc + 1) * 128], posT[0:64, kv, l:l + 1],
                           start=(l == 0), stop=(l == 31), rd=[w, posT], wr=[bk])
                    TT(biasc[:, kv, cc:cc + 1], bk[:, 0:1], b1T[:, kv, cc:cc + 1], ALU.add, rd=[bk, b1T], wr=[biasc])
                R.done(ca)
                R.done(cb_)

            for b in range(nseq):
                seq_prologue(b)
                for grp in range(2):
                    with ExitStack() as ph:
                        T = alloc_mixer(ph, f"{b}_{grp}")
                        for sth in range(2):
                            mixer_supertile(T, b, grp * 2 + sth)
                    P.barrier()
                    if dbg:
                        for j in range(8):
                            r0 = b * S + grp * 1024 + j * 128
                            DMA('sp', dbg_d[r0:r0 + 128, :], acc[:, j, :], f'dbgst', [('acc', j)], ['dbgout'])
                        P.barrier()
                    with ExitStack() as ph:
                        T = alloc_moe(ph, f"{b}_{grp}")
                        moe_group(T, b, grp)
                    P.barrier()
            P.barrier()

        def seq_prologue(b):
            DMA('pool', crep[:], crep_d[b], 'crep', (), [crep])
            DMA('sp', g1bc[:], adabbc_d[:, 0, :], 'g1l', (), [g1bc])
            DMA('sp', g2bc[:], adabbc_d[:, 1, :], 'g2l', (), [g2bc])
            for (dst, c0) in [(g1bc, 2048), (g2bc, 5120)]:
                for hf in range(2):
                    ci, w = R.get(wchunk(adaw_d[:, c0 + 512 * hf:c0 + 512 * hf + 512], 512))
                    wv = V3(w[:, :], 8)
                    bk = PS()
                    for k in range(8):
                        MM(bk[:, :], crep[:, k, :], wv[:, k, :], start=(k == 0), stop=(k == 7), rd=[w, crep], wr=[bk])
                    R.done(ci)
                    TT(dst[:, hf * 512:(hf + 1) * 512], bk[:, :], dst[:, hf * 512:(hf + 1) * 512], ALU.add,
                       rd=[bk, dst], wr=[dst])

        def alloc_mixer(ph, tag):
            T = {}

            def a(name, shape, dt):
                T[name] = sb(f"{name}_{tag}", shape, dt, ph)
            a('xn0', [128, D], BF16)
            a('xn1', [128, D], BF16)
            a('ss', [128, 4], F32)
            a('lnv', [128, 4], F32)
            a('rstd', [128, 4], F32)
            a('h1T', [128, 8, 512], BF16)
            a('qz0', [128, 4, 512], BF16)
            a('qz1', [128, 4, 512], BF16)
            for pp in range(2):
                for g in range(2):
                    a(f'selbT{pp}{g}', [128, 4, 128], BF16)
            a('u_tm', [128, 4, 512], BF16)
            a('pooledT', [128, 4, 512], BF16)
            a('ypoolT', [128, 4, 512], BF16)
            a('hidk', [128, 2, 32], BF16)
            for i in range(6):
                a(f'pe{i}', [128, 512], BF16)
            for g in range(2):
                a(f'pc{g}', [128, 512], BF16)
            a('ocrp0', [128, 2, 4, 97], F32)
            a('ocrp1', [128, 2, 4, 97], F32)
            a('osel', [128, 2, 4, 65], F32)
            a('owin', [128, 2, 4, 65], F32)
            a('rs', [128, 2, 4], F32)
            a('tmp4', [128, 2, 4, 32], F32)
            a('psc', [128, 2, 32], F32)
            a('score', [128, 2, 32], F32)
            a('m8', [128, 2, 8], F32)
            a('selb', [128, 2, 32], BF16)
            a('den', [128, 3, 2, 4], F32)
            a('coef', [128, 3, 2, 4], F32)
            a('o1', [128, 2, 4, 64], F32)
            a('o2', [128, 2, 4, 64], F32)
            a('oattn0', [128, 512], BF16)
            a('oattn1', [128, 512], BF16)
            a('oattnT', [128, 4, 512], BF16)
            a('sig', [128, 512], F32)
            a('mixT', [128, 8, 512], BF16)
            a('tmp', [128, 512], F32)
            return T

        def mixer_supertile(T, b, st):
            h1T, u_tm = T['h1T'], T['u_tm']
            qz = [T['qz0'], T['qz1']]
            if 'zeroed' not in T:
                T['zeroed'] = True
                P.emit('dve', lambda e: e.memset(T['qz0'][:], 0.0), (), [T['qz0']])
                P.emit('dve', lambda e: e.memset(T['qz1'][:], 0.0), (), [T['qz1']])
                for nm in ['selbT00', 'selbT01', 'selbT10', 'selbT11']:
                    P.emit('dve', lambda e, nm=nm: e.memset(T[nm][:], 0.0), (), [T[nm]])
            tok0 = st * 512
            jb = (st % 2) * 4
            for i in range(4):
                r0 = b * S + tok0 + i * 128
                DMA('sp', acc[:, jb + i, :], x_d[r0:r0 + 128, :], f'xl{jb + i}', (), [('acc', jb + i)])
            ss, lnv, rstd = T['ss'], T['lnv'], T['rstd']
            for i in range(4):
                ACT(T['xn0'][:], acc[:, jb + i, :], AF.Square, rd=[('acc', jb + i)], wr=[T['xn0'], (ss.name, i)],
                    accum_out=ss[:, i:i + 1])
            ACT(lnv[:], ss[:], AF.Ln, rd=[(ss.name, i) for i in range(4)], wr=[lnv], scale=1.0 / D, bias=EPS)
            ACT(rstd[:], lnv[:], AF.Exp, rd=[lnv], wr=[rstd], scale=-0.5)
            tb = [PS() for _ in range(4)]
            tbv = [t[:, :].bitcast(BF16) for t in tb]
            for i in range(4):
                xn = T[f'xn{i % 2}']
                TS(xn[:], acc[:, jb + i, :], rstd[:, i:i + 1], None, ALU.mult, rd=[('acc', jb + i), rstd], wr=[xn])
                for c in range(8):
                    o = tbv[c // 2][:, (c % 2) * 512 + i * 128:(c % 2) * 512 + (i + 1) * 128]
                    TR(o, xn[:, c * 128:(c + 1) * 128], identb[:], rd=[xn], wr=[tb[c // 2]])
            for c in range(8):
                TS(h1T[:, c, :], tbv[c // 2][:, (c % 2) * 512:(c % 2 + 1) * 512], s1T[:, c, b:b + 1], modT[:, c, b:b + 1],
                   ALU.mult, ALU.add, rd=[tb[c // 2], s1T, modT], wr=[h1T])
            if CUT < 10:
                return
            ci, w = R.get(wchunk(win_d[:, 0:512], 512))
            wv = V3(w[:, :], 8)
            for i in range(4):
                bk = PS()
                for k in range(8):
                    MM(bk[:, :], h1T[:, k, i * 128:(i + 1) * 128], wv[:, k, :], start=(k == 0), stop=(k == 7), rd=[h1T, w], wr=[bk])
                ACOPY(u_tm[:, i, :], bk[:, :], rd=[bk], wr=[u_tm])
            R.done(ci)
            ci, w = R.get(wchunk(win_d[:, 512:792], 280))
            wv = w[:, 0:8 * 280].rearrange("p (k n) -> p k n", k=8)
            for i in range(4):
                tt = st * 4 + i
                bk = PS()
                for k in range(8):
                    MM(bk[:, 0:280], h1T[:, k, i * 128:(i + 1) * 128], wv[:, k, :], start=(k == 0), stop=(k == 7), rd=[h1T, w], wr=[bk])
                ACOPY(vs_aug[:, tt, :, 0:64], V3(bk[:, 0:128], 2), rd=[bk], wr=[vs_aug])
                ACOPY(vw_aug[:, tt, :, 0:64], V3(bk[:, 128:256], 2), rd=[bk], wr=[vw_aug])
                ACT(bg[:, tt, :], bk[:, 256:280], AF.Sigmoid, rd=[bk], wr=[bg])
            R.done(ci)
            ci, w = R.get(wchunk(win_d[:, 792:1304], 512))
            wv = V3(w[:, :], 8)
            for hh in range(4):
                bk = PS()
                for k in range(8):
                    MM(bk[:, :], wv[:, k, hh * 128:(hh + 1) * 128], h1T[:, k, :], start=(k == 0), stop=(k == 7), rd=[h1T, w], wr=[bk])
                if hh % 2 == 0:
                    ACOPY(qz[0][0:64, hh, :], bk[0:64, :], rd=[bk], wr=[qz[0]])
                    ACOPY(qz[1][64:128, hh, :], bk[64:128, :], rd=[bk], wr=[qz[1]])
                else:
                    CP(qz[0][0:64, hh, :], bk[0:64, :], rd=[bk], wr=[qz[0]])
                    CP(qz[1][64:128, hh, :], bk[64:128, :], rd=[bk], wr=[qz[1]])
            R.done(ci)
            ci, w = R.get(wchunk(win_d[:, 1304:1816], 512))
            wv = V3(w[:, :], 8)
            for n, dst in enumerate([kcT, vcT, ksT, kwT]):
                bk = PS()
                for k in range(8):
                    MM(bk[:, :], wv[:, k, n * 128:(n + 1) * 128], h1T[:, k, :], start=(k == 0), stop=(k == 7), rd=[h1T, w], wr=[bk])
                if n % 2 == 0:
                    ACOPY(dst[:, tok0:tok0 + 512], bk[:, :], rd=[bk], wr=[dst])
                else:
                    CP(dst[:, tok0:tok0 + 512], bk[:, :], rd=[bk], wr=[dst])
            R.done(ci)
            if CUT < 20:
                return
            pooledT, ypoolT = T['pooledT'], T['ypoolT']
            for g in range(4):
                bk = PS()
                gs = slice(g * 128, (g + 1) * 128)
                for i in range(4):
                    tt = st * 4 + i
                    o = bk[:, i * 128:(i + 1) * 128]
                    if tt == 0:
                        MM(o, u_tm[:, 0, gs], ptfirst[:, g, :], rd=[u_tm, ptfirst], wr=[bk])
                    else:
                        MM(o, u_tm[:, i, gs], ptcur[:, g, :], start=True, stop=False, rd=[u_tm, ptcur], wr=[bk])
                        if i > 0:
                            MM(o, u_tm[64:128, i - 1, gs], ptprev[64:128, g, :], start=False, stop=True, rd=[u_tm, ptprev], wr=[bk])
                        else:
                            MM(o, ucarry[64:128, gs], ptprev[64:128, g, :], start=False, stop=True, rd=[ucarry, ptprev], wr=[bk])
                ACOPY(pooledT[:, g, :], bk[:, :], rd=[bk], wr=[pooledT])
                bk2 = PS()
                MM(bk2[:, :], poolw[:, g, :], pooledT[:, g, :], rd=[poolw, pooledT], wr=[bk2])
                TS(ypoolT[:, g, :], bk2[:, :], pscT[:, g:g + 1], None, ALU.mult, rd=[bk2, pscT], wr=[ypoolT])
            CP(ucarry[:], u_tm[:, 3, :], rd=[u_tm], wr=[ucarry])
            if CUT < 30:
                return
            i0 = max(0, 32 * st - 1)
            i1 = 32 * (st + 1) - 1
            nb = i1 - i0
            hidk = T['hidk']
            for kv in range(2):
                src = kcT if kv == 0 else vcT
                ca, wa = R.get(w1chunk(kv, 0))
                cb_, wb = R.get(w1chunk(kv, 1))
                for g in range(2):
                    for cc in range(2):
                        bk = PS()
                        for l in range(32):
                            w = wa if l < 16 else wb
                            wv = V3(w[:, :], 16)
                            t0 = 16 * i0 + l
                            MM(bk[:, 0:nb], wv[64 * g:64 * g + 64, l % 16, cc * 128:(cc + 1) * 128],
                               src[64 * g:64 * g + 64, t0:t0 + 16 * (nb - 1) + 1:16],
                               start=(l == 0), stop=(l == 31), rd=[w, src], wr=[bk])
                        if kv == 0:
                            ACT(hidk[:, cc, 0:nb], bk[:, 0:nb], AF.Gelu_apprx_tanh, rd=[bk, biasc], wr=[hidk],
                                bias=biasc[:, kv, cc:cc + 1])
                        else:
                            ACT(hidv[:, g, cc, i0:i1], bk[:, 0:nb], AF.Gelu_apprx_tanh, rd=[bk, biasc], wr=[hidv],
                                bias=biasc[:, kv, cc:cc + 1])
                    bk = PS()
                    if kv == 0:
                        for cc in range(2):
                            MM(bk[64 * g:64 * g + 64, 0:nb], w2sb[:, 0, cc, :], hidk[:, cc, 0:nb],
                               start=(cc == 0), stop=(cc == 1), rd=[w2sb, hidk], wr=[bk])
                        CP(kcmpT[64 * g:64 * g + 64, i0:i1], bk[64 * g:64 * g + 64, 0:nb], rd=[bk], wr=[kcmpT])
                    else:
                        for cc in range(2):
                            MM(bk[:, 0:64], hidv[:, g, cc, :], w2sb[:, 1, cc, :],
                               start=(cc == 0), stop=(cc == 1), rd=[w2sb, hidv], wr=[bk])
                        CP(vcaug[:, g, 0:64], bk[:, 0:64], rd=[bk], wr=[vcaug])
                R.done(ca)
                R.done(cb_)
            if CUT < 40:
                return
            oattnT = T['oattnT']
            npe = [0]

            def next_pe():
                t = T[f'pe{npe[0] % 6}']
                npe[0] += 1
                return t

            def rq_(g, i):
                return qz[g][:, :, i * 128:(i + 1) * 128]

            def attn_cmpA(i, qi, g, par):
                pc = T[f'pc{g}']
                bk = PS()
                MM(V3(bk[:, :], 4), kcmpT[:, :], rq_(g, i), rd=[kcmpT, qz[g]], wr=[bk])
                ACT(pc[:], bk[:, :], AF.Exp, rd=[bk], wr=[pc], scale=0.125)
                TT(V3(pc[:, :], 4), V3(pc[:, :], 4), maskC[:, qi, :].unsqueeze(1).broadcast_to([128, 4, 128]), ALU.mult,
                   rd=[pc, maskC], wr=[pc])

            def attn_cmpB(i, qi, g, par):
                ocr = T[f'ocrp{par}']
                pcm = T[f'pc{g}']
                bk2 = PS()
                for hh in range(4):
                    MM(bk2[:, hh * 97:(hh + 1) * 97], pcm[:, hh * 128:(hh + 1) * 128], vcaug[:, g, :], rd=[pcm, vcaug], wr=[bk2])
                ACOPY(ocr[:, g].rearrange("p a b -> p (a b)"), bk2[:, 0:388], rd=[bk2], wr=[ocr])

            def attn_select(i, qi, par):
                ocr = T[f'ocrp{par}']
                rs, tmp4, psc, score, m8, selb = T['rs'], T['tmp4'], T['psc'], T['score'], T['m8'], T['selb']
                TS(rs[:], ocr[:, :, :, 64], 1e-30, None, ALU.max, rd=[ocr], wr=[rs])
                P.emit('dve', lambda e: e.reciprocal(out=rs[:], in_=rs[:]), [rs], [rs])
                TT(tmp4[:], ocr[:, :, :, 65:97], rs[:].unsqueeze(3).broadcast_to([128, 2, 4, 32]), ALU.mult, rd=[ocr, rs], wr=[tmp4])
                P.emit('dve', lambda e: e.tensor_reduce(out=psc[:], in_=tmp4[:].rearrange("p g h j -> p g j h"), axis=AX.X, op=ALU.add),
                       [tmp4], [psc])
                TT(score[:], psc[:], selvalid[:, qi, :].unsqueeze(1).broadcast_to([128, 2, 32]), ALU.mult, rd=[psc, selvalid], wr=[score])
                TT(score[:], score[:], selbiasc[:, qi, :].unsqueeze(1).broadcast_to([128, 2, 32]), ALU.add, rd=[score, selbiasc], wr=[score])
                for g in range(2):
                    P.emit('dve', lambda e, g=g: e.max(out=m8[:, g, :], in_=score[:, g, :]), [score], [m8])
                TT(score[:], score[:], m8[:, :, 7].unsqueeze(2).broadcast_to([128, 2, 32]), ALU.is_ge, rd=[score, m8], wr=[score])
                TS(selb[:], score[:], 1.0, 30000.0, ALU.subtract, ALU.mult, rd=[score], wr=[selb])

            def attn_select2(i, qi, par):
                selb = T['selb']
                bkt = PS()
                bktv = bkt[:, :].bitcast(BF16)
                for g in range(2):
                    sp_ = slice(64 * g, 64 * g + 32)
                    TR(bktv[sp_, 0:128], selb[:, g, :], identb[:], rd=[selb], wr=[bkt])
                for g in range(2):
                    sp_ = slice(64 * g, 64 * g + 32)
                    selbT = T[f'selbT{par}{g}']
                    CP(selbT[sp_], bktv[sp_, 0:128].unsqueeze(1).broadcast_to([32, 4, 128]), rd=[bkt], wr=[selbT])

            def attn_combine(i, qi, par, oattn):
                ocr, osel, owin = T[f'ocrp{par}'], T['osel'], T['owin']
                den, coef, o1, o2 = T['den'], T['coef'], T['o1'], T['o2']
                CP(den[:, 0], ocr[:, :, :, 64], rd=[ocr], wr=[den])
                CP(den[:, 1], osel[:, :, :, 64], rd=[osel], wr=[den])
                CP(den[:, 2], owin[:, :, :, 64], rd=[owin], wr=[den])
                TS(den[:], den[:], 1e-30, None, ALU.max, rd=[den], wr=[den])
                P.emit('dve', lambda e: e.reciprocal(out=den[:], in_=den[:]), [den], [den])
                gview = bg[:, qi, :].rearrange("p (r g h) -> p r g h", r=3, g=2)
                TT(coef[:], den[:], gview, ALU.mult, rd=[den, bg], wr=[coef])

                def bc(r):
                    return coef[:, r].unsqueeze(3).broadcast_to([128, 2, 4, 64])
                TT(o1[:], ocr[:, :, :, 0:64], bc(0), ALU.mult, rd=[ocr, coef], wr=[o1])
                TT(o2[:], osel[:, :, :, 0:64], bc(1), ALU.mult, rd=[osel, coef], wr=[o2])
                TT(o1[:], o1[:], o2[:], ALU.add, rd=[o1, o2], wr=[o1])
                TT(o2[:], owin[:, :, :, 0:64], bc(2), ALU.mult, rd=[owin, coef], wr=[o2])
                TT(oattn[:, :].rearrange("p (g h d) -> p g h d", g=2, h=4), o1[:], o2[:], ALU.add, rd=[o1, o2], wr=[oattn])

            def attn_transposes(i, oattn):
                bkt = PS()
                bktv = bkt[:, :].bitcast(BF16)
                for c in range(4):
                    TR(bktv[:, c * 128:(c + 1) * 128], oattn[:, c * 128:(c + 1) * 128], identb[:], rd=[oattn], wr=[bkt])
                CP(oattnT[:, :, i * 128:(i + 1) * 128], V3(bktv[:, 0:512], 4), rd=[bkt], wr=[oattnT])

            def prep_stages(i):
                qi_ = st * 4 + i
                pr = i % 2
                return [
                    lambda: [attn_cmpA(i, qi_, g, pr) for g in range(2)],
                    lambda: [attn_cmpB(i, qi_, g, pr) for g in range(2)],
                    lambda: attn_select(i, qi_, pr),
                    lambda: attn_select2(i, qi_, pr),
                ]

            pending_tr = None
            for f_ in prep_stages(0):
                f_()
            for i in range(4):
                qi = st * 4 + i
                par = i % 2
                oattn = T[f'oattn{i % 2}']
                hooks = {}
                if pending_tr is not None:
                    ptr_ = pending_tr
                    hooks[1] = lambda ptr_=ptr_: attn_transposes(*ptr_)
                    pending_tr = None
                if i + 1 < 4:
                    for pos, f_ in zip([2, 5, 8, 11], prep_stages(i + 1)):
                        hooks[pos] = f_
                tasks = []
                k0 = max(0, qi - 4)
                for g in range(2):
                    for kj in range(k0, qi + 1):
                        tasks.append(('win', g, kj, kj == k0, kj == qi))
                    for kj in range(qi + 1):
                        tasks.append(('sel', g, kj, kj == 0, kj == qi))
                nt = len(tasks)
                sbank = [None] * nt
                ptile = [None] * nt

                def S_(t):
                    br, g, kj, first, last = tasks[t]
                    bk = PS()
                    sbank[t] = bk
                    if br == 'win':
                        MM(V3(bk[:, :], 4), kwT[:, kj * 128:(kj + 1) * 128], rq_(g, i), rd=[kwT, qz[g]], wr=[bk])
                    else:
                        selbT = T[f'selbT{par}{g}']
                        MM(V3(bk[:, :], 4), ksT[:, kj * 128:(kj + 1) * 128], rq_(g, i), start=True, stop=False, rd=[ksT, qz[g]], wr=[bk])
                        MM(V3(bk[:, :], 4), Eall[:, kj * 128:(kj + 1) * 128], selbT[:], start=False, stop=True,
                           rd=[selbT], wr=[bk])

                def E_(t):
                    br, g, kj, first, last = tasks[t]
                    bk = sbank[t]
                    pt = next_pe()
                    ptile[t] = pt
                    ACT(pt[:], bk[:, :], AF.Exp, rd=[bk], wr=[pt], scale=0.125)
                    if kj == qi:
                        TT(pt[:], pt[:], maskD4[:], ALU.mult, rd=[pt], wr=[pt])
                    elif br == 'win' and kj == qi - 4:
                        TT(pt[:], pt[:], maskW4[:], ALU.mult, rd=[pt], wr=[pt])

                def V_(t):
                    br, g, kj, first, last = tasks[t]
                    pt = ptile[t]
                    accb = ACCB if br == 'win' else ACCA
                    vaug = vw_aug if br == 'win' else vs_aug
                    for hh in range(4):
                        MM(accb[:, hh * 65:(hh + 1) * 65], pt[:, hh * 128:(hh + 1) * 128], vaug[:, kj, g, :],
                           start=(first and hh == 0), stop=last, rd=[pt, vaug], wr=[accb])
                    if last:
                        dst = T['owin'] if br == 'win' else T['osel']
                        ACOPY(dst[:, g].rearrange("p a b -> p (a b)"), accb[:, 0:260], rd=[accb], wr=[dst])

                for t in range(nt + 2):
                    if t < nt:
                        S_(t)
                    if 0 <= t - 1 < nt:
                        E_(t - 1)
                    if 0 <= t - 2 < nt:
                        V_(t - 2)
                    if t in hooks:
                        hooks.pop(t)()
                for pos in sorted(hooks):
                    hooks[pos]()
                attn_combine(i, qi, par, oattn)
                pending_tr = (i, oattn)
            attn_transposes(*pending_tr)
            if CUT < 50:
                return
            sig, t2, mixT, tmp = T['sig'], T['tmp'], T['mixT'], T['tmp']
            for side in range(2):
                cu, wu_ = R.get(wchunk((wup_d if side == 0 else wua_d)[:, :], 1024, nk=4))
                wuv = V3(wu_[:, :], 4)
                srcT = ypoolT if side == 0 else oattnT
                for mh in range(2):
                    c0 = 1816 + side * 1024 + mh * 512
                    cm, wm = R.get(wchunk(win_d[:, c0:c0 + 512], 512))
                    wmv = V3(wm[:, :], 8)
                    for dq in range(4):
                        dc = mh * 4 + dq
                        bka = PS()
                        for kc in range(4):
                            MM(bka[:, :], wuv[:, kc, dc * 128:(dc + 1) * 128], srcT[:, kc, :], start=(kc == 0), stop=(kc == 3),
                               rd=[wu_, srcT], wr=[bka])
                        bkg = PS()
                        for k in range(8):
                            MM(bkg[:, :], wmv[:, k, dq * 128:(dq + 1) * 128], h1T[:, k, :], start=(k == 0), stop=(k == 7),
                               rd=[wm, h1T], wr=[bkg])
                        ACT(sig[:], bkg[:, :], AF.Sigmoid, rd=[bkg], wr=[sig])
                        if side == 0:
                            TT(mixT[:, dc, :], sig[:], bka[:, :], ALU.mult, rd=[sig, bka], wr=[mixT])
                        else:
                            TT(t2[:], sig[:], bka[:, :], ALU.mult, rd=[sig, bka], wr=[t2])
                            TT(mixT[:, dc, :], t2[:], mixT[:, dc, :], ALU.add, rd=[t2, mixT], wr=[mixT])
                    R.done(cm)
                R.done(cu)
            if CUT < 60:
                return
            for dh in range(2):
                co, wo = R.get(wchunk(wout_d[:, dh * 512:(dh + 1) * 512], 512))
                wov = V3(wo[:, :], 8)
                for i in range(4):
                    bk = PS()
                    for k in range(8):
                        MM(bk[:, :], mixT[:, k, i * 128:(i + 1) * 128], wov[:, k, :], start=(k == 0), stop=(k == 7), rd=[mixT, wo], wr=[bk])
                    TT(tmp[:], bk[:, :], g1bc[:, dh * 512:(dh + 1) * 512], ALU.mult, rd=[bk, g1bc], wr=[tmp])
                    a_ = acc[:, jb + i, dh * 512:(dh + 1) * 512]
                    TT(a_, a_, tmp[:], ALU.add, rd=[tmp, ('acc', jb + i)], wr=[('acc', jb + i)])
                R.done(co)

        def alloc_moe(ph, tag):
            T = {}

            def a(name, shape, dt):
                T[name] = sb(f"{name}_{tag}", shape, dt, ph)
            a('sqj', [128, D], BF16)
            a('ss', [128, 8], F32)
            a('lnv', [128, 8], F32)
            a('rstd', [128, 8], F32)
            a('xn2_0', [128, D], F32)
            a('xn2_1', [128, D], F32)
            a('h2f_0', [128, 8, 128], F32)
            a('h2f_1', [128, 8, 128], F32)
            a('h2T', [128, 8, 1024], BF16)
            a('lg', [128, 8, 36], F32)
            a('sm', [128, 8, 8], F32)
            a('goh', [128, 8, 4], F32)
            a('eg', [128, 8, 4], F32)
            a('lesel', [128, 8, 8], F32)
            a('m8', [128, 8, 8], F32)
            a('c8', [128, 8, 8], F32)
            a('c8b', [128, 8, 8], F32)
            a('sg0', [128, 512], BF16)
            a('sg1', [128, 512], BF16)
            a('he0', [128, 4, 512], BF16)
            a('he1', [128, 4, 512], BF16)
            a('ot0', [128, D], F32)
            a('ot1', [128, D], F32)
            return T

        def moe_group(T, b, grp):
            ss, lnv, rstd, h2T = T['ss'], T['lnv'], T['rstd'], T['h2T']
            lg, sm, goh, eg, lesel, m8, c8, c8b = T['lg'], T['sm'], T['goh'], T['eg'], T['lesel'], T['m8'], T['c8'], T['c8b']
            for j in range(8):
                ACT(T['sqj'][:], acc[:, j, :], AF.Square, rd=[('acc', j)], wr=[T['sqj'], (ss.name, j)], accum_out=ss[:, j:j + 1])
            ACT(lnv[:], ss[:], AF.Ln, rd=[(ss.name, j) for j in range(8)], wr=[lnv], scale=1.0 / D, bias=EPS)
            ACT(rstd[:], lnv[:], AF.Exp, rd=[lnv], wr=[rstd], scale=-0.5)
            def pre_a(j):
                xn2 = T[f'xn2_{j % 2}']
                h2f = T[f'h2f_{j % 2}']
                ACT(xn2[:], acc[:, j, :], AF.Identity, rd=[('acc', j), rstd], wr=[xn2], scale=rstd[:, j:j + 1])
                tb = [PS(), PS()]
                for c in range(8):
                    TR(tb[c // 4][:, (c % 4) * 128:(c % 4 + 1) * 128], xn2[:, c * 128:(c + 1) * 128], identf[:], rd=[xn2], wr=[tb[c // 4]])
                for c in range(8):
                    src_ = tb[c // 4][:, (c % 4) * 128:(c % 4 + 1) * 128]
                    if c < 4:
                        TS(h2f[:, c, :], src_, s2T[:, c, b:b + 1], modT[:, 16 + c, b:b + 1],
                           ALU.mult, ALU.add, rd=[tb[c // 4], s2T, modT], wr=[(h2f.name, 0)])
                    else:
                        ACT(h2f[:, c, :], src_, AF.Identity, rd=[tb[c // 4], s2T, modT], wr=[(h2f.name, 1)],
                            scale=s2T[:, c, b:b + 1], bias=modT[:, 16 + c, b:b + 1])

            def pre_b(j):
                h2f = T[f'h2f_{j % 2}']
                hk = [(h2f.name, 0), (h2f.name, 1)]
                ACOPY(h2T[:, :, j * 128:(j + 1) * 128], h2f[:], rd=hk, wr=[h2T])
                bk = PS()
                for c in range(8):
                    MM(bk[:, 0:36], h2f[:, c, :], rw[:, c, :], start=(c == 0), stop=(c == 7), rd=hk + [rw], wr=[bk])
                TT(lg[:, j, :], bk[:, 0:36], rb_bc[:], ALU.add, rd=[bk, rb_bc], wr=[lg])

            pre_a(0)
            for j in range(8):
                if j + 1 < 8:
                    pre_a(j + 1)
                pre_b(j)
            def b3(ap2, n):
                return ap2.unsqueeze(2).broadcast_to([128, 8, n])
            L4 = lg[:, :, 0:4]
            gmax, gsum, gp_ = sm[:, 0, :], sm[:, 1, :], sm[:, 2, :]
            dd, ed, w0, w1 = sm[:, 3, :], sm[:, 4, :], sm[:, 5, :], sm[:, 6, :]
            P.emit('dve', lambda e: e.tensor_reduce(out=gmax, in_=L4, axis=AX.X, op=ALU.max), [lg], [sm])
            TT(goh[:], L4, b3(gmax, 4), ALU.is_equal, rd=[lg, sm], wr=[goh])
            TT(eg[:], L4, b3(gmax, 4), ALU.subtract, rd=[lg, sm], wr=[eg])
            ACT(eg[:], eg[:], AF.Exp, rd=[eg], wr=[eg])
            P.emit('dve', lambda e: e.tensor_reduce(out=gsum, in_=eg[:], axis=AX.X, op=ALU.add), [eg], [sm])
            P.emit('dve', lambda e: e.reciprocal(out=gp_, in_=gsum), [sm], [sm])
            TT(lesel[:], lg[:, :, 4:12], b3(goh[:, :, 0], 8), ALU.mult, rd=[lg, goh], wr=[lesel])
            for g in range(1, 4):
                TT(c8b[:], lg[:, :, 4 + 8 * g:12 + 8 * g], b3(goh[:, :, g], 8), ALU.mult, rd=[lg, goh], wr=[c8b])
                TT(lesel[:], lesel[:], c8b[:], ALU.add, rd=[lesel, c8b], wr=[lesel])
            for j in range(8):
                P.emit('dve', lambda e, j=j: e.max(out=m8[:, j, :], in_=lesel[:, j, :]), [lesel], [m8])
            TT(dd, m8[:, :, 1], m8[:, :, 0], ALU.subtract, rd=[m8], wr=[sm])
            ACT(ed, dd, AF.Exp, rd=[sm], wr=[sm])
            TS(w0, ed, 1.0, None, ALU.add, rd=[sm], wr=[sm])
            P.emit('dve', lambda e: e.reciprocal(out=w0, in_=w0), [sm], [sm])
            TT(w1, ed, w0, ALU.mult, rd=[sm], wr=[sm])
            TT(w0, w0, gp_, ALU.mult, rd=[sm], wr=[sm])
            TT(w1, w1, gp_, ALU.mult, rd=[sm], wr=[sm])
            TT(c8[:], lesel[:], b3(m8[:, :, 0], 8), ALU.is_equal, rd=[lesel, m8], wr=[c8])
            TT(c8[:], c8[:], b3(w0, 8), ALU.mult, rd=[c8, sm], wr=[c8])
            TT(c8b[:], lesel[:], b3(m8[:, :, 1], 8), ALU.is_equal, rd=[lesel, m8], wr=[c8b])
            TT(c8b[:], c8b[:], b3(w1, 8), ALU.mult, rd=[c8b, sm], wr=[c8b])
            TT(c8[:], c8[:], c8b[:], ALU.add, rd=[c8, c8b], wr=[c8])
            for g in range(4):
                TT(comb[:, :, 8 * g:8 * g + 8], c8[:], b3(goh[:, :, g], 8), ALU.mult, rd=[c8, goh], wr=[comb])
            for e_ in range(NEXP if CUT >= 80 else 0):
                cg, wg = R.get(wchunk(eg_d[e_], 512))
                cu, wu_ = R.get(wchunk(eu_d[e_], 512))
                cd, wd = R.get(wchunk(ed_d[e_], 1024, nk=4))
                wgv, wuv, wdv = V3(wg[:, :], 8), V3(wu_[:, :], 8), V3(wd[:, :], 4)
                TT(wdv, wdv, g2bc[:].unsqueeze(1).broadcast_to([128, 4, D]), ALU.mult, rd=[wd, g2bc], wr=[wd])
                for half in range(2):
                    he = T[f'he{half}']
                    ts_ = slice(half * 512, (half + 1) * 512)
                    for fc in range(4):
                        bg_ = PS()
                        for k in range(8):
                            MM(bg_[:, :], wgv[:, k, fc * 128:(fc + 1) * 128], h2T[:, k, ts_], start=(k == 0), stop=(k == 7), rd=[wg, h2T], wr=[bg_])
                        bu_ = PS()
                        for k in range(8):
                            MM(bu_[:, :], wuv[:, k, fc * 128:(fc + 1) * 128], h2T[:, k, ts_], start=(k == 0), stop=(k == 7), rd=[wu_, h2T], wr=[bu_])
                        sg = T[f'sg{fc % 2}']
                        ACT(sg[:], bg_[:, :], AF.Silu, rd=[bg_], wr=[sg])
                        TT(he[:, fc, :], sg[:], bu_[:, :], ALU.mult, rd=[sg, bu_], wr=[he])
                    if half == 1:
                        R.done(cg)
                        R.done(cu)
                    for i in range(4):
                        j = half * 4 + i
                        for dh in range(2):
                            by = PS()
                            for fc in range(4):
                                MM(by[:, :], he[:, fc, i * 128:(i + 1) * 128], wdv[:, fc, dh * 512:(dh + 1) * 512],
                                   start=(fc == 0), stop=(fc == 3), rd=[he, wd], wr=[by])
                            a_ = acc[:, j, dh * 512:(dh + 1) * 512]
                            STT(a_, by[:, :], comb[:, j, e_:e_ + 1], a_, ALU.mult, ALU.add, rd=[by, comb, ('acc', j)], wr=[('acc', j)])
                R.done(cd)
            for j in range(8):
                ACT(T['sqj'][:], acc[:, j, :], AF.Square, rd=[('acc', j)], wr=[T['sqj'], (ss.name, j)], accum_out=ss[:, j:j + 1])
            ACT(lnv[:], ss[:], AF.Ln, rd=[(ss.name, j) for j in range(8)], wr=[lnv], scale=1.0 / D, bias=EPS)
            ACT(rstd[:], lnv[:], AF.Exp, rd=[lnv], wr=[rstd], scale=-0.5)
            for j in range(8):
                ot = T[f'ot{j % 2}']
                STT(ot[:], acc[:, j, :], rstd[:, j:j + 1], fg_bc[:], ALU.mult, ALU.mult, rd=[('acc', j), rstd, fg_bc], wr=[ot])
                r0 = b * S + grp * 1024 + j * 128
                DMA('sp', y_d[r0:r0 + 128, :], ot[:], f'st{j % 2}', [ot], [('yout', j % 2)])

        P.plan = True
        gen()
        P.plan = False
        gen()
        P.run()
    return nc


def _consts():
    f = np.float32
    c = {}
    c['identf'] = np.eye(128, dtype=f)
    k = np.arange(128)[:, None]
    q = np.arange(128)[None, :]
    c['maskD4'] = np.tile((k <= q).astype(f), (1, 4))
    c['maskW4'] = np.tile((k > q).astype(f), (1, 4))
    mc = np.zeros((128, 16, 128), f)
    n = np.arange(128)[:, None]
    for qi in range(16):
        t = qi * 128 + np.arange(128)[None, :]
        mc[:, qi, :] = ((16 * n + 31 <= t) & (n < 127)).astype(f)
    c['maskC'] = mc
    ea = np.zeros((32, 2048), f)
    ea[np.arange(2048) // 64, np.arange(2048)] = 1.0
    c['Eall'] = ea
    ptcur = np.zeros((128, 4, 128), f)
    ptfirst = np.zeros((128, 4, 128), f)
    ptprev = np.zeros((128, 4, 128), f)
    for g, w in enumerate((2, 4, 8, 16)):
        for t in range(128):
            for tp in range(max(0, t - w + 1), t + 1):
                ptcur[tp, g, t] += 1.0 / w
                ptfirst[tp, g, t] += 1.0 / min(t + 1, w)
            ptcur[t, g, t] -= 1.0
            ptfirst[t, g, t] -= 1.0
            for tp in range(t - w + 1, 0):
                ptprev[128 + tp, g, t] += 1.0 / w
    c['ptcur'], c['ptfirst'], c['ptprev'] = ptcur, ptfirst, ptprev
    s1 = np.arange(127)[:, None] * 16
    s2 = np.arange(32)[None, :] * 64
    ov = np.clip(np.minimum(s1 + 32, s2 + 64) - np.maximum(s1, s2), 0, None) / 32.0
    ovl = np.zeros((128, 33), f)
    ovl[:, 0] = 1.0
    ovl[:127, 1:] = ov
    c['ovl1'] = ovl
    sv = np.zeros((128, 16, 32), f)
    sbias = np.zeros((128, 16, 32), f)
    blk = np.arange(32)[None, :]
    for qi in range(16):
        t = qi * 128 + np.arange(128)
        cur = (t // 64)[:, None]
        valid = blk <= cur
        forced = (blk == 0) | (blk == cur) | (blk == cur - 1)
        sv[:, qi, :] = (valid & ~forced).astype(f)
        sbias[:, qi, :] = np.where(forced, 1e4, np.where(valid, 0.0, -1e30)).astype(f)
    c['selvalid'], c['selbias'] = sv, sbias
    return c


def _fm(v, nch):
    return np.ascontiguousarray(np.asarray(v, np.float32).reshape(nch, 128).T)


def prep_shared(inp):
    f = np.float32
    m = dict(_consts())
    m['ada_w'] = np.ascontiguousarray(inp['ada_w'][0], f)
    adab = np.asarray(inp['ada_b'][0], f)
    m['adabT'] = _fm(adab, 48)
    m['adab_bc'] = np.ascontiguousarray(np.broadcast_to(
        np.stack([adab[2048:3072], adab[5120:6144]], 0)[None], (128, 2, D)), f)
    m['n1g'] = _fm(inp['norm1_g'][0], 8)
    m['n2g'] = _fm(inp['norm2_g'][0], 8)
    m['fg_bc'] = np.ascontiguousarray(np.broadcast_to(np.asarray(inp['final_g'], f)[None], (128, D)), f)
    w_in = np.asarray(inp['w_in'][0], f)
    qperm = np.array([512 + (g * 4 + hh) * 64 + d for hh in range(4) for g in range(2) for d in range(64)])
    cols = np.concatenate([np.arange(0, 512), np.arange(1408, 1536), np.arange(1664, 1792), np.arange(1792, 1816),
                           qperm, np.arange(1024, 1152), np.arange(1152, 1280), np.arange(1280, 1408),
                           np.arange(1536, 1664), np.arange(1816, 3864)])
    m['w_in_p'] = np.ascontiguousarray(w_in[:, cols])
    m['pool_w_r'] = np.ascontiguousarray(np.transpose(np.asarray(inp['pool_w'][0], f), (1, 0, 2)))
    m['pscT'] = _fm(inp['pool_scale'][0], 4)
    pos = np.asarray(inp['cmp_pos'][0], f)
    pt = np.transpose(pos, (2, 0, 1))
    m['posT'] = np.ascontiguousarray(np.concatenate([pt, pt], 0))
    m['cmp_w1'] = np.ascontiguousarray(inp['cmp_w1'][0], f)
    b1 = np.asarray(inp['cmp_b1'][0], f)
    m['b1T'] = np.ascontiguousarray(np.transpose(b1.reshape(2, 2, 128), (2, 0, 1)))
    w2 = np.asarray(inp['cmp_w2'][0], f)
    m['w2r'] = np.ascontiguousarray(np.transpose(w2.reshape(2, 2, 128, 64), (2, 0, 1, 3)))
    m['w_up_pool'] = np.ascontiguousarray(inp['w_up_pool'][0], f)
    m['w_up_attn'] = np.ascontiguousarray(inp['w_up_attn'][0], f)
    m['w_out'] = np.ascontiguousarray(inp['w_out'][0], f)
    rwf = np.concatenate([np.asarray(inp['router_g_w'][0], f), np.asarray(inp['router_e_w'][0], f)], 1)
    m['rw'] = np.ascontiguousarray(np.transpose(rwf.reshape(8, 128, 36), (1, 0, 2)))
    rb = np.concatenate([np.asarray(inp['router_g_b'][0], f), np.asarray(inp['router_e_b'][0], f)], 0)
    m['rb_bc'] = np.ascontiguousarray(np.broadcast_to(rb[None], (128, 36)), f)
    m['exp_w_gate'] = np.ascontiguousarray(inp['exp_w_gate'][0], f)
    m['exp_w_up'] = np.ascontiguousarray(inp['exp_w_up'][0], f)
    m['exp_w_down'] = np.ascontiguousarray(inp['exp_w_down'][0], f)
    return m


def prep_core(inp, shared, b0, nseq):
    f = np.float32
    m = dict(shared)
    m['x'] = np.ascontiguousarray(np.asarray(inp['x'][b0:b0 + nseq], f).reshape(nseq * S, D))
    c = np.asarray(inp['c'][b0:b0 + nseq], f)
    ck = np.transpose(c.reshape(nseq, 8, 128), (2, 1, 0))
    m['cT'] = np.ascontiguousarray(ck)
    m['crep'] = np.ascontiguousarray(np.broadcast_to(np.transpose(c.reshape(nseq, 8, 128), (0, 2, 1))[:, :, :, None],
                                                     (nseq, 128, 8, 128)), f)
    return m


_NC_CACHE = {}


def kernel(**inputs):
    nseq = 32 // NCORES
    if nseq not in _NC_CACHE:
        _NC_CACHE[nseq] = build(nseq)
    nc = _NC_CACHE[nseq]
    shared = prep_shared(inputs)
    in_maps = [prep_core(inputs, shared, core * nseq, nseq) for core in range(NCORES)]
    res = run_bass_kernel_spmd(nc, in_maps, core_ids=list(range(NCORES)))
    out = np.concatenate([np.asarray(r["y"], np.float32).reshape(nseq, S, D) for r in res.results], axis=0)
    return out
```
